# Optimizing a Trainium2 kernel written in Bass

```python
import math
import jax, jax.numpy as jnp
from jax import lax
import numpy as np

D_MODEL = 2048
BATCH = 8
SEQ = 2048
DEPTH = 4

RET_HEADS = 8
RET_DK = 128
RET_DV = 128
RET_CHUNK = 128
GN_EPS = 1e-5
ML_HEADS = 8
ML_DK = 64
ML_DV = 128
ML_CHUNK = 128
GATE_SOFTCAP = 15.0
MB_HEADS = 8
MB_DH = 128
MB_BLOCK = 256
MB_TOPK = 3
MB_QCHUNK = 32
ROPE_THETA = 10000.0
D_FF = 5632
FFN_CONV = 3
EPS = 1e-6

RET_W = RET_HEADS * RET_DV
ML_W = ML_HEADS * ML_DV
MB_W = MB_HEADS * MB_DH
SPLIT_SIZES = (
    RET_HEADS * RET_DK, RET_HEADS * RET_DK, RET_W, RET_W,
    ML_HEADS * ML_DK, ML_HEADS * ML_DK, ML_W, ML_W, ML_HEADS, ML_HEADS,
    MB_W, MB_W, MB_W,
    D_MODEL, D_MODEL, D_MODEL,
)
N_IN = sum(SPLIT_SIZES)
SPLIT_IDX = tuple(int(s) for s in np.cumsum(SPLIT_SIZES)[:-1])

kernel_name = 'hybrid_retention_mlstm_moba_convglu'


def rms_norm(x, g):
    xf = x.astype(jnp.float32)
    y = xf * lax.rsqrt(jnp.mean(xf * xf, axis=-1, keepdims=True) + EPS)
    return (y * g.astype(jnp.float32)).astype(x.dtype)


def retnet_inv_freq():
    return 1.0 / (ROPE_THETA ** jnp.linspace(0.0, 1.0, RET_DK // 2, dtype=jnp.float32))


def rope_inv_freq():
    return 1.0 / (ROPE_THETA ** (jnp.arange(0, MB_DH, 2, dtype=jnp.float32) / MB_DH))


def rotary(x, inv_freq):
    S = x.shape[1]
    ang = jnp.arange(S, dtype=jnp.float32)[:, None] * inv_freq[None, :]
    cos = jnp.cos(ang)[None, :, None, :]
    sin = jnp.sin(ang)[None, :, None, :]
    x1, x2 = jnp.split(x.astype(jnp.float32), 2, axis=-1)
    return jnp.concatenate([x1 * cos - x2 * sin, x1 * sin + x2 * cos], axis=-1).astype(x.dtype)


def retention(q, k, v, g, gn_w):
    B, S = q.shape[0], q.shape[1]
    H, C, N = RET_HEADS, RET_CHUNK, S // RET_CHUNK
    f32 = jnp.float32
    qc = q.astype(f32).reshape(B, N, C, H, RET_DK)
    kc = (k.astype(f32) * RET_DK ** -0.5).reshape(B, N, C, H, RET_DK)
    vc = v.astype(f32).reshape(B, N, C, H, RET_DV)
    log_gamma = jnp.log1p(-jnp.exp2(-5.0 - jnp.arange(H, dtype=f32)))
    pos = jnp.arange(C, dtype=f32)
    diff = pos[:, None] - pos[None, :]
    decay = jnp.where(diff >= 0, jnp.exp(log_gamma[:, None, None] * jnp.maximum(diff, 0.0)), 0.0)
    scores = jnp.einsum('bnihd,bnjhd->bnhij', qc, kc) * decay
    intra = jnp.einsum('bnhij,bnjhe->bnihe', scores, vc)
    k_w = kc * jnp.exp((C - 1.0 - pos)[:, None] * log_gamma[None, :])[None, None, :, :, None]
    kv = jnp.einsum('bnjhd,bnjhe->bnhde', k_w, vc)
    chunk_decay = jnp.exp(C * log_gamma)[None, :, None, None]

    def step(state, kv_n):
        return state * chunk_decay + kv_n, state

    _, prev = lax.scan(step, jnp.zeros((B, H, RET_DK, RET_DV), f32), jnp.moveaxis(kv, 1, 0))
    prev = jnp.moveaxis(prev, 0, 1)
    q_w = qc * jnp.exp((pos + 1.0)[:, None] * log_gamma[None, :])[None, None, :, :, None]
    inter = jnp.einsum('bnihd,bnhde->bnihe', q_w, prev)
    o = (intra + inter).reshape(B, S, H, RET_DV)
    o = o - o.mean(axis=-1, keepdims=True)
    o = o * lax.rsqrt(jnp.mean(o * o, axis=-1, keepdims=True) + GN_EPS)
    o = o.reshape(B, S, H * RET_DV) * gn_w.astype(f32)
    return (jax.nn.silu(g.astype(f32)) * o).astype(q.dtype)


def softcap(x):
    return GATE_SOFTCAP * jnp.tanh(x / GATE_SOFTCAP)


def mlstm(q, k, v, o_pre, i_pre, f_pre, norm_w):
    B, S = q.shape[0], q.shape[1]
    H, C, N = ML_HEADS, ML_CHUNK, S // ML_CHUNK
    f32 = jnp.float32
    qc = q.astype(f32).reshape(B, N, C, H, ML_DK)
    kc = (k.astype(f32) * ML_DK ** -0.5).reshape(B, N, C, H, ML_DK)
    vc = v.astype(f32).reshape(B, N, C, H, ML_DV)
    i_log = softcap(i_pre.astype(f32)).reshape(B, N, C, H).transpose(0, 1, 3, 2)
    f_log = jax.nn.log_sigmoid(softcap(f_pre.astype(f32))).reshape(B, N, C, H).transpose(0, 1, 3, 2)
    a = jnp.cumsum(f_log, axis=-1)
    a_last = a[..., -1]
    causal = jnp.tril(jnp.ones((C, C), dtype=bool))
    d_log = jnp.where(causal, a[..., :, None] - a[..., None, :] + i_log[..., None, :], -jnp.inf)
    w_end = a_last[..., None] - a + i_log
    g_loc = jnp.max(w_end, axis=-1)
    w_exp = jnp.exp(w_end - g_loc[..., None])
    kv_loc = jnp.einsum('bnhj,bnjhd,bnjhe->bnhde', w_exp, kc, vc)
    n_loc = jnp.einsum('bnhj,bnjhd->bnhd', w_exp, kc)

    def step(carry, xs):
        c_st, n_st, m_st = carry
        kv_n, nn_n, g_n, a_n = xs
        m_new = jnp.maximum(a_n + m_st, g_n)
        s_old = jnp.exp(a_n + m_st - m_new)
        s_new = jnp.exp(g_n - m_new)
        c_new = s_old[..., None, None] * c_st + s_new[..., None, None] * kv_n
        n_new = s_old[..., None] * n_st + s_new[..., None] * nn_n
        return (c_new, n_new, m_new), (c_st, n_st, m_st)

    init = (jnp.zeros((B, H, ML_DK, ML_DV), f32), jnp.zeros((B, H, ML_DK), f32), jnp.zeros((B, H), f32))
    xs = (jnp.moveaxis(kv_loc, 1, 0), jnp.moveaxis(n_loc, 1, 0), jnp.moveaxis(g_loc, 1, 0), jnp.moveaxis(a_last, 1, 0))
    _, (c_prev, n_prev, m_prev) = lax.scan(step, init, xs)
    c_prev = jnp.moveaxis(c_prev, 0, 1)
    n_prev = jnp.moveaxis(n_prev, 0, 1)
    m_prev = jnp.moveaxis(m_prev, 0, 1)
    inter_log = a + m_prev[..., None]
    m_row = jnp.maximum(jnp.max(d_log, axis=-1), inter_log)
    qk = jnp.einsum('bnihd,bnjhd->bnhij', qc, kc) * jnp.exp(d_log - m_row[..., None])
    s_inter = jnp.exp(inter_log - m_row)
    num = jnp.einsum('bnhij,bnjhe->bnihe', qk, vc) + jnp.einsum('bnihd,bnhde->bnihe', qc, c_prev) * jnp.swapaxes(s_inter, 2, 3)[..., None]
    den = qk.sum(axis=-1) + jnp.einsum('bnihd,bnhd->bnhi', qc, n_prev) * s_inter
    den = jnp.maximum(jnp.abs(den), jnp.exp(-m_row))
    h = (num / jnp.swapaxes(den, 2, 3)[..., None]).reshape(B, S, H, ML_DV)
    h = h * lax.rsqrt(jnp.mean(h * h, axis=-1, keepdims=True) + EPS) * norm_w.astype(f32).reshape(H, ML_DV)
    return (jax.nn.sigmoid(o_pre.astype(f32)) * h.reshape(B, S, H * ML_DV)).astype(q.dtype)


def moba(q, k, v):
    B, S, H, D = q.shape
    nb = -(-S // MB_BLOCK)
    s_pad = nb * MB_BLOCK
    pad = ((0, 0), (0, s_pad - S), (0, 0), (0, 0))
    qh = jnp.pad(q, pad).transpose(0, 2, 1, 3)
    kb = jnp.pad(k, pad).transpose(0, 2, 1, 3).reshape(B, H, nb, MB_BLOCK, D)
    vb = jnp.pad(v, pad).transpose(0, 2, 1, 3).reshape(B, H, nb, MB_BLOCK, D)
    k_mean = kb.astype(jnp.float32).mean(axis=3)
    gate = jnp.einsum('bhsd,bhnd->bhsn', qh.astype(jnp.float32), k_mean)
    q_blk = jnp.arange(s_pad) // MB_BLOCK
    gate = jnp.where(jnp.arange(nb)[None, :] < q_blk[:, None], gate, -jnp.inf)
    topk = min(MB_TOPK, nb)
    _, idx = lax.top_k(gate, topk)
    valid = jnp.arange(topk)[None, :] < q_blk[:, None]
    n_qc = s_pad // MB_QCHUNK
    scale = D ** -0.5
    q_x = qh.reshape(B, H, n_qc, MB_QCHUNK, D).transpose(2, 0, 1, 3, 4)
    idx_x = idx.reshape(B, H, n_qc, MB_QCHUNK, topk).transpose(2, 0, 1, 3, 4)
    valid_x = valid.reshape(n_qc, MB_QCHUNK, topk)
    bi = jnp.arange(B)[:, None, None]
    hi = jnp.arange(H)[None, :, None]

    def attend(xs):
        ci, qc, ic, vm = xs
        blk = (ci * MB_QCHUNK) // MB_BLOCK
        qpos = ci * MB_QCHUNK + jnp.arange(MB_QCHUNK)
        kpos = blk * MB_BLOCK + jnp.arange(MB_BLOCK)
        k_own = lax.dynamic_index_in_dim(kb, blk, axis=2, keepdims=False)
        v_own = lax.dynamic_index_in_dim(vb, blk, axis=2, keepdims=False)
        s = jnp.einsum('bhqd,bhkd->bhqk', qc, k_own, preferred_element_type=jnp.float32) * scale
        s = jnp.where(kpos[None, :] <= qpos[:, None], s, -jnp.inf)
        m = jnp.max(s, axis=-1)
        p = jnp.exp(s - m[..., None])
        l = p.sum(axis=-1)
        acc = jnp.einsum('bhqk,bhkd->bhqd', p, v_own.astype(jnp.float32))
        for slot in range(topk):
            sel = ic[..., slot]
            k_s = kb[bi, hi, sel]
            v_s = vb[bi, hi, sel]
            sc = jnp.einsum('bhqd,bhqkd->bhqk', qc, k_s, preferred_element_type=jnp.float32) * scale
            sc = jnp.where(vm[:, slot][None, None, :, None], sc, -jnp.inf)
            m_new = jnp.maximum(m, jnp.max(sc, axis=-1))
            alpha = jnp.exp(m - m_new)
            p = jnp.exp(sc - m_new[..., None])
            l = alpha * l + p.sum(axis=-1)
            acc = alpha[..., None] * acc + jnp.einsum('bhqk,bhqkd->bhqd', p, v_s.astype(jnp.float32))
            m = m_new
        return (acc / l[..., None]).astype(q.dtype)

    out = lax.map(attend, (jnp.arange(n_qc), q_x, idx_x, valid_x))
    out = out.transpose(1, 0, 3, 2, 4).reshape(B, s_pad, H, D)
    return out[:, :S]


def conv_glu_ffn(h, w_up, conv_w, conv_b, w_down):
    S = h.shape[1]
    u = h @ w_up
    up = jnp.pad(u, ((0, 0), (FFN_CONV - 1, 0), (0, 0)))
    u = conv_b + sum(up[:, j:j + S] * conv_w[j] for j in range(FFN_CONV))
    a, b = jnp.split(u, 2, axis=-1)
    return (jax.nn.silu(a) * b) @ w_down


def setup_inputs(seed: int = 0) -> dict:
    key = jax.random.key(seed)
    ks = jax.random.split(key, 20)
    f32 = jnp.float32

    def nrm(k, shape, scale):
        return jax.random.normal(k, shape, f32) * scale

    return {
        'x': nrm(ks[0], (BATCH, SEQ, D_MODEL), 1.0),
        'w_in': nrm(ks[1], (DEPTH, D_MODEL, N_IN), D_MODEL ** -0.5),
        'b_ig': nrm(ks[2], (DEPTH, ML_HEADS), 0.1),
        'b_fg': jnp.linspace(3.0, 6.0, ML_HEADS, dtype=f32)[None, :] + nrm(ks[3], (DEPTH, ML_HEADS), 0.1),
        'norm_mix': 1.0 + nrm(ks[4], (DEPTH, D_MODEL), 0.1),
        'ret_gn': 1.0 + nrm(ks[5], (DEPTH, RET_W), 0.1),
        'ml_norm': 1.0 + nrm(ks[6], (DEPTH, ML_W), 0.1),
        'q_norm': 1.0 + nrm(ks[7], (DEPTH, MB_DH), 0.1),
        'k_norm': 1.0 + nrm(ks[8], (DEPTH, MB_DH), 0.1),
        'w_pa': nrm(ks[9], (DEPTH, RET_W, D_MODEL), RET_W ** -0.5),
        'w_pb': nrm(ks[10], (DEPTH, ML_W, D_MODEL), ML_W ** -0.5),
        'w_pc': nrm(ks[11], (DEPTH, MB_W, D_MODEL), MB_W ** -0.5),
        'w_out': nrm(ks[12], (DEPTH, D_MODEL, D_MODEL), D_MODEL ** -0.5),
        'norm_ffn': 1.0 + nrm(ks[13], (DEPTH, D_MODEL), 0.1),
        'w_up': nrm(ks[14], (DEPTH, D_MODEL, 2 * D_FF), D_MODEL ** -0.5),
        'conv_w': nrm(ks[15], (DEPTH, FFN_CONV, 2 * D_FF), FFN_CONV ** -0.5),
        'conv_b': nrm(ks[16], (DEPTH, 2 * D_FF), 0.01),
        'w_down': nrm(ks[17], (DEPTH, D_FF, D_MODEL), D_FF ** -0.5),
    }


def reference(x, w_in, b_ig, b_fg, norm_mix, ret_gn, ml_norm, q_norm, k_norm, w_pa, w_pb, w_pc, w_out, norm_ffn, w_up, conv_w, conv_b, w_down):
    B, S = x.shape[0], x.shape[1]
    ret_freq = retnet_inv_freq()
    rope_freq = rope_inv_freq()
    for l in range(DEPTH):
        h = rms_norm(x, norm_mix[l])
        z = h @ w_in[l]
        (rq, rk, rv, rg, mq, mk, mv, mo, mi, mf, aq, ak, av, ga, gb, gc) = jnp.split(z, SPLIT_IDX, axis=-1)
        rq = rotary(rq.reshape(B, S, RET_HEADS, RET_DK), ret_freq)
        rk = rotary(rk.reshape(B, S, RET_HEADS, RET_DK), ret_freq)
        y_a = retention(rq, rk, rv.reshape(B, S, RET_HEADS, RET_DV), rg, ret_gn[l])
        y_b = mlstm(mq.reshape(B, S, ML_HEADS, ML_DK), mk.reshape(B, S, ML_HEADS, ML_DK),
                    mv.reshape(B, S, ML_HEADS, ML_DV), mo, mi + b_ig[l], mf + b_fg[l], ml_norm[l])
        aq = rotary(rms_norm(aq.reshape(B, S, MB_HEADS, MB_DH), q_norm[l]), rope_freq)
        ak = rotary(rms_norm(ak.reshape(B, S, MB_HEADS, MB_DH), k_norm[l]), rope_freq)
        y_c = moba(aq, ak, av.reshape(B, S, MB_HEADS, MB_DH)).reshape(B, S, MB_W)
        merged = (jax.nn.sigmoid(ga) * (y_a @ w_pa[l]) + jax.nn.sigmoid(gb) * (y_b @ w_pb[l])
                  + jax.nn.sigmoid(gc) * (y_c @ w_pc[l]))
        x = x + merged @ w_out[l]
        x = x + conv_glu_ffn(rms_norm(x, norm_ffn[l]), w_up[l], conv_w[l], conv_b[l], w_down[l])
    return x
```

```python
import math
from contextlib import ExitStack

import numpy as np
import concourse.bass as bass
import concourse.mybir as mybir
from concourse.bass_utils import run_bass_kernel_spmd

F32 = mybir.dt.float32
BF16 = mybir.dt.bfloat16
AF = mybir.ActivationFunctionType
ALU = mybir.AluOpType
AX = mybir.AxisListType

ENGS = ["pe", "act", "dve", "pool", "sp"]

DEPTH = 4
SEQ = 2048
DM = 2048
NT = 16
N_IN = 16400
D_FF = 5632
NFC = 44
C_RQ, C_RK, C_RV, C_RG = 0, 1024, 2048, 3072
C_MQ, C_MK, C_MV, C_MO, C_MI, C_MF = 4096, 4608, 5120, 6144, 7168, 7176
C_AQ, C_AK, C_AV = 7184, 8208, 9232
C_GA = 10256


class Buf:
    __slots__ = ("name", "last_w", "readers")

    def __init__(self, name=""):
        self.name = name
        self.last_w = None
        self.readers = {}


class DSem:
    __slots__ = ("name", "count")

    def __init__(self, name):
        self.name = name
        self.count = 0


class Sched:
    def __init__(self, nc):
        self.nc = nc
        self.streams = {e: [] for e in ENGS}
        self.cnt = {e: 0 for e in ENGS}
        self.waited = {e: {} for e in ENGS}
        self.semnames = list(ENGS)
        self.dsems = []

    def dsem(self):
        d = DSem("d%d" % len(self.dsems))
        self.dsems.append(d)
        self.semnames.append(d.name)
        return d

    def _collect(self, eng, reads, writes):
        deps = {}
        for b in reads:
            t = b.last_w
            if t is not None and deps.get(t[0], 0) < t[1]:
                deps[t[0]] = t[1]
        for b in writes:
            t = b.last_w
            if t is not None and deps.get(t[0], 0) < t[1]:
                deps[t[0]] = t[1]
            for s, v in b.readers.items():
                if deps.get(s, 0) < v:
                    deps[s] = v
        waits = []
        w = self.waited[eng]
        for s, v in deps.items():
            if s == "pe" and eng == "pe":
                continue
            if w.get(s, 0) >= v:
                continue
            w[s] = v
            waits.append((s, v))
        return waits

    def _commit(self, tok, reads, writes):
        s, v = tok
        for b in reads:
            if b.readers.get(s, 0) < v:
                b.readers[s] = v
        for b in writes:
            b.last_w = tok
            b.readers = {}

    def op(self, eng, fn, reads=(), writes=(), inc=True):
        waits = self._collect(eng, reads, writes)
        if inc:
            self.cnt[eng] += 1
            tok = (eng, self.cnt[eng])
        else:
            tok = (eng, self.cnt[eng] + 1)
        self._commit(tok, reads, writes)
        self.streams[eng].append((fn, waits, eng if inc else None, 1))

    def dma(self, q, out, in_, reads, writes, dsem):
        waits = self._collect(q, reads, writes)
        dsem.count += 16
        tok = (dsem.name, dsem.count)
        self._commit(tok, reads, writes)
        self.streams[q].append(
            (lambda e, out=out, in_=in_: e.dma_start(out=out, in_=in_), waits, dsem.name, 16))

    def barrier(self):
        cur = {e: self.cnt[e] for e in ENGS if self.cnt[e] > 0}
        for d in self.dsems:
            if d.count > 0:
                cur[d.name] = d.count
        for e in ENGS:
            w = self.waited[e]
            waits = []
            for s, v in cur.items():
                if s == e and e == "pe":
                    w[s] = v
                    continue
                if w.get(s, 0) >= v:
                    continue
                w[s] = v
                waits.append((s, v))
            if waits:
                self.streams[e].append((None, waits, None, 0))

    def emit(self, stack):
        nc = self.nc
        sems = {}
        for n in self.semnames:
            sems[n] = stack.enter_context(nc.semaphore("s_" + n))
        block = stack.enter_context(nc.Block())
        handles = {"pe": block.tensor, "act": block.scalar, "dve": block.vector,
                   "pool": block.gpsimd, "sp": block.sync}
        for e in ENGS:
            stream = self.streams[e]

            def body(eng, stream=stream):
                for fn, waits, incsem, incv in stream:
                    for s, v in waits:
                        eng.wait_ge(sems[s], v)
                    if fn is None:
                        continue
                    ins = fn(eng)
                    if incsem is not None:
                        ins.then_inc(sems[incsem], incv)
            handles[e](body)


def _dtsize(dt):
    return 2 if dt == BF16 else 4


class KB:
    def __init__(self, n_layers=DEPTH, dbg=False, wdepth=DEPTH):
        self.n_layers = n_layers
        self.wdepth = wdepth
        self.dbg = dbg
        self.nc = bass.Bass("TRN2", target_bir_lowering=False)
        self.S = Sched(self.nc)

    def tt(self, eng, out, in0, in1, op, R, W):
        self.S.op(eng, lambda e: e.tensor_tensor(out=out, in0=in0, in1=in1, op=op), R, W)

    def ts(self, eng, out, in0, s1, s2, op0, op1, R, W):
        if s2 is None:
            self.S.op(eng, lambda e: e.tensor_scalar(out=out, in0=in0, scalar1=s1, scalar2=None, op0=op0), R, W)
        else:
            self.S.op(eng, lambda e: e.tensor_scalar(out=out, in0=in0, scalar1=s1, scalar2=s2, op0=op0, op1=op1), R, W)

    def stt(self, out, in0, scalar, in1, op0, op1, R, W):
        self.S.op("dve", lambda e: e.scalar_tensor_tensor(out=out, in0=in0, scalar=scalar, in1=in1, op0=op0, op1=op1), R, W)

    def act(self, out, in_, func, R, W, scale=1.0, bias=0.0):
        self.S.op("act", lambda e: e.activation(out=out, in_=in_, func=func, bias=bias, scale=scale), R, W)

    def cp(self, eng, out, in_, R, W):
        if eng == "act":
            self.S.op("act", lambda e: e.activation(out=out, in_=in_, func=AF.Copy), R, W)
        else:
            self.S.op(eng, lambda e: e.tensor_copy(out=out, in_=in_), R, W)

    def mm(self, out, lhsT, rhs, start, stop, R, W, inc):
        self.S.op("pe", lambda e: e.matmul(out, lhsT=lhsT, rhs=rhs, start=start, stop=stop), R, W, inc=inc)

    def tr(self, out, in_, ident, R, W, inc):
        self.S.op("pe", lambda e: e.transpose(out=out, in_=in_, identity=ident), R, W, inc=inc)

    def red(self, out, in_, op, R, W):
        self.S.op("dve", lambda e: e.tensor_reduce(out=out, in_=in_, axis=AX.X, op=op), R, W)

    def recip(self, out, in_, R, W):
        self.S.op("dve", lambda e: e.reciprocal(out=out, in_=in_), R, W)

    def memset(self, eng, ap, val, R, W):
        self.S.op(eng, lambda e: e.memset(ap, val), R, W)

    def arena_reset(self):
        self.aoff = 0

    def alloc(self, shape, dt):
        nel = 1
        for s in shape[1:]:
            nel *= s
        nbytes = nel * _dtsize(dt)
        nbytes = (nbytes + 31) // 32 * 32
        off = self.aoff
        self.aoff += nbytes
        assert self.aoff <= self.arena_bytes, ("arena overflow", self.aoff, self.arena_bytes)
        w0 = off // 4
        ap = self.arena[0:shape[0], w0:w0 + nbytes // 4]
        if dt != F32:
            ap = ap.bitcast(dt)
        ap = ap[:, 0:nel]
        if len(shape) == 3:
            ap = ap.rearrange("p (a b) -> p a b", a=shape[1])
        elif len(shape) == 4:
            ap = ap.rearrange("p (a b c) -> p a b c", a=shape[1], b=shape[2])
        return ap

    def bufs(self, n):
        return [Buf() for _ in range(n)]

    def build(self):
        nc = self.nc
        S = self.S
        dbg = self.dbg
        L = self.n_layers
        self.stack = ExitStack()
        st = self.stack

        def din(name, shape, dt=F32):
            return nc.dram_tensor(name, list(shape), dt, kind="ExternalInput").ap()

        def dscr(name, shape, dt=F32):
            kind = "ExternalOutput" if dbg else "Internal"
            return nc.dram_tensor(name, list(shape), dt, kind=kind).ap()

        self.xT = din("xT", [DM, SEQ])
        self.w_in = din("w_in", [self.wdepth, DM, N_IN])
        self.w_pa = din("w_pa", [self.wdepth, 1024, DM])
        self.w_pb = din("w_pb", [self.wdepth, 1024, DM])
        self.w_pc = din("w_pc", [self.wdepth, 1024, DM])
        self.w_out = din("w_out", [self.wdepth, DM, DM])
        self.w_up = din("w_up", [self.wdepth, DM, 2 * D_FF])
        self.w_down = din("w_down", [self.wdepth, D_FF, DM])
        self.i_norm_mix = din("norm_mix_t", [self.wdepth, 128, 16])
        self.i_norm_ffn = din("norm_ffn_t", [self.wdepth, 128, 16])
        self.i_big = din("b_ig_t", [self.wdepth, 8, 1])
        self.i_bfg = din("b_fg_t", [self.wdepth, 8, 1])
        self.i_retgn = din("ret_gn_rep", [self.wdepth, 128, 1024])
        self.i_mlnorm = din("ml_norm_rep", [self.wdepth, 128, 1024])
        self.i_qnorm = din("q_norm_rep", [self.wdepth, 128, 128])
        self.i_knorm = din("k_norm_rep", [self.wdepth, 128, 128])
        self.i_convw = din("conv_w_t", [self.wdepth, 128, 88, 3])
        self.i_convb = din("conv_b_t", [self.wdepth, 128, 88])
        self.i_tabs = din("c_tabs", [128, 4, 16, 64])
        self.i_small = din("c_small", [128, 24])
        self.i_mask = din("c_mask", [128, 128])
        self.i_ident = din("c_ident", [128, 128])
        self.i_negm = din("c_negm", [128, 8, 8])
        self.i_selfix = din("c_selfix", [128, 2, 8, 8])
        self.outT = nc.dram_tensor("outT", [DM, SEQ], F32, kind="ExternalOutput").ap()
        self.xres = dscr("xres", [DM, SEQ])
        self.z = {}
        for nm, w in (("rq", 1024), ("rk", 1024), ("rv", 1024), ("rg", 1024), ("mq", 512), ("mk", 512),
                      ("mv", 1024), ("mo", 1024), ("aq", 1024), ("ak", 1024), ("av", 1024)):
            self.z[nm] = dscr("z_" + nm, [SEQ, w], F32 if nm in ("aq", "ak") else BF16)
        self.sgT = dscr("sgT", [3 * DM, SEQ], BF16)
        self.yT = [dscr("yT%d" % b, [1024, SEQ], BF16) for b in range(3)]
        self.actT = dscr("actT", [D_FF, SEQ], BF16)
        self.Bx = [[Buf() for _ in range(4)] for _ in range(16)]
        self.Bz = {nm: [[Buf() for _ in range(2)] for _ in range(NT)] for nm in self.z}
        self.Bsg = [[Buf() for _ in range(4)] for _ in range(48)]
        self.ByT = [[Buf() for _ in range(32)] for _ in range(3)]
        self.Bact = [[Buf() for _ in range(4)] for _ in range(NFC)]

        def sb(name, shape, dt):
            return st.enter_context(nc.sbuf_tensor(name, list(shape), dt))

        self.tabs = sb("tabs", [128, 4, 16, 64], F32)
        self.small = sb("small", [128, 24], F32)
        self.maskf = sb("maskf", [128, 128], F32)
        self.maskb = sb("maskb", [128, 128], BF16)
        self.identf = sb("identf", [128, 128], F32)
        self.identb = sb("identb", [128, 128], BF16)
        self.onesf = sb("onesf", [128, 128], F32)
        self.onescb = sb("onescb", [128, 2], BF16)
        self.onesb = sb("onesb", [128, 128], BF16)
        self.kmcol = sb("kmcol", [128, 2], F32)
        self.negm = sb("negm", [128, 8, 8], F32)
        self.selfix = sb("selfix", [128, 2, 8, 8], F32)
        self.gmix = sb("gmix", [128, 16], F32)
        self.gffn = sb("gffn", [128, 16], F32)
        self.big = sb("big", [8, 1], F32)
        self.bfg = sb("bfg", [8, 1], F32)
        self.retgn = sb("retgn", [128, 1024], F32)
        self.mlnorm = sb("mlnorm", [128, 1024], F32)
        self.qnw = sb("qnw", [128, 128], F32)
        self.knw = sb("knw", [128, 128], F32)
        self.convw = sb("convw", [128, 88, 3], F32)
        self.convb = sb("convb", [128, 88], F32)
        self.mtab = sb("mtab", [128, 3, 16, 8], F32)
        self.Bconst = Buf()
        self.Blayer = Buf()
        self.Bmtab = Buf()
        self.arena_bytes = (nc.sbuf_bytes_remaining // 32) * 32 - 64
        self.arena = sb("arena", [128, self.arena_bytes // 4], F32)
        self.bank = [st.enter_context(nc.psum_tensor("bank%d" % i, [128, 512], F32)) for i in range(8)]
        self.Bbank = [Buf() for _ in range(8)]
        self.ds = [S.dsem() for _ in range(40)]
        self.dconst = S.dsem()

        dc = self.dconst
        S.dma("sp", self.tabs[:], self.i_tabs[:, :, :, :], [], [self.Bconst], dc)
        S.dma("sp", self.small[:], self.i_small[:, :], [], [self.Bconst], dc)
        S.dma("sp", self.maskf[:], self.i_mask[:, :], [], [self.Bconst], dc)
        S.dma("sp", self.identf[:], self.i_ident[:, :], [], [self.Bconst], dc)
        S.dma("sp", self.negm[:], self.i_negm[:, :, :], [], [self.Bconst], dc)
        S.dma("sp", self.selfix[:], self.i_selfix[:, :, :, :], [], [self.Bconst], dc)
        S.barrier()
        self.cp("dve", self.maskb[:], self.maskf[:], [self.Bconst], [self.Bconst])
        self.cp("dve", self.identb[:], self.identf[:], [self.Bconst], [self.Bconst])
        self.memset("dve", self.onesf[:], 1.0, [], [self.Bconst])
        self.memset("dve", self.onescb[:], 1.0, [], [self.Bconst])
        self.memset("dve", self.onesb[:], 1.0, [], [self.Bconst])
        self.memset("dve", self.kmcol[:], 1.0 / 256.0, [], [self.Bconst])
        S.barrier()

        for l in range(L):
            self.layer(l)

        S.barrier()
        S.emit(st)
        return nc

    def layer(self, l):
        S = self.S
        dc = self.dconst
        xsrc = self.xT if l == 0 else self.xres
        xdst_final = self.outT if l == self.n_layers - 1 else self.xres
        for dst, src in ((self.gmix, self.i_norm_mix[l]), (self.gffn, self.i_norm_ffn[l]),
                         (self.big, self.i_big[l]), (self.bfg, self.i_bfg[l]),
                         (self.retgn, self.i_retgn[l]), (self.mlnorm, self.i_mlnorm[l]),
                         (self.qnw, self.i_qnorm[l]), (self.knw, self.i_knorm[l]),
                         (self.convw, self.i_convw[l]), (self.convb, self.i_convb[l])):
            S.dma("sp", dst[:], src, [], [self.Blayer], dc)
        S.barrier()
        self.ts("dve", self.qnw[:], self.qnw[:], 128.0 ** -0.5, None, ALU.mult, None, [self.Blayer], [self.Blayer])
        S.barrier()

        self.phase_norm(xsrc, self.gmix)
        self.phase_proj(l)
        S.barrier()
        self.phase_moba(l)
        S.barrier()
        if self.stop_after == "C":
            return
        self.phase_merge(l, xsrc)
        S.barrier()
        if self.stop_after == "G":
            return
        self.phase_norm(self.xres, self.gffn)
        self.phase_ffn_up(l)
        S.barrier()
        if self.stop_after == "F1":
            return
        self.phase_ffn_down(l, xdst_final)
        S.barrier()

    stop_after = None

    def phase_norm(self, xsrc, g):
        S = self.S
        self.arena_reset()
        self.hT = self.alloc([128, 16, SEQ], BF16)
        self.BhT = [[Buf() for _ in range(8)] for _ in range(16)]
        mark = self.aoff
        xb = [self.alloc([128, 16, 256], F32) for _ in range(2)]
        Bxb = self.bufs(2)
        sq = [self.alloc([128, 256], F32) for _ in range(4)]
        Bsq = self.bufs(4)
        sd = [self.alloc([128, 256], F32) for _ in range(2)]
        Bsd = self.bufs(2)
        xv = xsrc.rearrange("(c p) t -> p c t", p=128)
        for t in range(8):
            b = t % 2
            S.dma("sp", xb[b], xv[:, :, t * 256:(t + 1) * 256], [self.Bx[c][t // 2] for c in range(16)], [Bxb[b]], self.ds[b])
            ps = self.bank[b][:, 0:256]
            Bps = self.Bbank[b]
            for c in range(16):
                k = c % 4
                self.act(sq[k], xb[b][:, c, :], AF.Square, [Bxb[b]], [Bsq[k]])
                self.mm(ps, self.onesf[:], sq[k], c == 0, c == 15, [Bsq[k], self.Bconst], [Bps], inc=True)
            self.act(sd[b], ps, AF.Sqrt, [Bps], [Bsd[b]], scale=1.0 / DM, bias=1e-6)
            self.recip(sd[b], sd[b], [Bsd[b]], [Bsd[b]])
            for c in range(16):
                self.stt(self.hT[:, c, t * 256:(t + 1) * 256], xb[b][:, c, :], g[:, c:c + 1], sd[b],
                         ALU.mult, ALU.mult, [Bxb[b], Bsd[b], self.Blayer], [self.BhT[c][t]])
        self.aoff = mark
        S.barrier()

    def rot4(self, eng, dst, src, cos, sin, tmp, Btmp, R, Bsrc, Bdst, nh):
        sv = src.rearrange("p (h two d) -> p h two d", h=nh, two=2)
        dv = dst.rearrange("p (h two d) -> p h two d", h=nh, two=2)
        x1 = sv[:, :, 0, :]
        x2 = sv[:, :, 1, :]
        cb = cos.unsqueeze(1).broadcast_to([128, nh, 64])
        sbb = sin.unsqueeze(1).broadcast_to([128, nh, 64])
        t1v = tmp[0].rearrange("p (h d) -> p h d", h=nh)
        t2v = tmp[1].rearrange("p (h d) -> p h d", h=nh)
        self.tt(eng, t1v, x1, cb, ALU.mult, [Bsrc] + R, [Btmp[0]])
        self.tt(eng, t2v, x2, sbb, ALU.mult, [Bsrc] + R, [Btmp[1]])
        self.tt(eng, dv[:, :, 0, :], t1v, t2v, ALU.subtract, [Btmp[0], Btmp[1]], [Bdst])
        self.tt(eng, t1v, x1, sbb, ALU.mult, [Bsrc] + R, [Btmp[0]])
        self.tt(eng, t2v, x2, cb, ALU.mult, [Bsrc] + R, [Btmp[1]])
        self.tt(eng, dv[:, :, 1, :], t1v, t2v, ALU.add, [Btmp[0], Btmp[1]], [Bdst])

    def phase_proj(self, l):
        S = self.S
        hT = self.hT
        BhT = self.BhT
        bank, Bbank = self.bank, self.Bbank
        NW = 2
        Wt = [self.alloc([128, 16, 512], BF16) for _ in range(NW)]
        BW = self.bufs(NW)
        NR = 4
        stg = [self.alloc([128, 512], F32) for _ in range(NR)]
        Bstg = self.bufs(NR)
        stgb = [self.alloc([128, 512], BF16) for _ in range(NR)]
        Bstgb = self.bufs(NR)
        rt = {e: [self.alloc([128, 256], F32) for _ in range(2)] for e in ("dve", "pool")}
        Brt = {e: self.bufs(2) for e in ("dve", "pool")}
        r4 = [self.alloc([128, 4], F32) for _ in range(NR)]
        Br4 = self.bufs(NR)
        wv = self.w_in[l].rearrange("(c p) n -> p c n", p=128)
        self.pcnt = 0
        Wg = self.alloc([128, 16, 16], BF16)
        BWg = Buf()
        S.dma("pool", Wg, wv[:, :, C_MI:C_MI + 16], [], [BWg], self.ds[6])
        r8 = [self.alloc([128, 4], F32) for _ in range(8)]
        Br8 = self.bufs(8)
        mixer_base = self.aoff
        stg2 = stg3 = stg4 = None
        Bstg2 = self.bufs(8)
        Bstg3 = self.bufs(8)
        Bstg4 = self.bufs(8)
        A = self.alloc([8, SEQ], F32)
        Bm = self.alloc([8, SEQ], F32)
        Cc = self.alloc([8, SEQ], F32)
        Dd = self.alloc([8, SEQ], F32)
        BA, BB, BC, BD = self.bufs(4)
        blocks = []
        seg_of = []
        order = (("rq", C_RQ, 1024, 0), ("rk", C_RK, 1024, 0), ("rv", C_RV, 1024, 0), ("rg", C_RG, 1024, 0),
                 ("mq", C_MQ, 512, 1), ("mk", C_MK, 512, 1), ("mv", C_MV, 1024, 1), ("mo", C_MO, 1024, 1),
                 ("av", C_AV, 1024, 2))
        for nm, c0, w, seg in order:
            for j in range(w // 512):
                blocks.append(("tm", nm, c0 + j * 512, j))
                seg_of.append(seg)
        for gb in range(12):
            blocks.append(("fm", None, C_GA + gb * 512, gb))
            seg_of.append(2)
        for nm, c0, w, seg in (("aq", C_AQ, 1024, 3), ("ak", C_AK, 1024, 3)):
            for j in range(w // 512):
                blocks.append(("tm", nm, c0 + j * 512, j))
                seg_of.append(seg)
        nblk = len(blocks)

        def load(bi):
            kind, nm, c0, j = blocks[bi]
            s_ = bi % NW
            S.dma("pool", Wt[s_], wv[:, :, c0:c0 + 512], [], [BW[s_]], self.ds[0 + s_])

        load(0)
        load(1)
        for which, dstT, Bd in ((0, A, BA), (1, Bm, BB)):
            for tb in range(4):
                k = self.pcnt % 2
                self.pcnt += 1
                ps = bank[k]
                Bps = Bbank[k]
                for c in range(16):
                    self.mm(ps[0:8, :], Wg[:, c, which * 8:(which + 1) * 8], hT[:, c, tb * 512:(tb + 1) * 512],
                            c == 0, c == 15, [BhT[c][2 * tb], BhT[c][2 * tb + 1], BWg], [Bps], inc=(c == 15))
                self.cp("act", dstT[:, tb * 512:(tb + 1) * 512], ps[0:8, :], [Bps], [Bd])
        self.ts("dve", A, A, self.big[:, 0:1], 1.0 / 15.0, ALU.add, ALU.mult, [BA, self.Blayer], [BA])
        self.act(A, A, AF.Tanh, [BA], [BA])
        self.ts("dve", Bm, Bm, self.bfg[:, 0:1], 1.0 / 15.0, ALU.add, ALU.mult, [BB, self.Blayer], [BB])
        self.act(Bm, Bm, AF.Tanh, [BB], [BB])
        self.act(Bm, Bm, AF.Exp, [BB], [BB], scale=-15.0)
        self.act(Bm, Bm, AF.Ln, [BB], [BB], bias=1.0)
        for n in range(NT):
            sl = slice(n * 128, (n + 1) * 128)
            self.S.op("dve", lambda e, sl=sl: e.tensor_tensor_scan(out=Cc[:, sl], data0=self.onesf[0:8, :], data1=Bm[:, sl],
                                                                  initial=0.0, op0=ALU.mult, op1=ALU.subtract),
                      [BB, self.Bconst], [BC])
        self.act(Dd, Cc, AF.Exp, [BC], [BD])
        self.stt(A, A, 15.0, Cc, ALU.mult, ALU.subtract, [BA, BC], [BA])
        self.act(A, A, AF.Exp, [BA], [BA], bias=math.log(0.125))
        cl = Cc.rearrange("p (n t) -> p n t", t=128)[:, :, 127:128].broadcast_to([8, NT, 128])
        self.act(Bm.rearrange("p (n t) -> p n t", t=128), cl, AF.Exp, [BC], [BB])
        k = self.pcnt % 2
        self.pcnt += 1
        ps = bank[k]
        Bps = Bbank[k]
        idx = 0
        for wi, (src, Bs) in enumerate(((Dd, BD), (A, BA), (Bm, BB))):
            for n in range(NT):
                idx += 1
                col = (wi * NT + n) * 8
                self.tr(ps[:, col:col + 8], src[:, n * 128:(n + 1) * 128], self.identf[0:8, 0:8],
                        [Bs, self.Bconst], [Bps], inc=(idx == 48))
        self.cp("act", self.mtab[:].rearrange("p a n h -> p (a n h)"), ps[:, 0:384], [Bps], [self.Bmtab])
        S.barrier()
        self.aoff = mixer_base


        cosr, sinr = self.tabs[:, 0], self.tabs[:, 1]
        cosp, sinp = self.tabs[:, 2], self.tabs[:, 3]
        sq_r = self.small[:, 0:8]
        sk_r = self.small[:, 8:16]

        def evac(nm, j, i, ps, Bps, k):
            eng = "dve" if i % 2 == 0 else "pool"
            dst = self.z[nm]
            rows = slice(i * 128, (i + 1) * 128)
            cols = slice(j * 512, (j + 1) * 512)
            if nm in ("rv", "mv", "av"):
                self.cp("act", stgb[k], ps[:], [Bps], [Bstgb[k]])
                out_t, Bout = stgb[k], Bstgb[k]
            elif nm in ("rq", "rk"):
                sc = sq_r if nm == "rq" else sk_r
                for hh in range(4):
                    h = j * 4 + hh
                    self.S.op("act", lambda e, hh=hh, h=h, sc=sc: e.activation(out=stg[k][:, hh * 128:(hh + 1) * 128],
                                                                             in_=ps[:, hh * 128:(hh + 1) * 128], func=AF.Copy,
                                                                             scale=sc[:, h:h + 1]),
                              [Bps, self.Bconst], [Bstg[k]])
                self.rot4(eng, stgb[k], stg[k], cosr[:, i, :], sinr[:, i, :], rt[eng], Brt[eng], [self.Bconst], Bstg[k], Bstgb[k], 4)
                out_t, Bout = stgb[k], Bstgb[k]
            elif nm in ("rg", "mo"):
                fn = AF.Silu if nm == "rg" else AF.Sigmoid
                gw = self.retgn if nm == "rg" else self.mlnorm
                self.act(stg[k], ps[:], fn, [Bps], [Bstg[k]])
                self.tt(eng, stgb[k], stg[k], gw[:, cols], ALU.mult, [Bstg[k], self.Blayer], [Bstgb[k]])
                out_t, Bout = stgb[k], Bstgb[k]
            elif nm in ("mq", "mk"):
                tab = self.mtab[:, 0 if nm == "mq" else 1, i, :]
                tb_ = tab.unsqueeze(2).broadcast_to([128, 8, 64])
                v8 = lambda ap: ap.rearrange("p (h d) -> p h d", h=8)
                if i % 2 == 0:
                    self.tt("dve", v8(stgb[k]), v8(ps[:]), tb_, ALU.mult, [Bps, self.Bmtab], [Bstgb[k]])
                else:
                    self.cp("act", stg[k], ps[:], [Bps], [Bstg[k]])
                    self.tt("pool", v8(stgb[k]), v8(stg[k]), tb_, ALU.mult, [Bstg[k], self.Bmtab], [Bstgb[k]])
                out_t, Bout = stgb[k], Bstgb[k]
            else:
                gw = self.qnw if nm == "aq" else self.knw
                v4_ = lambda ap: ap.rearrange("p (h d) -> p h d", h=4)
                k8 = (j * NT + i) % 8
                self.act(stg2[k8], ps[:], AF.Square, [Bps], [Bstg2[k8]])
                self.cp("act", stg3[k8], ps[:], [Bps], [Bstg3[k8]])
                self.red(r8[k8], v4_(stg2[k8]), ALU.add, [Bstg2[k8]], [Br8[k8]])
                self.act(r8[k8], r8[k8], AF.Sqrt, [Br8[k8]], [Br8[k8]], scale=1.0 / 128.0, bias=1e-6)
                self.recip(r8[k8], r8[k8], [Br8[k8]], [Br8[k8]])
                self.tt(eng, v4_(stg3[k8]), v4_(stg3[k8]), r8[k8].unsqueeze(2).broadcast_to([128, 4, 128]), ALU.mult,
                        [Bstg3[k8], Br8[k8]], [Bstg3[k8]])
                self.tt(eng, v4_(stg3[k8]), v4_(stg3[k8]), gw[:].unsqueeze(1).broadcast_to([128, 4, 128]), ALU.mult,
                        [Bstg3[k8], self.Blayer], [Bstg3[k8]])
                self.rot4(eng, stg4[k8], stg3[k8], cosp[:, i, :], sinp[:, i, :], rt[eng], Brt[eng], [self.Bconst], Bstg3[k8], Bstg4[k8], 4)
                out_t, Bout = stg4[k8], Bstg4[k8]
                S.dma("sp", dst[rows, cols], out_t, [Bout], [self.Bz[nm][i][j]], self.ds[28 + k8 % 4])
                return
            S.dma("sp", dst[rows, cols], out_t, [Bout], [self.Bz[nm][i][j]], self.ds[28 + k])

        gen = None
        cur_seg = 0
        stepno = 0
        stride = {0: 1, 1: 1, 2: 2, 3: 1}

        def step(force=False):
            nonlocal gen, stepno
            stepno += 1
            if gen is not None and (force or stepno % stride[cur_seg] == 0):
                try:
                    next(gen)
                except StopIteration:
                    gen = None

        def drain():
            nonlocal gen
            while gen is not None:
                step(True)

        for bi, (kind, nm, c0, j) in enumerate(blocks):
            if seg_of[bi] != cur_seg:
                drain()
                cur_seg = seg_of[bi]
                S.barrier()
                self.aoff = mixer_base
                if cur_seg in (1, 2):
                    gen = self.ret_gen(l) if cur_seg == 1 else self.mlstm_gen(l)
                else:
                    stg2 = [self.alloc([128, 512], F32) for _ in range(8)]
                    stg3 = [self.alloc([128, 512], F32) for _ in range(8)]
                    stg4 = [self.alloc([128, 512], F32) for _ in range(8)]
            if bi + 1 < nblk and bi >= 1:
                load(bi + 1)
            s_ = bi % NW
            if kind == "tm":
                for i in range(NT):
                    k = self.pcnt % (4 if cur_seg in (0, 3) else 2)
                    self.pcnt += 1
                    ps = bank[k]
                    Bps = Bbank[k]
                    for c in range(16):
                        self.mm(ps[:], hT[:, c, i * 128:(i + 1) * 128], Wt[s_][:, c, :], c == 0, c == 15,
                                [BhT[c][i // 2], BW[s_]], [Bps], inc=(c == 15))
                    evac(nm, j, i, ps, Bps, k)
                    step()
            else:
                for nn in range(4):
                    for tb in range(4):
                        k = self.pcnt % 2
                        self.pcnt += 1
                        ps = bank[k]
                        Bps = Bbank[k]
                        for c in range(16):
                            self.mm(ps[:], Wt[s_][:, c, nn * 128:(nn + 1) * 128], hT[:, c, tb * 512:(tb + 1) * 512],
                                    c == 0, c == 15, [BhT[c][2 * tb], BhT[c][2 * tb + 1], BW[s_]], [Bps], inc=(c == 15))
                        self.act(stgb[k], ps[:], AF.Sigmoid, [Bps], [Bstgb[k]])
                        row = (j * 4 + nn) * 128
                        S.dma("sp", self.sgT[row:row + 128, tb * 512:(tb + 1) * 512], stgb[k], [Bstgb[k]],
                              [self.Bsg[j * 4 + nn][tb]], self.ds[32 + k])
                        step()
        drain()

    def bcast_h(self, ap8, d):
        return ap8.unsqueeze(2).broadcast_to([ap8.shape[0], 8, d])

    def ret_gen(self, l):
        S = self.S
        bank, Bbank = self.bank, self.Bbank
        NL = 3
        ld = []
        for b in range(NL):
            ld.append(dict(q=self.alloc([128, 1024], BF16), k=self.alloc([128, 1024], BF16),
                           g=self.alloc([128, 1024], BF16), v=self.alloc([128, 1024], BF16),
                           Bq=Buf(), Bk=Buf(), Bg=Buf(), Bv=Buf()))
        qT = [self.alloc([128, 1024], BF16) for _ in range(2)]
        kT = [self.alloc([128, 1024], BF16) for _ in range(2)]
        BqT, BkT = self.bufs(2), self.bufs(2)
        sTm = [self.alloc([128, 1024], BF16) for _ in range(2)]
        BsTm = [self.bufs(2) for _ in range(2)]
        R32 = self.alloc([128, 1024], F32)
        Rbf = self.alloc([128, 1024], BF16)
        Aa = self.alloc([128, 1024], F32)
        BR32, BRbf, BA = Buf(), Buf(), Buf()
        osb = self.alloc([128, 1024], F32)
        sqt = self.alloc([128, 1024], F32)
        cc = sqt
        Bosb, Bsqt, Bcc = self.bufs(3)
        yb = [self.alloc([128, 1024], BF16) for _ in range(2)]
        Byb = self.bufs(2)
        yTs = [self.alloc([128, 8, 128], BF16) for _ in range(2)]
        ByTs = self.bufs(2)
        st8 = [self.alloc([128, 8], F32) for _ in range(6)]
        Bst = self.bufs(6)
        gC = self.small[:, 16:24]
        zq, zk, zv, zg = self.z["rq"], self.z["rk"], self.z["rv"], self.z["rg"]
        Bz = self.Bz
        yTa = self.yT[0].rearrange("(h e) t -> e h t", e=128)
        v4 = lambda ap: ap.rearrange("p (h d) -> p h d", h=8)
        maskb4 = self.maskb[:].unsqueeze(1).broadcast_to([128, 4, 128])
        pq = bank[2][:].bitcast(BF16)
        pk = bank[3][:].bitcast(BF16)

        def loads(n):
            b = n % NL
            L_ = ld[b]
            rs = slice(n * 128, (n + 1) * 128)
            S.dma("sp", L_["q"], zq[rs, :], Bz["rq"][n], [L_["Bq"]], self.ds[4 + b])
            S.dma("sp", L_["k"], zk[rs, :], Bz["rk"][n], [L_["Bk"]], self.ds[7 + b])
            S.dma("sp", L_["g"], zg[rs, :], Bz["rg"][n], [L_["Bg"]], self.ds[10 + b])
            S.dma("sp", L_["v"], zv[rs, :], Bz["rv"][n], [L_["Bv"]], self.ds[13 + b])

        def front(n):
            L_ = ld[n % NL]
            s_ = n % 2
            for h in range(8):
                self.tr(pq[:, h * 128:(h + 1) * 128], L_["q"][:, h * 128:(h + 1) * 128], self.identb[:], [L_["Bq"], self.Bconst], [Bbank[2]], inc=(h == 7))
            for h in range(8):
                self.tr(pk[:, h * 128:(h + 1) * 128], L_["k"][:, h * 128:(h + 1) * 128], self.identb[:], [L_["Bk"], self.Bconst], [Bbank[3]], inc=(h == 7))
            self.cp("act", qT[s_], pq, [Bbank[2]], [BqT[s_]])
            self.cp("dve", kT[s_], pk, [Bbank[3]], [BkT[s_]])
            for h in range(8):
                hb = 4 + h // 4
                self.mm(bank[hb][:, (h % 4) * 128:(h % 4 + 1) * 128], kT[s_][:, h * 128:(h + 1) * 128], qT[s_][:, h * 128:(h + 1) * 128],
                        True, True, [BkT[s_], BqT[s_]], [Bbank[hb]], inc=(h % 4 == 3))
            for hb in range(2):
                self.tt("dve", sTm[s_][:, hb * 512:(hb + 1) * 512].rearrange("p (h d) -> p h d", h=4),
                        bank[4 + hb][:].rearrange("p (h d) -> p h d", h=4), maskb4, ALU.mult,
                        [Bbank[4 + hb], self.Bconst], [BsTm[s_][hb]])

        def mid(n):
            L_ = ld[n % NL]
            s_ = n % 2
            for h in range(8):
                hb = 6 + h // 4
                osl = bank[hb][:, (h % 4) * 128:(h % 4 + 1) * 128]
                hs = slice(h * 128, (h + 1) * 128)
                self.mm(osl, sTm[s_][:, hs], L_["v"][:, hs], True, n == 0, [BsTm[s_][h // 4], L_["Bv"]], [Bbank[hb]],
                        inc=(n == 0 and h % 4 == 3))
                if n > 0:
                    self.mm(osl, qT[s_][:, hs], Rbf[:, hs], False, True, [BqT[s_], BRbf], [Bbank[hb]], inc=(h % 4 == 3))
            if n + 1 < NT:
                for h in range(8):
                    hb = 4 + h // 4
                    hs = slice(h * 128, (h + 1) * 128)
                    self.mm(bank[hb][:, (h % 4) * 128:(h % 4 + 1) * 128], L_["k"][:, hs], L_["v"][:, hs], True, True,
                            [L_["Bk"], L_["Bv"]], [Bbank[hb]], inc=(h % 4 == 3))
                for hb in range(2):
                    hsl = slice(hb * 512, (hb + 1) * 512)
                    if n == 0:
                        self.cp("dve", Aa[:, hsl], bank[4 + hb][:], [Bbank[4 + hb]], [BA])
                    else:
                        self.tt("dve", Aa[:, hsl], bank[4 + hb][:], R32[:, hsl], ALU.add, [Bbank[4 + hb], BR32], [BA])
                self.tt("pool", v4(R32), v4(Aa), self.bcast_h(gC, 128), ALU.mult, [BA, self.Bconst], [BR32])
                self.cp("act", Rbf, R32, [BR32], [BRbf])

        def epi(n):
            L_ = ld[n % NL]
            for hb in range(2):
                self.cp("act", osb[:, hb * 512:(hb + 1) * 512], bank[6 + hb][:], [Bbank[6 + hb]], [Bosb])
            self.act(sqt, osb, AF.Square, [Bosb], [Bsqt])
            s1, s2, mean, msq, var, rstd = st8
            self.red(s1, v4(osb), ALU.add, [Bosb], [Bst[0]])
            self.red(s2, v4(sqt), ALU.add, [Bsqt], [Bst[1]])
            self.ts("dve", mean, s1, 1.0 / 128.0, None, ALU.mult, None, [Bst[0]], [Bst[2]])
            self.tt("dve", msq, mean, mean, ALU.mult, [Bst[2]], [Bst[3]])
            self.stt(var, s2, 1.0 / 128.0, msq, ALU.mult, ALU.subtract, [Bst[1], Bst[3]], [Bst[4]])
            self.act(rstd, var, AF.Sqrt, [Bst[4]], [Bst[5]], bias=1e-5)
            self.recip(rstd, rstd, [Bst[5]], [Bst[5]])
            self.tt("pool", v4(cc), v4(osb), self.bcast_h(mean, 128), ALU.subtract, [Bosb, Bst[2]], [Bcc])
            self.tt("pool", v4(cc), v4(cc), self.bcast_h(rstd, 128), ALU.mult, [Bcc, Bst[5]], [Bcc])
            self.tt("dve", yb[n % 2], cc, L_["g"], ALU.mult, [Bcc, L_["Bg"]], [Byb[n % 2]])

        def outT(n):
            for h in range(8):
                self.tr(pq[:, h * 128:(h + 1) * 128], yb[n % 2][:, h * 128:(h + 1) * 128], self.identb[:], [Byb[n % 2], self.Bconst], [Bbank[2]], inc=(h == 7))
            ys = yTs[n % 2]
            self.cp("act", ys.rearrange("p h t -> p (h t)"), pq, [Bbank[2]], [ByTs[n % 2]])
            S.dma("sp", yTa[:, :, n * 128:(n + 1) * 128], ys, [ByTs[n % 2]], [self.ByT[0][n]], self.ds[16 + n % 2])

        loads(0)
        loads(1)
        front(0)
        yield
        for n in range(NT):
            if n + 2 < NT:
                loads(n + 2)
            mid(n)
            yield
            if n + 1 < NT:
                front(n + 1)
                yield
            epi(n)
            yield
            if n > 0:
                outT(n - 1)
                yield
        outT(NT - 1)
        yield

    def mlstm_gen(self, l):
        S = self.S
        bank, Bbank = self.bank, self.Bbank
        NL = 3
        ld = []
        for b in range(NL):
            ld.append(dict(q=self.alloc([128, 512], BF16), k=self.alloc([128, 512], BF16),
                           o=self.alloc([128, 1024], BF16), v=self.alloc([128, 1024], BF16),
                           Bq=Buf(), Bk=Buf(), Bo=Buf(), Bv=Buf()))
        qT = [self.alloc([64, 1024], BF16) for _ in range(2)]
        kT = [self.alloc([64, 1024], BF16) for _ in range(2)]
        BqT, BkT = self.bufs(2), self.bufs(2)
        sTm = [self.alloc([128, 1024], BF16) for _ in range(2)]
        BsTm = [self.bufs(2) for _ in range(2)]
        C32 = self.alloc([64, 1024], F32)
        Cbf = self.alloc([64, 1024], BF16)
        Aa = self.alloc([64, 1024], F32)
        n32 = self.alloc([64, 8], F32)
        nbf = self.alloc([64, 8], BF16)
        An = self.alloc([64, 8], F32)
        BC32, BCbf, BA, Bn32, Bnbf, BAn = self.bufs(6)
        hv = self.alloc([128, 1024], F32)
        sqt = self.alloc([128, 1024], F32)
        Bhv, Bsqt = self.bufs(2)
        yb = [self.alloc([128, 1024], BF16) for _ in range(2)]
        Byb = self.bufs(2)
        yTs = [self.alloc([128, 8, 128], BF16) for _ in range(2)]
        ByTs = self.bufs(2)
        st8 = [self.alloc([128, 8], F32) for _ in range(3)]
        Bst = self.bufs(3)
        zq, zk, zv, zo = self.z["mq"], self.z["mk"], self.z["mv"], self.z["mo"]
        Bz = self.Bz
        yTb = self.yT[1].rearrange("(h e) t -> e h t", e=128)
        v4 = lambda ap: ap.rearrange("p (h d) -> p h d", h=8)
        maskb4 = self.maskb[:].unsqueeze(1).broadcast_to([128, 4, 128])
        mtab = self.mtab
        pq = bank[2][:].bitcast(BF16)
        pk = pq
        pD = bank[7]

        def loads(n):
            b = n % NL
            L_ = ld[b]
            rs = slice(n * 128, (n + 1) * 128)
            S.dma("sp", L_["q"], zq[rs, :], [Bz["mq"][n][0]], [L_["Bq"]], self.ds[36 + b])
            S.dma("sp", L_["k"], zk[rs, :], [Bz["mk"][n][0]], [L_["Bk"]], self.ds[18 + b])
            S.dma("sp", L_["o"], zo[rs, :], Bz["mo"][n], [L_["Bo"]], self.ds[21 + b])
            S.dma("sp", L_["v"], zv[rs, :], Bz["mv"][n], [L_["Bv"]], self.ds[24 + b])

        def front(n):
            L_ = ld[n % NL]
            s_ = n % 2
            for h in range(8):
                self.tr(pq[0:64, h * 128:(h + 1) * 128], L_["q"][:, h * 64:(h + 1) * 64], self.identb[:], [L_["Bq"], self.Bconst], [Bbank[2]], inc=(h == 7))
            self.cp("act", qT[s_], pq[0:64, :], [Bbank[2]], [BqT[s_]])
            for h in range(8):
                self.tr(pk[0:64, h * 128:(h + 1) * 128], L_["k"][:, h * 64:(h + 1) * 64], self.identb[:], [L_["Bk"], self.Bconst], [Bbank[2]], inc=(h == 7))
            self.cp("dve", kT[s_], pk[0:64, :], [Bbank[2]], [BkT[s_]])
            for h in range(8):
                hb = 3 + h // 4
                hs = slice(h * 128, (h + 1) * 128)
                self.mm(bank[hb][:, (h % 4) * 128:(h % 4 + 1) * 128], kT[s_][:, hs], qT[s_][:, hs], True, True, [BkT[s_], BqT[s_]], [Bbank[hb]], inc=(h % 4 == 3))
            for hb in range(2):
                self.tt("dve", sTm[s_][:, hb * 512:(hb + 1) * 512].rearrange("p (h d) -> p h d", h=4),
                        bank[3 + hb][:].rearrange("p (h d) -> p h d", h=4), maskb4, ALU.mult,
                        [Bbank[3 + hb], self.Bconst], [BsTm[s_][hb]])

        def mid(n):
            L_ = ld[n % NL]
            s_ = n % 2
            eal = mtab[0:64, 2, n, :]
            for h in range(8):
                hb = 5 + h // 4
                osl = bank[hb][:, (h % 4) * 128:(h % 4 + 1) * 128]
                hs = slice(h * 128, (h + 1) * 128)
                self.mm(osl, sTm[s_][:, hs], L_["v"][:, hs], True, n == 0, [BsTm[s_][h // 4], L_["Bv"]], [Bbank[hb]], inc=False)
                if n > 0:
                    self.mm(osl, qT[s_][:, hs], Cbf[:, hs], False, True, [BqT[s_], BCbf], [Bbank[hb]], inc=False)
                self.mm(pD[:, h:h + 1], sTm[s_][:, hs], self.onescb[:, 0:1], True, n == 0, [BsTm[s_][h // 4], self.Bconst], [Bbank[7]],
                        inc=(n == 0 and h == 7))
                if n > 0:
                    self.mm(pD[:, h:h + 1], qT[s_][:, hs], nbf[:, h:h + 1], False, True, [BqT[s_], Bnbf], [Bbank[7]], inc=(h == 7))
            dm, rden, rstd = st8
            self.act(dm, pD[:, 0:8], AF.Abs, [Bbank[7]], [Bst[0]])
            self.ts("dve", dm, dm, 1.0, None, ALU.max, None, [Bst[0]], [Bst[0]])
            self.recip(rden, dm, [Bst[0]], [Bst[1]])
            for hb in range(2):
                self.tt("dve", hv[:, hb * 512:(hb + 1) * 512].rearrange("p (h d) -> p h d", h=4),
                        bank[5 + hb][:].rearrange("p (h d) -> p h d", h=4),
                        rden[:, hb * 4:(hb + 1) * 4].unsqueeze(2).broadcast_to([128, 4, 128]), ALU.mult,
                        [Bbank[5 + hb], Bbank[7], Bst[1]], [Bhv])
            if n + 1 < NT:
                pKn = bank[7]
                for h in range(8):
                    hb = 3 + h // 4
                    hs = slice(h * 128, (h + 1) * 128)
                    self.mm(bank[hb][0:64, (h % 4) * 128:(h % 4 + 1) * 128], L_["k"][:, h * 64:(h + 1) * 64], L_["v"][:, hs], True, True,
                            [L_["Bk"], L_["Bv"]], [Bbank[hb]], inc=(h % 4 == 3))
                for h in range(8):
                    self.mm(pKn[0:64, 8 + h:9 + h], L_["k"][:, h * 64:(h + 1) * 64], self.onescb[:, 0:1], True, True,
                            [L_["Bk"], self.Bconst], [Bbank[7]], inc=(h == 7))
                ealb = eal.unsqueeze(2).broadcast_to([64, 8, 128])
                for hb in range(2):
                    hsl = slice(hb * 512, (hb + 1) * 512)
                    if n == 0:
                        self.cp("dve", Aa[:, hsl], bank[3 + hb][0:64, :], [Bbank[3 + hb]], [BA])
                    else:
                        self.tt("dve", Aa[:, hsl], bank[3 + hb][0:64, :], C32[:, hsl], ALU.add, [Bbank[3 + hb], BC32], [BA])
                self.tt("pool", v4(C32), v4(Aa), ealb, ALU.mult, [BA, self.Bmtab], [BC32])
                self.cp("act", Cbf, C32, [BC32], [BCbf])
                if n == 0:
                    self.cp("dve", An, pKn[0:64, 8:16], [Bbank[7]], [BAn])
                else:
                    self.tt("dve", An, pKn[0:64, 8:16], n32, ALU.add, [Bbank[7], Bn32], [BAn])
                self.tt("dve", n32, An, eal, ALU.mult, [BAn, self.Bmtab], [Bn32])
                self.cp("dve", nbf, n32, [Bn32], [Bnbf])

        def epi(n):
            L_ = ld[n % NL]
            dm, rden, rstd = st8
            self.act(sqt, hv, AF.Square, [Bhv], [Bsqt])
            self.red(rstd, v4(sqt), ALU.add, [Bsqt], [Bst[2]])
            self.act(rstd, rstd, AF.Sqrt, [Bst[2]], [Bst[2]], scale=1.0 / 128.0, bias=1e-6)
            self.recip(rstd, rstd, [Bst[2]], [Bst[2]])
            self.tt("pool", v4(hv), v4(hv), self.bcast_h(rstd, 128), ALU.mult, [Bhv, Bst[2]], [Bhv])
            self.tt("dve", yb[n % 2], hv, L_["o"], ALU.mult, [Bhv, L_["Bo"]], [Byb[n % 2]])

        def outT(n):
            for h in range(8):
                self.tr(pq[:, h * 128:(h + 1) * 128], yb[n % 2][:, h * 128:(h + 1) * 128], self.identb[:], [Byb[n % 2], self.Bconst], [Bbank[2]], inc=(h == 7))
            ys = yTs[n % 2]
            self.cp("act", ys.rearrange("p h t -> p (h t)"), pq, [Bbank[2]], [ByTs[n % 2]])
            S.dma("sp", yTb[:, :, n * 128:(n + 1) * 128], ys, [ByTs[n % 2]], [self.ByT[1][n]], self.ds[2 + n % 2])

        loads(0)
        loads(1)
        front(0)
        yield
        for n in range(NT):
            if n + 2 < NT:
                loads(n + 2)
            mid(n)
            yield
            if n + 1 < NT:
                front(n + 1)
                yield
            epi(n)
            yield
            if n > 0:
                outT(n - 1)
                yield
        outT(NT - 1)
        yield

    def phase_moba(self, l):
        S = self.S
        self.arena_reset()
        bank, Bbank = self.bank, self.Bbank
        qT_all = self.alloc([128, 8, SEQ], BF16)
        kT_all = self.alloc([128, 8, SEQ], BF16)
        v_all = self.alloc([128, NT, 1024], BF16)
        sel_all = self.alloc([128, NT, 8, 8], F32)
        BqTa = [Buf() for _ in range(NT)]
        BkTa = [Buf() for _ in range(NT)]
        Bva = [Buf() for _ in range(NT)]
        Bsel = [Buf() for _ in range(NT)]
        ksum = self.alloc([128, 8, NT], F32)
        kmean = self.alloc([128, 8, 8], F32)
        Bksum, Bkmean = Buf(), Buf()
        nbT = self.alloc([128, SEQ], BF16)
        BnbT = [Buf() for _ in range(NT)]
        Esel = self.alloc([128, 64, 128], BF16)
        BEsel = Buf()
        mark = self.aoff
        NL = 3
        ld = []
        for b in range(NL):
            ld.append(dict(q=self.alloc([128, 1024], F32), k=self.alloc([128, 1024], F32), Bq=Buf(), Bk=Buf()))
        nbt = [self.alloc([128, 64], F32) for _ in range(2)]
        Bnbt = self.bufs(2)
        self.cp("pool", Esel, self.identf[:, 0:64].unsqueeze(2).broadcast_to([128, 64, 128]), [self.Bconst], [BEsel])
        self.memset("pool", nbT, 0.0, [], BnbT)
        qT32 = [self.alloc([128, 1024], F32) for _ in range(2)]
        BqT32 = self.bufs(2)
        gm = self.alloc([128, 8, 8], F32)
        mx = self.alloc([128, 8, 8], F32)
        Bgm, Bmx = Buf(), Buf()
        zq, zk, zv = self.z["aq"], self.z["ak"], self.z["av"]
        Bz = self.Bz
        v4 = lambda ap: ap.rearrange("p (h d) -> p h d", h=8)
        self.memset("dve", kmean, 0.0, [], [Bkmean])
        self.memset("dve", sel_all, 0.0, [], Bsel)

        def loads(i):
            b = i % NL
            L_ = ld[b]
            rs = slice(i * 128, (i + 1) * 128)
            S.dma("sp", L_["q"], zq[rs, :], Bz["aq"][i], [L_["Bq"]], self.ds[0 + b])
            S.dma("sp", L_["k"], zk[rs, :], Bz["ak"][i], [L_["Bk"]], self.ds[3 + b])
            S.dma("sp", v_all[:, i, :], zv[rs, :], Bz["av"][i], [Bva[i]], self.ds[6 + b])

        loads(0)
        loads(1)
        for i in range(NT):
            if i + 2 < NT:
                loads(i + 2)
            L_ = ld[i % NL]
            qr, kr, Bqr, Bkr = L_["q"], L_["k"], L_["Bq"], L_["Bk"]
            q32 = qT32[i % 2]
            Bq32 = BqT32[i % 2]
            bq = i // 2
            for h in range(8):
                hb = h // 4
                self.tr(bank[hb][:, (h % 4) * 128:(h % 4 + 1) * 128], qr[:, h * 128:(h + 1) * 128], self.identf[:],
                        [Bqr, self.Bconst], [Bbank[hb]], inc=(h % 4 == 3))
            for h in range(8):
                hb = 2 + h // 4
                self.tr(bank[hb][:, (h % 4) * 128:(h % 4 + 1) * 128], kr[:, h * 128:(h + 1) * 128], self.identf[:],
                        [Bkr, self.Bconst], [Bbank[hb]], inc=(h % 4 == 3))
            for hb in range(2):
                self.cp("act", q32[:, hb * 512:(hb + 1) * 512], bank[hb][:], [Bbank[hb]], [Bq32])
                self.cp("dve", kT_all[:, hb * 4:(hb + 1) * 4, i * 128:(i + 1) * 128],
                        bank[2 + hb][:].rearrange("p (h t) -> p h t", h=4), [Bbank[2 + hb]], [BkTa[i]])
            self.cp("pool", qT_all[:, :, i * 128:(i + 1) * 128], v4(q32), [Bq32], [BqTa[i]])
            pks = bank[4 + (i % 2) * 2]
            Bpks = Bbank[4 + (i % 2) * 2]
            for h in range(8):
                self.mm(pks[:, h:h + 1], kr[:, h * 128:(h + 1) * 128], self.kmcol[:, 0:1], True, True, [Bkr, self.Bconst], [Bpks], inc=(h == 7))
            self.cp("act", ksum[:, :, i], pks[:, 0:8], [Bpks], [Bksum])
            if bq >= 1:
                pg = bank[5 + (i % 2) * 2]
                Bpg = Bbank[5 + (i % 2) * 2]
                for h in range(8):
                    self.mm(pg[:, h * 8:(h + 1) * 8], q32[:, h * 128:(h + 1) * 128], kmean[:, h, :], True, True,
                            [Bq32, Bkmean], [Bpg], inc=(h == 7))
                self.tt("dve", gm, pg[:, 0:64].rearrange("p (h n) -> p h n", h=8),
                        self.negm[:, bq, :].unsqueeze(1).broadcast_to([128, 8, 8]), ALU.add, [Bpg, self.Bconst], [Bgm])
                for h in range(8):
                    self.S.op("dve", lambda e, h=h: e.max(out=mx[:, h, :], in_=gm[:, h, :]), [Bgm], [Bmx])
                self.tt("dve", gm, gm, mx[:, :, 2:3].broadcast_to([128, 8, 8]), ALU.is_ge, [Bgm, Bmx], [Bgm])
                self.tt("dve", gm, gm, self.selfix[:, 0, bq, :].unsqueeze(1).broadcast_to([128, 8, 8]), ALU.mult, [Bgm, self.Bconst], [Bgm])
                self.tt("dve", sel_all[:, i], gm, self.selfix[:, 1, bq, :].unsqueeze(1).broadcast_to([128, 8, 8]), ALU.add,
                        [Bgm, self.Bconst], [Bsel[i]])
            else:
                self.cp("dve", sel_all[:, i], self.selfix[:, 1, 0:1, :].broadcast_to([128, 8, 8]), [self.Bconst], [Bsel[i]])
            if i % 2 == 1:
                self.tt("dve", kmean[:, :, bq], ksum[:, :, i - 1], ksum[:, :, i], ALU.add, [Bksum], [Bkmean])
            self.ts("dve", nbt[i % 2], sel_all[:, i].rearrange("p h n -> p (h n)"), -1.0, 30000.0, ALU.add, ALU.mult, [Bsel[i]], [Bnbt[i % 2]])
            self.tr(pks[0:64, 128:256], nbt[i % 2], self.identf[:], [Bnbt[i % 2], self.Bconst], [Bpks], inc=True)
            self.cp("act", nbT[0:64, i * 128:(i + 1) * 128], pks[0:64, 128:256], [Bpks], [BnbT[i]])
        S.barrier()
        self.aoff = mark
        NPT = 4
        LA = 2
        PT = [self.alloc([128, 512], BF16) for _ in range(NPT)]
        BPT = self.bufs(NPT)
        rd = [self.alloc([128, 512], F32) for _ in range(2)]
        Brd = self.bufs(2)
        yst = [self.alloc([128, 512], BF16) for _ in range(2)]
        Byst = self.bufs(2)
        items = []
        for h in range(8):
            for Q in range(4):
                for jt in range(4 * Q + 4):
                    items.append((h, Q, jt))

        def geom(Q, jt):
            c0 = 128 * max(0, jt - 4 * Q)
            return c0, 512 - c0, 4 * Q * 128 + c0, (4 * Q + 4) * 128

        def qk(idx):
            h, Q, jt = items[idx]
            n = jt // 2
            c0, N, qlo, qhi = geom(Q, jt)
            need_bias = n <= 2 * Q
            pS = bank[idx % 4]
            BpS = Bbank[idx % 4]
            pt = PT[idx % NPT]
            Bpt = BPT[idx % NPT]
            qdeps = [BqTa[x] for x in range(qlo // 128, qhi // 128)]
            self.mm(pS[:, 0:N], kT_all[:, h, jt * 128:(jt + 1) * 128], qT_all[:, h, qlo:qhi], True, not need_bias,
                    [BkTa[jt]] + qdeps, [BpS], inc=not need_bias)
            if need_bias:
                self.mm(pS[:, 0:N], Esel[:, h * 8 + n, :], nbT[:, qlo:qhi], False, True,
                        [BEsel] + [BnbT[x] for x in range(qlo // 128, qhi // 128)], [BpS], inc=True)
            self.act(pt[:, 0:N], pS[:, 0:N], AF.Exp, [BpS], [Bpt])
            if jt >= 4 * Q:
                self.tt("pool", pt[:, 0:128], pt[:, 0:128], self.maskb[:], ALU.mult, [Bpt, self.Bconst], [Bpt])

        def pv(idx):
            h, Q, jt = items[idx]
            gi = h * 4 + Q
            g2 = gi % 2
            hs = slice(h * 128, (h + 1) * 128)
            c0, N, qlo, qhi = geom(Q, jt)
            ntile = 4 * Q + 4
            pO, BpO = bank[4 + g2], Bbank[4 + g2]
            pDn, BpDn = bank[6 + g2], Bbank[6 + g2]
            pt = PT[idx % NPT]
            Bpt = BPT[idx % NPT]
            self.mm(pO[:, c0:512], v_all[:, jt, hs], pt[:, 0:N], jt == 0, jt == ntile - 1, [Bva[jt], Bpt], [BpO], inc=False)
            self.mm(pDn[:, c0:512], self.onesb[:], pt[:, 0:N], jt == 0, jt == ntile - 1, [self.Bconst, Bpt], [BpDn], inc=True)
            if jt == ntile - 1:
                self.recip(rd[g2], pDn[:], [BpDn], [Brd[g2]])
                self.tt("dve", yst[g2], pO[:], rd[g2], ALU.mult, [BpO, BpDn, Brd[g2]], [Byst[g2]])
                S.dma("sp", self.yT[2][h * 128:(h + 1) * 128, Q * 512:(Q + 1) * 512], yst[g2], [Byst[g2]],
                      [self.ByT[2][h * 4 + Q]], self.ds[12 + g2])

        for idx in range(len(items) + LA):
            if idx < len(items):
                qk(idx)
            if idx >= LA:
                pv(idx - LA)

    def phase_merge(self, l, xsrc):
        S = self.S
        self.arena_reset()
        bank, Bbank = self.bank, self.Bbank
        mT = self.alloc([128, 16, SEQ], BF16)
        BmT = [[Buf() for _ in range(4)] for _ in range(16)]
        yTb = [self.alloc([128, 8, SEQ], BF16) for _ in range(2)]
        ByTb = [self.bufs(4) for _ in range(2)]
        Wp = [self.alloc([128, 8, 128], BF16) for _ in range(4)]
        BWp = self.bufs(4)
        sgt = [self.alloc([128, 512], BF16) for _ in range(4)]
        Bsgt = self.bufs(4)
        prod = [self.alloc([128, 512], F32) for _ in range(2)]
        Bprod = self.bufs(2)
        Wo = [self.alloc([128, 16, 128], BF16) for _ in range(3)]
        BWo = self.bufs(3)
        xt = [self.alloc([128, 512], F32) for _ in range(4)]
        Bxt = self.bufs(4)
        ost = [self.alloc([128, 512], F32) for _ in range(4)]
        Bost = self.bufs(4)
        wps = [self.w_pa, self.w_pb, self.w_pc]

        def loady(b):
            yv = self.yT[b].rearrange("(c p) t -> p c t", p=128)
            for tb in range(4):
                if b < 2:
                    rd = [self.ByT[b][4 * tb + x] for x in range(4)]
                else:
                    rd = [self.ByT[2][h * 4 + tb] for h in range(8)]
                S.dma("sp", yTb[b % 2][:, :, tb * 512:(tb + 1) * 512], yv[:, :, tb * 512:(tb + 1) * 512], rd, [ByTb[b % 2][tb]], self.ds[0 + (b % 2) * 4 + tb])

        items = [(b, ncx, tb) for b in range(3) for ncx in range(16) for tb in range(4)]

        def loadw(b, ncx):
            wv = wps[b][l].rearrange("(c p) n -> p c n", p=128)
            s_ = (b * 16 + ncx) % 4
            S.dma("pool", Wp[s_], wv[:, :, ncx * 128:(ncx + 1) * 128], [], [BWp[s_]], self.ds[8 + s_])

        def loadsg(ii):
            b, ncx, tb = items[ii]
            k = ii % 4
            S.dma("sp", sgt[k], self.sgT[(b * 16 + ncx) * 128:(b * 16 + ncx + 1) * 128, tb * 512:(tb + 1) * 512],
                  [self.Bsg[b * 16 + ncx][tb]], [Bsgt[k]], self.ds[12 + k])

        wlist = [(b, ncx) for b in range(3) for ncx in range(16)]
        loady(0)
        loady(1)
        loadw(*wlist[0])
        loadw(*wlist[1])
        loadsg(0)
        loadsg(1)
        for ii, (b, ncx, tb) in enumerate(items):
            wi = b * 16 + ncx
            if tb == 0 and wi + 2 < len(wlist):
                loadw(*wlist[wi + 2])
            if ii + 2 < len(items):
                loadsg(ii + 2)
            if b == 1 and ncx == 0 and tb == 0:
                loady(2)
            s_ = wi % 4
            k = ii % 4
            ps = bank[k]
            Bps = Bbank[k]
            for c in range(8):
                self.mm(ps[:], Wp[s_][:, c, :], yTb[b % 2][:, c, tb * 512:(tb + 1) * 512], c == 0, c == 7,
                        [BWp[s_], ByTb[b % 2][tb]], [Bps], inc=(c == 7))
            msl = mT[:, ncx, tb * 512:(tb + 1) * 512]
            if b == 0:
                self.tt("dve", msl, ps[:], sgt[k], ALU.mult, [Bps, Bsgt[k]], [BmT[ncx][tb]])
            else:
                pr = prod[k % 2]
                self.tt("dve", pr, ps[:], sgt[k], ALU.mult, [Bps, Bsgt[k]], [Bprod[k % 2]])
                self.tt("pool", msl, pr, msl, ALU.add, [Bprod[k % 2], BmT[ncx][tb]], [BmT[ncx][tb]])
        wv = self.w_out[l].rearrange("(c p) n -> p c n", p=128)

        def loadwo(dcx):
            s_ = dcx % 3
            S.dma("pool", Wo[s_], wv[:, :, dcx * 128:(dcx + 1) * 128], [], [BWo[s_]], self.ds[16 + s_])

        oitems = [(dcx, tb) for dcx in range(16) for tb in range(4)]

        def loadx(ii):
            dcx, tb = oitems[ii]
            k = ii % 4
            S.dma("sp", xt[k], xsrc[dcx * 128:(dcx + 1) * 128, tb * 512:(tb + 1) * 512], [self.Bx[dcx][tb]], [Bxt[k]], self.ds[19 + k])

        loadwo(0)
        loadwo(1)
        loadx(0)
        loadx(1)
        for ii, (dcx, tb) in enumerate(oitems):
            if tb == 0 and dcx + 2 < 16:
                loadwo(dcx + 2)
            if ii + 2 < len(oitems):
                loadx(ii + 2)
            s_ = dcx % 3
            k = ii % 4
            ps = bank[4 + k]
            Bps = Bbank[4 + k]
            for c in range(16):
                self.mm(ps[:], Wo[s_][:, c, :], mT[:, c, tb * 512:(tb + 1) * 512], c == 0, c == 15,
                        [BWo[s_], BmT[c][tb]], [Bps], inc=(c == 15))
            self.tt("dve", ost[k], ps[:], xt[k], ALU.add, [Bps, Bxt[k]], [Bost[k]])
            S.dma("sp", self.xres[dcx * 128:(dcx + 1) * 128, tb * 512:(tb + 1) * 512], ost[k], [Bost[k]], [self.Bx[dcx][tb]], self.ds[23 + k])

    def phase_ffn_up(self, l):
        S = self.S
        hT, BhT = self.hT, self.BhT
        bank, Bbank = self.bank, self.Bbank
        NW = 3
        Wa = [self.alloc([128, 16, 128], BF16) for _ in range(NW)]
        Wb = [self.alloc([128, 16, 128], BF16) for _ in range(NW)]
        BWa, BWb = self.bufs(NW), self.bufs(NW)
        ua = [self.alloc([128, 2 + SEQ], F32) for _ in range(2)]
        ub = [self.alloc([128, 2 + SEQ], F32) for _ in range(2)]
        Bua = [[Buf() for _ in range(5)] for _ in range(2)]
        Bub = [[Buf() for _ in range(5)] for _ in range(2)]
        ta = [self.alloc([128, 512], F32) for _ in range(2)]
        tb_ = [self.alloc([128, 512], F32) for _ in range(2)]
        Bta, Btb = self.bufs(2), self.bufs(2)
        sl_ = [self.alloc([128, 512], F32) for _ in range(2)]
        Bsl = self.bufs(2)
        ast = [self.alloc([128, SEQ], BF16) for _ in range(2)]
        Bast = self.bufs(2)
        wv = self.w_up[l].rearrange("(c p) n -> p c n", p=128)
        for s_ in range(2):
            self.memset("dve", ua[s_][:, 0:2], 0.0, [], [Bua[s_][4]])
            self.memset("dve", ub[s_][:, 0:2], 0.0, [], [Bub[s_][4]])

        def loadw(fc):
            s = fc % NW
            S.dma("pool", Wa[s], wv[:, :, fc * 128:(fc + 1) * 128], [], [BWa[s]], self.ds[0 + s])
            S.dma("pool", Wb[s], wv[:, :, D_FF + fc * 128:D_FF + (fc + 1) * 128], [], [BWb[s]], self.ds[3 + s])
        loadw(0)
        loadw(1)
        cnt = 0
        cw, cb = self.convw, self.convb
        for fc in range(NFC):
            if fc + 2 < NFC:
                loadw(fc + 2)
            s = fc % NW
            u = fc % 2
            for tb in range(4):
                for (W_, BW_, uu, Buu, tt_, Btt, ch, pb) in ((Wa[s], BWa[s], ua[u], Bua[u], ta, Bta, fc, 0),
                                                            (Wb[s], BWb[s], ub[u], Bub[u], tb_, Btb, NFC + fc, 1)):
                    k = cnt % 4
                    cnt += 1
                    ps = bank[k]
                    Bps = Bbank[k]
                    for c in range(16):
                        self.mm(ps[:], W_[:, c, :], hT[:, c, tb * 512:(tb + 1) * 512], c == 0, c == 15,
                                [BW_, BhT[c][2 * tb], BhT[c][2 * tb + 1]], [Bps], inc=(c == 15))
                    self.cp("act", uu[:, 2 + tb * 512:2 + (tb + 1) * 512], ps[:], [Bps], [Buu[tb]])
                    prev = [Buu[tb - 1]] if tb > 0 else [Buu[4]]
                    t_ = tt_[tb % 2]
                    Bt_ = Btt[tb % 2]
                    self.ts("dve", t_, uu[:, 2 + tb * 512:2 + (tb + 1) * 512], cw[:, ch, 2:3], cb[:, ch:ch + 1], ALU.mult, ALU.add,
                            [Buu[tb], self.Blayer], [Bt_])
                    self.stt(t_, uu[:, 1 + tb * 512:1 + (tb + 1) * 512], cw[:, ch, 1:2], t_, ALU.mult, ALU.add,
                             [Buu[tb], self.Blayer, Bt_] + prev, [Bt_])
                    self.stt(t_, uu[:, tb * 512:(tb + 1) * 512], cw[:, ch, 0:1], t_, ALU.mult, ALU.add,
                             [Buu[tb], self.Blayer, Bt_] + prev, [Bt_])
                sx = sl_[tb % 2]
                self.act(sx, ta[tb % 2], AF.Silu, [Bta[tb % 2]], [Bsl[tb % 2]])
                self.tt("pool", ast[u][:, tb * 512:(tb + 1) * 512], sx, tb_[tb % 2], ALU.mult, [Bsl[tb % 2], Btb[tb % 2]], [Bast[u]])
            S.dma("sp", self.actT[fc * 128:(fc + 1) * 128, :], ast[u], [Bast[u]], self.Bact[fc], self.ds[6 + u])

    def phase_ffn_down(self, l, xdst):
        S = self.S
        self.arena_reset()
        bank, Bbank = self.bank, self.Bbank
        NSB = 11
        Wd = [self.alloc([128, NFC, 512], BF16) for _ in range(2)]
        BWd = [self.bufs(4) for _ in range(2)]
        At = self.alloc([128, NFC, 512], BF16)
        BAt = self.bufs(NSB)
        xt = [self.alloc([128, 512], F32) for _ in range(4)]
        Bxt = self.bufs(4)
        ost = [self.alloc([128, 512], F32) for _ in range(4)]
        Bost = self.bufs(4)
        wv = self.w_down[l].rearrange("(c p) n -> p c n", p=128)
        av = self.actT.rearrange("(c p) t -> p c t", p=128)

        def loadw(dg):
            for sblk in range(4):
                cs = slice(sblk * 11, (sblk + 1) * 11)
                S.dma("pool", Wd[dg % 2][:, cs, :], wv[:, cs, dg * 512:(dg + 1) * 512], [], [BWd[dg % 2][sblk]], self.ds[0 + (dg % 2) * 4 + sblk])

        its = [(dg, tb) for dg in range(4) for tb in range(4)]

        def loada(ii):
            dg, tb = its[ii]
            for sblk in range(NSB):
                cs = slice(sblk * 4, (sblk + 1) * 4)
                S.dma("sp", At[:, cs, :], av[:, cs, tb * 512:(tb + 1) * 512],
                      [self.Bact[c][tb] for c in range(sblk * 4, (sblk + 1) * 4)], [BAt[sblk]], self.ds[8 + sblk])

        loadw(0)
        loada(0)
        cnt = 0
        for ii, (dg, tb) in enumerate(its):
            if tb == 0 and dg + 1 < 4:
                loadw(dg + 1)
            for dcl in range(4):
                dcx = dg * 4 + dcl
                k = cnt % 4
                cnt += 1
                ps = bank[k]
                Bps = Bbank[k]
                S.dma("sp", xt[k], self.xres[dcx * 128:(dcx + 1) * 128, tb * 512:(tb + 1) * 512], [self.Bx[dcx][tb]], [Bxt[k]], self.ds[19 + k])
                for c in range(NFC):
                    self.mm(ps[:], Wd[dg % 2][:, c, dcl * 128:(dcl + 1) * 128], At[:, c, :], c == 0, c == NFC - 1,
                            [BWd[dg % 2][c // 11], BAt[c // 4]], [Bps], inc=(c == NFC - 1 or (dcl == 3 and c % 4 == 3)))
                if dcl == 3 and ii + 1 < len(its):
                    loada(ii + 1)
                self.tt("dve", ost[k], ps[:], xt[k], ALU.add, [Bps, Bxt[k]], [Bost[k]])
                S.dma("sp", xdst[dcx * 128:(dcx + 1) * 128, tb * 512:(tb + 1) * 512], ost[k], [Bost[k]], [self.Bx[dcx][tb]], self.ds[23 + k])


def _const_inputs():
    f32 = np.float32
    pos = np.arange(SEQ, dtype=f32)
    ret_freq = (1.0 / (10000.0 ** np.linspace(0.0, 1.0, 64, dtype=f32))).astype(f32)
    rope_freq = (1.0 / (10000.0 ** (np.arange(0, 128, 2, dtype=f32) / f32(128)))).astype(f32)
    tabs = np.zeros((128, 4, 16, 64), f32)
    for ti, fr in ((0, ret_freq), (2, rope_freq)):
        ang = (pos[:, None] * fr[None, :]).astype(f32)
        c = np.cos(ang).astype(f32).reshape(16, 128, 64).transpose(1, 0, 2)
        s = np.sin(ang).astype(f32).reshape(16, 128, 64).transpose(1, 0, 2)
        tabs[:, ti] = c
        tabs[:, ti + 1] = s
    h = np.arange(8, dtype=np.float64)
    log_gamma = np.log1p(-np.exp2(-5.0 - h))
    p = np.arange(128, dtype=np.float64)
    small = np.zeros((128, 24), f32)
    small[:, 0:8] = np.exp((p[:, None] + 1.0) * log_gamma[None, :])
    small[:, 8:16] = (128.0 ** -0.5) * np.exp(-(p[:, None] + 1.0) * log_gamma[None, :])
    small[:, 16:24] = np.exp(128.0 * log_gamma)[None, :]
    j = np.arange(128)
    mask = (j[None, :] >= j[:, None]).astype(f32)
    ident = np.eye(128, dtype=f32)
    negm = np.zeros((128, 8, 8), f32)
    selfix = np.zeros((128, 2, 8, 8), f32)
    for bq in range(8):
        for n in range(8):
            negm[:, bq, n] = 0.0 if n < bq else -1e30
            selfix[:, 0, bq, n] = 1.0 if n < bq else 0.0
            selfix[:, 1, bq, n] = 1.0 if n == bq else 0.0
    return dict(c_tabs=tabs, c_small=small, c_mask=mask, c_ident=ident, c_negm=negm, c_selfix=selfix)


def _layout_inputs(inputs):
    f32 = np.float32
    A = lambda k: np.ascontiguousarray(np.asarray(inputs[k], dtype=f32))
    shared = {}
    for k in ("w_in", "w_pa", "w_pb", "w_pc", "w_out", "w_up", "w_down"):
        shared[k] = A(k)
    shared["norm_mix_t"] = np.ascontiguousarray(A("norm_mix").reshape(DEPTH, 16, 128).transpose(0, 2, 1))
    shared["norm_ffn_t"] = np.ascontiguousarray(A("norm_ffn").reshape(DEPTH, 16, 128).transpose(0, 2, 1))
    shared["b_ig_t"] = A("b_ig").reshape(DEPTH, 8, 1)
    shared["b_fg_t"] = A("b_fg").reshape(DEPTH, 8, 1)
    shared["ret_gn_rep"] = np.ascontiguousarray(np.broadcast_to(A("ret_gn")[:, None, :], (DEPTH, 128, 1024)))
    shared["ml_norm_rep"] = np.ascontiguousarray(np.broadcast_to(A("ml_norm")[:, None, :], (DEPTH, 128, 1024)))
    shared["q_norm_rep"] = np.ascontiguousarray(np.broadcast_to(A("q_norm")[:, None, :], (DEPTH, 128, 128)))
    shared["k_norm_rep"] = np.ascontiguousarray(np.broadcast_to(A("k_norm")[:, None, :], (DEPTH, 128, 128)))
    cw = A("conv_w")
    shared["conv_w_t"] = np.ascontiguousarray(cw.reshape(DEPTH, 3, 88, 128).transpose(0, 3, 2, 1))
    shared["conv_b_t"] = np.ascontiguousarray(A("conv_b").reshape(DEPTH, 88, 128).transpose(0, 2, 1))
    shared.update(_const_inputs())
    return shared


_NC_CACHE = {}


def _get_nc(n_layers=DEPTH, dbg=False, stop_after=None):
    key = (n_layers, dbg, stop_after)
    if key not in _NC_CACHE:
        kb = KB(n_layers, dbg)
        kb.stop_after = stop_after
        _NC_CACHE[key] = (kb.build(), kb)
    return _NC_CACHE[key][0]


def kernel(**inputs):
    x = np.asarray(inputs["x"], dtype=np.float32)
    B = x.shape[0]
    shared = _layout_inputs(inputs)
    nc = _get_nc()
    in_maps = []
    for b in range(B):
        m = dict(shared)
        m["xT"] = np.ascontiguousarray(x[b].T)
        in_maps.append(m)
    res = run_bass_kernel_spmd(nc, in_maps, core_ids=list(range(B)))
    out = np.stack([np.ascontiguousarray(np.asarray(r["outT"]).T) for r in res.results], axis=0)
    return out.astype(np.float32)
```

```python
import math
from contextlib import ExitStack

import numpy as np
import concourse.bass as bass
import concourse.mybir as mybir
from concourse.bass_utils import run_bass_kernel_spmd

F32 = mybir.dt.float32
BF16 = mybir.dt.bfloat16
AF = mybir.ActivationFunctionType
ALU = mybir.AluOpType
AX = mybir.AxisListType

ENGS = ["pe", "act", "dve", "pool", "sp"]

DEPTH = 4
SEQ = 2048
DM = 2048
NT = 16
N_IN = 16400
D_FF = 5632
NFC = 44
C_RQ, C_RK, C_RV, C_RG = 0, 1024, 2048, 3072
C_MQ, C_MK, C_MV, C_MO, C_MI, C_MF = 4096, 4608, 5120, 6144, 7168, 7176
C_AQ, C_AK, C_AV = 7184, 8208, 9232
C_GA = 10256


class Buf:
    __slots__ = ("name", "last_w", "readers")

    def __init__(self, name=""):
        self.name = name
        self.last_w = None
        self.readers = {}


class DSem:
    __slots__ = ("name", "count")

    def __init__(self, name):
        self.name = name
        self.count = 0


class Sched:
    def __init__(self, nc):
        self.nc = nc
        self.streams = {e: [] for e in ENGS}
        self.cnt = {e: 0 for e in ENGS}
        self.waited = {e: {} for e in ENGS}
        self.semnames = list(ENGS)
        self.dsems = []

    def dsem(self):
        d = DSem("d%d" % len(self.dsems))
        self.dsems.append(d)
        self.semnames.append(d.name)
        return d

    def _collect(self, eng, reads, writes):
        deps = {}
        for b in reads:
            t = b.last_w
            if t is not None and deps.get(t[0], 0) < t[1]:
                deps[t[0]] = t[1]
        for b in writes:
            t = b.last_w
            if t is not None and deps.get(t[0], 0) < t[1]:
                deps[t[0]] = t[1]
            for s, v in b.readers.items():
                if deps.get(s, 0) < v:
                    deps[s] = v
        waits = []
        w = self.waited[eng]
        for s, v in deps.items():
            if s == "pe" and eng == "pe":
                continue
            if w.get(s, 0) >= v:
                continue
            w[s] = v
            waits.append((s, v))
        return waits

    def _commit(self, tok, reads, writes):
        s, v = tok
        for b in reads:
            if b.readers.get(s, 0) < v:
                b.readers[s] = v
        for b in writes:
            b.last_w = tok
            b.readers = {}

    def op(self, eng, fn, reads=(), writes=(), inc=True):
        waits = self._collect(eng, reads, writes)
        if inc:
            self.cnt[eng] += 1
            tok = (eng, self.cnt[eng])
        else:
            tok = (eng, self.cnt[eng] + 1)
        self._commit(tok, reads, writes)
        self.streams[eng].append((fn, waits, eng if inc else None, 1))

    def dma(self, q, out, in_, reads, writes, dsem):
        waits = self._collect(q, reads, writes)
        dsem.count += 16
        tok = (dsem.name, dsem.count)
        self._commit(tok, reads, writes)
        self.streams[q].append(
            (lambda e, out=out, in_=in_: e.dma_start(out=out, in_=in_), waits, dsem.name, 16))

    def barrier(self):
        cur = {e: self.cnt[e] for e in ENGS if self.cnt[e] > 0}
        for d in self.dsems:
            if d.count > 0:
                cur[d.name] = d.count
        for e in ENGS:
            w = self.waited[e]
            waits = []
            for s, v in cur.items():
                if s == e and e == "pe":
                    w[s] = v
                    continue
                if w.get(s, 0) >= v:
                    continue
                w[s] = v
                waits.append((s, v))
            if waits:
                self.streams[e].append((None, waits, None, 0))

    def emit(self, stack):
        nc = self.nc
        sems = {}
        for n in self.semnames:
            sems[n] = stack.enter_context(nc.semaphore("s_" + n))
        block = stack.enter_context(nc.Block())
        handles = {"pe": block.tensor, "act": block.scalar, "dve": block.vector,
                   "pool": block.gpsimd, "sp": block.sync}
        for e in ENGS:
            stream = self.streams[e]

            def body(eng, stream=stream):
                for fn, waits, incsem, incv in stream:
                    for s, v in waits:
                        eng.wait_ge(sems[s], v)
                    if fn is None:
                        continue
                    ins = fn(eng)
                    if incsem is not None:
                        ins.then_inc(sems[incsem], incv)
            handles[e](body)


def _dtsize(dt):
    return 2 if dt == BF16 else 4


class KB:
    def __init__(self, n_layers=DEPTH, dbg=False, wdepth=DEPTH):
        self.n_layers = n_layers
        self.wdepth = wdepth
        self.dbg = dbg
        self.nc = bass.Bass("TRN2", target_bir_lowering=False)
        self.S = Sched(self.nc)

    def tt(self, eng, out, in0, in1, op, R, W):
        self.S.op(eng, lambda e: e.tensor_tensor(out=out, in0=in0, in1=in1, op=op), R, W)

    def ts(self, eng, out, in0, s1, s2, op0, op1, R, W):
        if s2 is None:
            self.S.op(eng, lambda e: e.tensor_scalar(out=out, in0=in0, scalar1=s1, scalar2=None, op0=op0), R, W)
        else:
            self.S.op(eng, lambda e: e.tensor_scalar(out=out, in0=in0, scalar1=s1, scalar2=s2, op0=op0, op1=op1), R, W)

    def stt(self, out, in0, scalar, in1, op0, op1, R, W):
        self.S.op("dve", lambda e: e.scalar_tensor_tensor(out=out, in0=in0, scalar=scalar, in1=in1, op0=op0, op1=op1), R, W)

    def act(self, out, in_, func, R, W, scale=1.0, bias=0.0):
        self.S.op("act", lambda e: e.activation(out=out, in_=in_, func=func, bias=bias, scale=scale), R, W)

    def cp(self, eng, out, in_, R, W):
        if eng == "act":
            self.S.op("act", lambda e: e.activation(out=out, in_=in_, func=AF.Copy), R, W)
        else:
            self.S.op(eng, lambda e: e.tensor_copy(out=out, in_=in_), R, W)

    def mm(self, out, lhsT, rhs, start, stop, R, W, inc):
        self.S.op("pe", lambda e: e.matmul(out, lhsT=lhsT, rhs=rhs, start=start, stop=stop), R, W, inc=inc)

    def tr(self, out, in_, ident, R, W, inc):
        self.S.op("pe", lambda e: e.transpose(out=out, in_=in_, identity=ident), R, W, inc=inc)

    def red(self, out, in_, op, R, W):
        self.S.op("dve", lambda e: e.tensor_reduce(out=out, in_=in_, axis=AX.X, op=op), R, W)

    def recip(self, out, in_, R, W):
        self.S.op("dve", lambda e: e.reciprocal(out=out, in_=in_), R, W)

    def memset(self, eng, ap, val, R, W):
        self.S.op(eng, lambda e: e.memset(ap, val), R, W)

    def arena_reset(self):
        self.aoff = 0

    def alloc(self, shape, dt):
        nel = 1
        for s in shape[1:]:
            nel *= s
        nbytes = nel * _dtsize(dt)
        nbytes = (nbytes + 31) // 32 * 32
        off = self.aoff
        self.aoff += nbytes
        assert self.aoff <= self.arena_bytes, ("arena overflow", self.aoff, self.arena_bytes)
        w0 = off // 4
        ap = self.arena[0:shape[0], w0:w0 + nbytes // 4]
        if dt != F32:
            ap = ap.bitcast(dt)
        ap = ap[:, 0:nel]
        if len(shape) == 3:
            ap = ap.rearrange("p (a b) -> p a b", a=shape[1])
        elif len(shape) == 4:
            ap = ap.rearrange("p (a b c) -> p a b c", a=shape[1], b=shape[2])
        return ap

    def bufs(self, n):
        return [Buf() for _ in range(n)]

    def build(self):
        nc = self.nc
        S = self.S
        dbg = self.dbg
        L = self.n_layers
        self.stack = ExitStack()
        st = self.stack

        def din(name, shape, dt=F32):
            return nc.dram_tensor(name, list(shape), dt, kind="ExternalInput").ap()

        def dscr(name, shape, dt=F32):
            kind = "ExternalOutput" if dbg else "Internal"
            return nc.dram_tensor(name, list(shape), dt, kind=kind).ap()

        self.xT = din("xT", [DM, SEQ])
        self.w_in = din("w_in", [self.wdepth, DM, N_IN])
        self.w_pa = din("w_pa", [self.wdepth, 1024, DM])
        self.w_pb = din("w_pb", [self.wdepth, 1024, DM])
        self.w_pc = din("w_pc", [self.wdepth, 1024, DM])
        self.w_out = din("w_out", [self.wdepth, DM, DM])
        self.w_up = din("w_up", [self.wdepth, DM, 2 * D_FF])
        self.w_down = din("w_down", [self.wdepth, D_FF, DM])
        self.i_norm_mix = din("norm_mix_t", [self.wdepth, 128, 16])
        self.i_norm_ffn = din("norm_ffn_t", [self.wdepth, 128, 16])
        self.i_big = din("b_ig_t", [self.wdepth, 8, 1])
        self.i_bfg = din("b_fg_t", [self.wdepth, 8, 1])
        self.i_retgn = din("ret_gn_rep", [self.wdepth, 128, 1024])
        self.i_mlnorm = din("ml_norm_rep", [self.wdepth, 128, 1024])
        self.i_qnorm = din("q_norm_rep", [self.wdepth, 128, 128])
        self.i_knorm = din("k_norm_rep", [self.wdepth, 128, 128])
        self.i_convw = din("conv_w_t", [self.wdepth, 128, 88, 3])
        self.i_convb = din("conv_b_t", [self.wdepth, 128, 88])
        self.i_tabs = din("c_tabs", [128, 4, 16, 64])
        self.i_small = din("c_small", [128, 24])
        self.i_mask = din("c_mask", [128, 128])
        self.i_ident = din("c_ident", [128, 128])
        self.i_negm = din("c_negm", [128, 8, 8])
        self.i_selfix = din("c_selfix", [128, 2, 8, 8])
        self.outT = nc.dram_tensor("outT", [DM, SEQ], F32, kind="ExternalOutput").ap()
        self.xres = dscr("xres", [DM, SEQ])
        self.z = {}
        for nm, w in (("rq", 1024), ("rk", 1024), ("rv", 1024), ("rg", 1024), ("mq", 512), ("mk", 512),
                      ("mv", 1024), ("mo", 1024), ("aq", 1024), ("ak", 1024), ("av", 1024)):
            self.z[nm] = dscr("z_" + nm, [SEQ, w], F32 if nm in ("aq", "ak") else BF16)
        self.sgT = dscr("sgT", [3 * DM, SEQ], BF16)
        self.yT = [dscr("yT%d" % b, [1024, SEQ], BF16) for b in range(3)]
        self.actT = dscr("actT", [D_FF, SEQ], BF16)
        self.Bx = [[Buf() for _ in range(4)] for _ in range(16)]
        self.Bz = {nm: [[Buf() for _ in range(2)] for _ in range(NT)] for nm in self.z}
        self.Bsg = [[Buf() for _ in range(4)] for _ in range(48)]
        self.ByT = [[Buf() for _ in range(32)] for _ in range(3)]
        self.Bact = [[Buf() for _ in range(4)] for _ in range(NFC)]

        def sb(name, shape, dt):
            return st.enter_context(nc.sbuf_tensor(name, list(shape), dt))

        self.tabs = sb("tabs", [128, 4, 16, 64], F32)
        self.small = sb("small", [128, 24], F32)
        self.maskf = sb("maskf", [128, 128], F32)
        self.maskb = sb("maskb", [128, 128], BF16)
        self.identf = sb("identf", [128, 128], F32)
        self.identb = sb("identb", [128, 128], BF16)
        self.onesf = sb("onesf", [128, 128], F32)
        self.onescb = sb("onescb", [128, 2], BF16)
        self.onesb = sb("onesb", [128, 128], BF16)
        self.kmcol = sb("kmcol", [128, 2], F32)
        self.negm = sb("negm", [128, 8, 8], F32)
        self.selfix = sb("selfix", [128, 2, 8, 8], F32)
        self.gmix = sb("gmix", [128, 16], F32)
        self.gffn = sb("gffn", [128, 16], F32)
        self.big = sb("big", [8, 1], F32)
        self.bfg = sb("bfg", [8, 1], F32)
        self.retgn = sb("retgn", [128, 1024], F32)
        self.mlnorm = sb("mlnorm", [128, 1024], F32)
        self.qnw = sb("qnw", [128, 128], F32)
        self.knw = sb("knw", [128, 128], F32)
        self.convw = sb("convw", [128, 88, 3], F32)
        self.convb = sb("convb", [128, 88], F32)
        self.mtab = sb("mtab", [128, 3, 16, 8], F32)
        self.Bconst = Buf()
        self.Blayer = Buf()
        self.Bmtab = Buf()
        self.arena_bytes = (nc.sbuf_bytes_remaining // 32) * 32 - 64
        self.arena = sb("arena", [128, self.arena_bytes // 4], F32)
        self.bank = [st.enter_context(nc.psum_tensor("bank%d" % i, [128, 512], F32)) for i in range(8)]
        self.Bbank = [Buf() for _ in range(8)]
        self.ds = [S.dsem() for _ in range(40)]
        self.dconst = S.dsem()

        dc = self.dconst
        S.dma("sp", self.tabs[:], self.i_tabs[:, :, :, :], [], [self.Bconst], dc)
        S.dma("sp", self.small[:], self.i_small[:, :], [], [self.Bconst], dc)
        S.dma("sp", self.maskf[:], self.i_mask[:, :], [], [self.Bconst], dc)
        S.dma("sp", self.identf[:], self.i_ident[:, :], [], [self.Bconst], dc)
        S.dma("sp", self.negm[:], self.i_negm[:, :, :], [], [self.Bconst], dc)
        S.dma("sp", self.selfix[:], self.i_selfix[:, :, :, :], [], [self.Bconst], dc)
        S.barrier()
        self.cp("dve", self.maskb[:], self.maskf[:], [self.Bconst], [self.Bconst])
        self.cp("dve", self.identb[:], self.identf[:], [self.Bconst], [self.Bconst])
        self.memset("dve", self.onesf[:], 1.0, [], [self.Bconst])
        self.memset("dve", self.onescb[:], 1.0, [], [self.Bconst])
        self.memset("dve", self.onesb[:], 1.0, [], [self.Bconst])
        self.memset("dve", self.kmcol[:], 1.0 / 256.0, [], [self.Bconst])
        S.barrier()

        for l in range(L):
            self.layer(l)

        S.barrier()
        S.emit(st)
        return nc

    def layer(self, l):
        S = self.S
        dc = self.dconst
        xsrc = self.xT if l == 0 else self.xres
        xdst_final = self.outT if l == self.n_layers - 1 else self.xres
        for dst, src in ((self.gmix, self.i_norm_mix[l]), (self.gffn, self.i_norm_ffn[l]),
                         (self.big, self.i_big[l]), (self.bfg, self.i_bfg[l]),
                         (self.retgn, self.i_retgn[l]), (self.mlnorm, self.i_mlnorm[l]),
                         (self.qnw, self.i_qnorm[l]), (self.knw, self.i_knorm[l]),
                         (self.convw, self.i_convw[l]), (self.convb, self.i_convb[l])):
            S.dma("sp", dst[:], src, [], [self.Blayer], dc)
        S.barrier()
        self.ts("dve", self.qnw[:], self.qnw[:], 128.0 ** -0.5, None, ALU.mult, None, [self.Blayer], [self.Blayer])
        S.barrier()

        self.phase_norm(xsrc, self.gmix)
        self.phase_proj(l)
        S.barrier()
        self.phase_moba(l)
        S.barrier()
        if self.stop_after == "C":
            return
        self.phase_merge(l, xsrc)
        S.barrier()
        if self.stop_after == "G":
            return
        self.phase_norm(self.xres, self.gffn)
        self.phase_ffn_up(l)
        S.barrier()
        if self.stop_after == "F1":
            return
        self.phase_ffn_down(l, xdst_final)
        S.barrier()

    stop_after = None

    def phase_norm(self, xsrc, g):
        S = self.S
        self.arena_reset()
        self.hT = self.alloc([128, 16, SEQ], BF16)
        self.BhT = [[Buf() for _ in range(8)] for _ in range(16)]
        mark = self.aoff
        xb = [self.alloc([128, 16, 256], F32) for _ in range(2)]
        Bxb = self.bufs(2)
        sq = [self.alloc([128, 256], F32) for _ in range(4)]
        Bsq = self.bufs(4)
        sd = [self.alloc([128, 256], F32) for _ in range(2)]
        Bsd = self.bufs(2)
        xv = xsrc.rearrange("(c p) t -> p c t", p=128)
        for t in range(8):
            b = t % 2
            S.dma("sp", xb[b], xv[:, :, t * 256:(t + 1) * 256], [self.Bx[c][t // 2] for c in range(16)], [Bxb[b]], self.ds[b])
            ps = self.bank[b][:, 0:256]
            Bps = self.Bbank[b]
            for c in range(16):
                k = c % 4
                self.act(sq[k], xb[b][:, c, :], AF.Square, [Bxb[b]], [Bsq[k]])
                self.mm(ps, self.onesf[:], sq[k], c == 0, c == 15, [Bsq[k], self.Bconst], [Bps], inc=True)
            self.act(sd[b], ps, AF.Sqrt, [Bps], [Bsd[b]], scale=1.0 / DM, bias=1e-6)
            self.recip(sd[b], sd[b], [Bsd[b]], [Bsd[b]])
            for c in range(16):
                self.stt(self.hT[:, c, t * 256:(t + 1) * 256], xb[b][:, c, :], g[:, c:c + 1], sd[b],
                         ALU.mult, ALU.mult, [Bxb[b], Bsd[b], self.Blayer], [self.BhT[c][t]])
        self.aoff = mark
        S.barrier()

    def rot4(self, eng, dst, src, cos, sin, tmp, Btmp, R, Bsrc, Bdst, nh):
        sv = src.rearrange("p (h two d) -> p h two d", h=nh, two=2)
        dv = dst.rearrange("p (h two d) -> p h two d", h=nh, two=2)
        x1 = sv[:, :, 0, :]
        x2 = sv[:, :, 1, :]
        cb = cos.unsqueeze(1).broadcast_to([128, nh, 64])
        sbb = sin.unsqueeze(1).broadcast_to([128, nh, 64])
        t1v = tmp[0].rearrange("p (h d) -> p h d", h=nh)
        t2v = tmp[1].rearrange("p (h d) -> p h d", h=nh)
        self.tt(eng, t1v, x1, cb, ALU.mult, [Bsrc] + R, [Btmp[0]])
        self.tt(eng, t2v, x2, sbb, ALU.mult, [Bsrc] + R, [Btmp[1]])
        self.tt(eng, dv[:, :, 0, :], t1v, t2v, ALU.subtract, [Btmp[0], Btmp[1]], [Bdst])
        self.tt(eng, t1v, x1, sbb, ALU.mult, [Bsrc] + R, [Btmp[0]])
        self.tt(eng, t2v, x2, cb, ALU.mult, [Bsrc] + R, [Btmp[1]])
        self.tt(eng, dv[:, :, 1, :], t1v, t2v, ALU.add, [Btmp[0], Btmp[1]], [Bdst])

    def phase_proj(self, l):
        S = self.S
        hT = self.hT
        BhT = self.BhT
        bank, Bbank = self.bank, self.Bbank
        NW = 2
        Wt = [self.alloc([128, 16, 512], BF16) for _ in range(NW)]
        BW = self.bufs(NW)
        NR = 4
        stg = [self.alloc([128, 512], F32) for _ in range(NR)]
        Bstg = self.bufs(NR)
        stgb = [self.alloc([128, 512], BF16) for _ in range(NR)]
        Bstgb = self.bufs(NR)
        rt = {e: [self.alloc([128, 256], F32) for _ in range(2)] for e in ("dve", "pool")}
        Brt = {e: self.bufs(2) for e in ("dve", "pool")}
        r4 = [self.alloc([128, 4], F32) for _ in range(NR)]
        Br4 = self.bufs(NR)
        wv = self.w_in[l].rearrange("(c p) n -> p c n", p=128)
        self.pcnt = 0
        Wg = self.alloc([128, 16, 16], BF16)
        BWg = Buf()
        S.dma("pool", Wg, wv[:, :, C_MI:C_MI + 16], [], [BWg], self.ds[6])
        r8 = [self.alloc([128, 4], F32) for _ in range(8)]
        Br8 = self.bufs(8)
        mixer_base = self.aoff
        stg2 = stg3 = stg4 = None
        Bstg2 = self.bufs(8)
        Bstg3 = self.bufs(8)
        Bstg4 = self.bufs(8)
        A = self.alloc([8, SEQ], F32)
        Bm = self.alloc([8, SEQ], F32)
        Cc = self.alloc([8, SEQ], F32)
        Dd = self.alloc([8, SEQ], F32)
        BA, BB, BC, BD = self.bufs(4)
        blocks = []
        seg_of = []
        order = (("rq", C_RQ, 1024, 0), ("rk", C_RK, 1024, 0), ("rv", C_RV, 1024, 0), ("rg", C_RG, 1024, 0),
                 ("mq", C_MQ, 512, 1), ("mk", C_MK, 512, 1), ("mv", C_MV, 1024, 1), ("mo", C_MO, 1024, 1),
                 ("av", C_AV, 1024, 2))
        for nm, c0, w, seg in order:
            for j in range(w // 512):
                blocks.append(("tm", nm, c0 + j * 512, j))
                seg_of.append(seg)
        for gb in range(12):
            blocks.append(("fm", None, C_GA + gb * 512, gb))
            seg_of.append(2)
        for nm, c0, w, seg in (("aq", C_AQ, 1024, 3), ("ak", C_AK, 1024, 3)):
            for j in range(w // 512):
                blocks.append(("tm", nm, c0 + j * 512, j))
                seg_of.append(seg)
        nblk = len(blocks)

        def load(bi):
            kind, nm, c0, j = blocks[bi]
            s_ = bi % NW
            S.dma("pool", Wt[s_], wv[:, :, c0:c0 + 512], [], [BW[s_]], self.ds[0 + s_])

        load(0)
        load(1)
        for which, dstT, Bd in ((0, A, BA), (1, Bm, BB)):
            for tb in range(4):
                k = self.pcnt % 2
                self.pcnt += 1
                ps = bank[k]
                Bps = Bbank[k]
                for c in range(16):
                    self.mm(ps[0:8, :], Wg[:, c, which * 8:(which + 1) * 8], hT[:, c, tb * 512:(tb + 1) * 512],
                            c == 0, c == 15, [BhT[c][2 * tb], BhT[c][2 * tb + 1], BWg], [Bps], inc=(c == 15))
                self.cp("act", dstT[:, tb * 512:(tb + 1) * 512], ps[0:8, :], [Bps], [Bd])
        self.ts("dve", A, A, self.big[:, 0:1], 1.0 / 15.0, ALU.add, ALU.mult, [BA, self.Blayer], [BA])
        self.act(A, A, AF.Tanh, [BA], [BA])
        self.ts("dve", Bm, Bm, self.bfg[:, 0:1], 1.0 / 15.0, ALU.add, ALU.mult, [BB, self.Blayer], [BB])
        self.act(Bm, Bm, AF.Tanh, [BB], [BB])
        self.act(Bm, Bm, AF.Exp, [BB], [BB], scale=-15.0)
        self.act(Bm, Bm, AF.Ln, [BB], [BB], bias=1.0)
        for n in range(NT):
            sl = slice(n * 128, (n + 1) * 128)
            self.S.op("dve", lambda e, sl=sl: e.tensor_tensor_scan(out=Cc[:, sl], data0=self.onesf[0:8, :], data1=Bm[:, sl],
                                                                  initial=0.0, op0=ALU.mult, op1=ALU.subtract),
                      [BB, self.Bconst], [BC])
        self.act(Dd, Cc, AF.Exp, [BC], [BD])
        self.stt(A, A, 15.0, Cc, ALU.mult, ALU.subtract, [BA, BC], [BA])
        self.act(A, A, AF.Exp, [BA], [BA], bias=math.log(0.125))
        cl = Cc.rearrange("p (n t) -> p n t", t=128)[:, :, 127:128].broadcast_to([8, NT, 128])
        self.act(Bm.rearrange("p (n t) -> p n t", t=128), cl, AF.Exp, [BC], [BB])
        k = self.pcnt % 2
        self.pcnt += 1
        ps = bank[k]
        Bps = Bbank[k]
        idx = 0
        for wi, (src, Bs) in enumerate(((Dd, BD), (A, BA), (Bm, BB))):
            for n in range(NT):
                idx += 1
                col = (wi * NT + n) * 8
                self.tr(ps[:, col:col + 8], src[:, n * 128:(n + 1) * 128], self.identf[0:8, 0:8],
                        [Bs, self.Bconst], [Bps], inc=(idx == 48))
        self.cp("act", self.mtab[:].rearrange("p a n h -> p (a n h)"), ps[:, 0:384], [Bps], [self.Bmtab])
        S.barrier()
        self.aoff = mixer_base


        cosr, sinr = self.tabs[:, 0], self.tabs[:, 1]
        cosp, sinp = self.tabs[:, 2], self.tabs[:, 3]
        sq_r = self.small[:, 0:8]
        sk_r = self.small[:, 8:16]

        def evac(nm, j, i, ps, Bps, k):
            eng = "dve" if i % 2 == 0 else "pool"
            dst = self.z[nm]
            rows = slice(i * 128, (i + 1) * 128)
            cols = slice(j * 512, (j + 1) * 512)
            if nm in ("rv", "mv", "av"):
                self.cp("act", stgb[k], ps[:], [Bps], [Bstgb[k]])
                out_t, Bout = stgb[k], Bstgb[k]
            elif nm in ("rq", "rk"):
                sc = sq_r if nm == "rq" else sk_r
                for hh in range(4):
                    h = j * 4 + hh
                    self.S.op("act", lambda e, hh=hh, h=h, sc=sc: e.activation(out=stg[k][:, hh * 128:(hh + 1) * 128],
                                                                             in_=ps[:, hh * 128:(hh + 1) * 128], func=AF.Copy,
                                                                             scale=sc[:, h:h + 1]),
                              [Bps, self.Bconst], [Bstg[k]])
                self.rot4(eng, stgb[k], stg[k], cosr[:, i, :], sinr[:, i, :], rt[eng], Brt[eng], [self.Bconst], Bstg[k], Bstgb[k], 4)
                out_t, Bout = stgb[k], Bstgb[k]
            elif nm in ("rg", "mo"):
                fn = AF.Silu if nm == "rg" else AF.Sigmoid
                gw = self.retgn if nm == "rg" else self.mlnorm
                self.act(stg[k], ps[:], fn, [Bps], [Bstg[k]])
                self.tt(eng, stgb[k], stg[k], gw[:, cols], ALU.mult, [Bstg[k], self.Blayer], [Bstgb[k]])
                out_t, Bout = stgb[k], Bstgb[k]
            elif nm in ("mq", "mk"):
                tab = self.mtab[:, 0 if nm == "mq" else 1, i, :]
                tb_ = tab.unsqueeze(2).broadcast_to([128, 8, 64])
                v8 = lambda ap: ap.rearrange("p (h d) -> p h d", h=8)
                if i % 2 == 0:
                    self.tt("dve", v8(stgb[k]), v8(ps[:]), tb_, ALU.mult, [Bps, self.Bmtab], [Bstgb[k]])
                else:
                    self.cp("act", stg[k], ps[:], [Bps], [Bstg[k]])
                    self.tt("pool", v8(stgb[k]), v8(stg[k]), tb_, ALU.mult, [Bstg[k], self.Bmtab], [Bstgb[k]])
                out_t, Bout = stgb[k], Bstgb[k]
            else:
                gw = self.qnw if nm == "aq" else self.knw
                v4_ = lambda ap: ap.rearrange("p (h d) -> p h d", h=4)
                k8 = (j * NT + i) % 8
                self.act(stg2[k8], ps[:], AF.Square, [Bps], [Bstg2[k8]])
                self.cp("act", stg3[k8], ps[:], [Bps], [Bstg3[k8]])
                self.red(r8[k8], v4_(stg2[k8]), ALU.add, [Bstg2[k8]], [Br8[k8]])
                self.act(r8[k8], r8[k8], AF.Sqrt, [Br8[k8]], [Br8[k8]], scale=1.0 / 128.0, bias=1e-6)
                self.recip(r8[k8], r8[k8], [Br8[k8]], [Br8[k8]])
                self.tt(eng, v4_(stg3[k8]), v4_(stg3[k8]), r8[k8].unsqueeze(2).broadcast_to([128, 4, 128]), ALU.mult,
                        [Bstg3[k8], Br8[k8]], [Bstg3[k8]])
                self.tt(eng, v4_(stg3[k8]), v4_(stg3[k8]), gw[:].unsqueeze(1).broadcast_to([128, 4, 128]), ALU.mult,
                        [Bstg3[k8], self.Blayer], [Bstg3[k8]])
                self.rot4(eng, stg4[k8], stg3[k8], cosp[:, i, :], sinp[:, i, :], rt[eng], Brt[eng], [self.Bconst], Bstg3[k8], Bstg4[k8], 4)
                out_t, Bout = stg4[k8], Bstg4[k8]
                S.dma("sp", dst[rows, cols], out_t, [Bout], [self.Bz[nm][i][j]], self.ds[28 + k8])
                return
            S.dma("sp", dst[rows, cols], out_t, [Bout], [self.Bz[nm][i][j]], self.ds[28 + k])

        gen = None
        cur_seg = 0
        stepno = 0
        stride = {0: 1, 1: 1, 2: 2, 3: 1}

        def step(force=False):
            nonlocal gen, stepno
            stepno += 1
            if gen is not None and (force or stepno % stride[cur_seg] == 0):
                try:
                    next(gen)
                except StopIteration:
                    gen = None

        def drain():
            nonlocal gen
            while gen is not None:
                step(True)

        for bi, (kind, nm, c0, j) in enumerate(blocks):
            if seg_of[bi] != cur_seg:
                drain()
                cur_seg = seg_of[bi]
                S.barrier()
                self.aoff = mixer_base
                if cur_seg in (1, 2):
                    gen = self.ret_gen(l) if cur_seg == 1 else self.mlstm_gen(l)
                else:
                    stg2 = [self.alloc([128, 512], F32) for _ in range(8)]
                    stg3 = [self.alloc([128, 512], F32) for _ in range(8)]
                    stg4 = [self.alloc([128, 512], F32) for _ in range(8)]
            if bi + 1 < nblk and bi >= 1:
                load(bi + 1)
            s_ = bi % NW
            if kind == "tm":
                for i in range(NT):
                    k = self.pcnt % (4 if cur_seg in (0, 3) else 2)
                    self.pcnt += 1
                    ps = bank[k]
                    Bps = Bbank[k]
                    for c in range(16):
                        self.mm(ps[:], hT[:, c, i * 128:(i + 1) * 128], Wt[s_][:, c, :], c == 0, c == 15,
                                [BhT[c][i // 2], BW[s_]], [Bps], inc=(c == 15))
                    evac(nm, j, i, ps, Bps, k)
                    step()
            else:
                for nn in range(4):
                    for tb in range(4):
                        k = self.pcnt % 2
                        self.pcnt += 1
                        ps = bank[k]
                        Bps = Bbank[k]
                        for c in range(16):
                            self.mm(ps[:], Wt[s_][:, c, nn * 128:(nn + 1) * 128], hT[:, c, tb * 512:(tb + 1) * 512],
                                    c == 0, c == 15, [BhT[c][2 * tb], BhT[c][2 * tb + 1], BW[s_]], [Bps], inc=(c == 15))
                        self.act(stgb[k], ps[:], AF.Sigmoid, [Bps], [Bstgb[k]])
                        row = (j * 4 + nn) * 128
                        S.dma("sp", self.sgT[row:row + 128, tb * 512:(tb + 1) * 512], stgb[k], [Bstgb[k]],
                              [self.Bsg[j * 4 + nn][tb]], self.ds[32 + k])
                        step()
        drain()

    def bcast_h(self, ap8, d):
        return ap8.unsqueeze(2).broadcast_to([ap8.shape[0], 8, d])

    def ret_gen(self, l):
        S = self.S
        bank, Bbank = self.bank, self.Bbank
        NL = 3
        ld = []
        for b in range(NL):
            ld.append(dict(q=self.alloc([128, 1024], BF16), k=self.alloc([128, 1024], BF16),
                           g=self.alloc([128, 1024], BF16), v=self.alloc([128, 1024], BF16),
                           Bq=Buf(), Bk=Buf(), Bg=Buf(), Bv=Buf()))
        qT = [self.alloc([128, 1024], BF16) for _ in range(2)]
        kT = [self.alloc([128, 1024], BF16) for _ in range(2)]
        BqT, BkT = self.bufs(2), self.bufs(2)
        sTm = [self.alloc([128, 1024], BF16) for _ in range(2)]
        BsTm = [self.bufs(2) for _ in range(2)]
        R32 = self.alloc([128, 1024], F32)
        Rbf = self.alloc([128, 1024], BF16)
        Aa = self.alloc([128, 1024], F32)
        BR32, BRbf, BA = Buf(), Buf(), Buf()
        osb = self.alloc([128, 1024], F32)
        sqt = self.alloc([128, 1024], F32)
        cc = sqt
        Bosb, Bsqt, Bcc = self.bufs(3)
        yb = [self.alloc([128, 1024], BF16) for _ in range(2)]
        Byb = self.bufs(2)
        yTs = [self.alloc([128, 8, 128], BF16) for _ in range(2)]
        ByTs = self.bufs(2)
        st8 = [self.alloc([128, 8], F32) for _ in range(6)]
        Bst = self.bufs(6)
        gC = self.small[:, 16:24]
        zq, zk, zv, zg = self.z["rq"], self.z["rk"], self.z["rv"], self.z["rg"]
        Bz = self.Bz
        yTa = self.yT[0].rearrange("(h e) t -> e h t", e=128)
        v4 = lambda ap: ap.rearrange("p (h d) -> p h d", h=8)
        maskb4 = self.maskb[:].unsqueeze(1).broadcast_to([128, 4, 128])
        pq = bank[2][:].bitcast(BF16)
        pk = bank[3][:].bitcast(BF16)

        def loads(n):
            b = n % NL
            L_ = ld[b]
            rs = slice(n * 128, (n + 1) * 128)
            S.dma("sp", L_["q"], zq[rs, :], Bz["rq"][n], [L_["Bq"]], self.ds[4 + b])
            S.dma("sp", L_["k"], zk[rs, :], Bz["rk"][n], [L_["Bk"]], self.ds[7 + b])
            S.dma("sp", L_["g"], zg[rs, :], Bz["rg"][n], [L_["Bg"]], self.ds[10 + b])
            S.dma("sp", L_["v"], zv[rs, :], Bz["rv"][n], [L_["Bv"]], self.ds[13 + b])

        def front(n):
            L_ = ld[n % NL]
            s_ = n % 2
            for h in range(8):
                self.tr(pq[:, h * 128:(h + 1) * 128], L_["q"][:, h * 128:(h + 1) * 128], self.identb[:], [L_["Bq"], self.Bconst], [Bbank[2]], inc=(h == 7))
            for h in range(8):
                self.tr(pk[:, h * 128:(h + 1) * 128], L_["k"][:, h * 128:(h + 1) * 128], self.identb[:], [L_["Bk"], self.Bconst], [Bbank[3]], inc=(h == 7))
            self.cp("act", qT[s_], pq, [Bbank[2]], [BqT[s_]])
            self.cp("dve", kT[s_], pk, [Bbank[3]], [BkT[s_]])
            for h in range(8):
                hb = 4 + h // 4
                self.mm(bank[hb][:, (h % 4) * 128:(h % 4 + 1) * 128], kT[s_][:, h * 128:(h + 1) * 128], qT[s_][:, h * 128:(h + 1) * 128],
                        True, True, [BkT[s_], BqT[s_]], [Bbank[hb]], inc=(h % 4 == 3))
            for hb in range(2):
                self.tt("dve", sTm[s_][:, hb * 512:(hb + 1) * 512].rearrange("p (h d) -> p h d", h=4),
                        bank[4 + hb][:].rearrange("p (h d) -> p h d", h=4), maskb4, ALU.mult,
                        [Bbank[4 + hb], self.Bconst], [BsTm[s_][hb]])

        def mid(n):
            L_ = ld[n % NL]
            s_ = n % 2
            for h in range(8):
                hb = 6 + h // 4
                osl = bank[hb][:, (h % 4) * 128:(h % 4 + 1) * 128]
                hs = slice(h * 128, (h + 1) * 128)
                self.mm(osl, sTm[s_][:, hs], L_["v"][:, hs], True, n == 0, [BsTm[s_][h // 4], L_["Bv"]], [Bbank[hb]],
                        inc=(n == 0 and h % 4 == 3))
                if n > 0:
                    self.mm(osl, qT[s_][:, hs], Rbf[:, hs], False, True, [BqT[s_], BRbf], [Bbank[hb]], inc=(h % 4 == 3))
            if n + 1 < NT:
                for h in range(8):
                    hb = 4 + h // 4
                    hs = slice(h * 128, (h + 1) * 128)
                    self.mm(bank[hb][:, (h % 4) * 128:(h % 4 + 1) * 128], L_["k"][:, hs], L_["v"][:, hs], True, True,
                            [L_["Bk"], L_["Bv"]], [Bbank[hb]], inc=(h % 4 == 3))
                for hb in range(2):
                    hsl = slice(hb * 512, (hb + 1) * 512)
                    if n == 0:
                        self.cp("dve", Aa[:, hsl], bank[4 + hb][:], [Bbank[4 + hb]], [BA])
                    else:
                        self.tt("dve", Aa[:, hsl], bank[4 + hb][:], R32[:, hsl], ALU.add, [Bbank[4 + hb], BR32], [BA])
                self.tt("pool", v4(R32), v4(Aa), self.bcast_h(gC, 128), ALU.mult, [BA, self.Bconst], [BR32])
                self.cp("act", Rbf, R32, [BR32], [BRbf])

        def epi(n):
            L_ = ld[n % NL]
            for hb in range(2):
                self.cp("act", osb[:, hb * 512:(hb + 1) * 512], bank[6 + hb][:], [Bbank[6 + hb]], [Bosb])
            self.act(sqt, osb, AF.Square, [Bosb], [Bsqt])
            s1, s2, mean, msq, var, rstd = st8
            self.red(s1, v4(osb), ALU.add, [Bosb], [Bst[0]])
            self.red(s2, v4(sqt), ALU.add, [Bsqt], [Bst[1]])
            self.ts("dve", mean, s1, 1.0 / 128.0, None, ALU.mult, None, [Bst[0]], [Bst[2]])
            self.tt("dve", msq, mean, mean, ALU.mult, [Bst[2]], [Bst[3]])
            self.stt(var, s2, 1.0 / 128.0, msq, ALU.mult, ALU.subtract, [Bst[1], Bst[3]], [Bst[4]])
            self.act(rstd, var, AF.Sqrt, [Bst[4]], [Bst[5]], bias=1e-5)
            self.recip(rstd, rstd, [Bst[5]], [Bst[5]])
            self.tt("pool", v4(cc), v4(osb), self.bcast_h(mean, 128), ALU.subtract, [Bosb, Bst[2]], [Bcc])
            self.tt("pool", v4(cc), v4(cc), self.bcast_h(rstd, 128), ALU.mult, [Bcc, Bst[5]], [Bcc])
            self.tt("dve", yb[n % 2], cc, L_["g"], ALU.mult, [Bcc, L_["Bg"]], [Byb[n % 2]])

        def outT(n):
            for h in range(8):
                self.tr(pq[:, h * 128:(h + 1) * 128], yb[n % 2][:, h * 128:(h + 1) * 128], self.identb[:], [Byb[n % 2], self.Bconst], [Bbank[2]], inc=(h == 7))
            ys = yTs[n % 2]
            self.cp("act", ys.rearrange("p h t -> p (h t)"), pq, [Bbank[2]], [ByTs[n % 2]])
            S.dma("sp", yTa[:, :, n * 128:(n + 1) * 128], ys, [ByTs[n % 2]], [self.ByT[0][n]], self.ds[16 + n % 2])

        loads(0)
        loads(1)
        front(0)
        yield
        for n in range(NT):
            if n + 2 < NT:
                loads(n + 2)
            mid(n)
            yield
            if n + 1 < NT:
                front(n + 1)
                yield
            epi(n)
            yield
            if n > 0:
                outT(n - 1)
                yield
        outT(NT - 1)
        yield

    def mlstm_gen(self, l):
        S = self.S
        bank, Bbank = self.bank, self.Bbank
        NL = 3
        ld = []
        for b in range(NL):
            ld.append(dict(q=self.alloc([128, 512], BF16), k=self.alloc([128, 512], BF16),
                           o=self.alloc([128, 1024], BF16), v=self.alloc([128, 1024], BF16),
                           Bq=Buf(), Bk=Buf(), Bo=Buf(), Bv=Buf()))
        qT = [self.alloc([64, 1024], BF16) for _ in range(2)]
        kT = [self.alloc([64, 1024], BF16) for _ in range(2)]
        BqT, BkT = self.bufs(2), self.bufs(2)
        sTm = [self.alloc([128, 1024], BF16) for _ in range(2)]
        BsTm = [self.bufs(2) for _ in range(2)]
        C32 = self.alloc([64, 1024], F32)
        Cbf = self.alloc([64, 1024], BF16)
        Aa = self.alloc([64, 1024], F32)
        n32 = self.alloc([64, 8], F32)
        nbf = self.alloc([64, 8], BF16)
        An = self.alloc([64, 8], F32)
        BC32, BCbf, BA, Bn32, Bnbf, BAn = self.bufs(6)
        hv = self.alloc([128, 1024], F32)
        sqt = self.alloc([128, 1024], F32)
        Bhv, Bsqt = self.bufs(2)
        yb = [self.alloc([128, 1024], BF16) for _ in range(2)]
        Byb = self.bufs(2)
        yTs = [self.alloc([128, 8, 128], BF16) for _ in range(2)]
        ByTs = self.bufs(2)
        st8 = [self.alloc([128, 8], F32) for _ in range(3)]
        Bst = self.bufs(3)
        zq, zk, zv, zo = self.z["mq"], self.z["mk"], self.z["mv"], self.z["mo"]
        Bz = self.Bz
        yTb = self.yT[1].rearrange("(h e) t -> e h t", e=128)
        v4 = lambda ap: ap.rearrange("p (h d) -> p h d", h=8)
        maskb4 = self.maskb[:].unsqueeze(1).broadcast_to([128, 4, 128])
        mtab = self.mtab
        pq = bank[2][:].bitcast(BF16)
        pk = pq
        pD = bank[7]
        BKn = Buf()

        def loads(n):
            b = n % NL
            L_ = ld[b]
            rs = slice(n * 128, (n + 1) * 128)
            S.dma("sp", L_["q"], zq[rs, :], [Bz["mq"][n][0]], [L_["Bq"]], self.ds[36 + b])
            S.dma("sp", L_["k"], zk[rs, :], [Bz["mk"][n][0]], [L_["Bk"]], self.ds[18 + b])
            S.dma("sp", L_["o"], zo[rs, :], Bz["mo"][n], [L_["Bo"]], self.ds[21 + b])
            S.dma("sp", L_["v"], zv[rs, :], Bz["mv"][n], [L_["Bv"]], self.ds[24 + b])

        def front(n):
            L_ = ld[n % NL]
            s_ = n % 2
            for h in range(8):
                self.tr(pq[0:64, h * 128:(h + 1) * 128], L_["q"][:, h * 64:(h + 1) * 64], self.identb[:], [L_["Bq"], self.Bconst], [Bbank[2]], inc=(h == 7))
            self.cp("act", qT[s_], pq[0:64, :], [Bbank[2]], [BqT[s_]])
            for h in range(8):
                self.tr(pk[0:64, h * 128:(h + 1) * 128], L_["k"][:, h * 64:(h + 1) * 64], self.identb[:], [L_["Bk"], self.Bconst], [Bbank[2]], inc=(h == 7))
            self.cp("dve", kT[s_], pk[0:64, :], [Bbank[2]], [BkT[s_]])
            for h in range(8):
                hb = 3 + h // 4
                hs = slice(h * 128, (h + 1) * 128)
                self.mm(bank[hb][:, (h % 4) * 128:(h % 4 + 1) * 128], kT[s_][:, hs], qT[s_][:, hs], True, True, [BkT[s_], BqT[s_]], [Bbank[hb]], inc=(h % 4 == 3))
            for hb in range(2):
                self.tt("dve", sTm[s_][:, hb * 512:(hb + 1) * 512].rearrange("p (h d) -> p h d", h=4),
                        bank[3 + hb][:].rearrange("p (h d) -> p h d", h=4), maskb4, ALU.mult,
                        [Bbank[3 + hb], self.Bconst], [BsTm[s_][hb]])

        def mid(n):
            L_ = ld[n % NL]
            s_ = n % 2
            eal = mtab[0:64, 2, n, :]
            for h in range(8):
                hb = 5 + h // 4
                osl = bank[hb][:, (h % 4) * 128:(h % 4 + 1) * 128]
                hs = slice(h * 128, (h + 1) * 128)
                self.mm(osl, sTm[s_][:, hs], L_["v"][:, hs], True, n == 0, [BsTm[s_][h // 4], L_["Bv"]], [Bbank[hb]], inc=False)
                if n > 0:
                    self.mm(osl, qT[s_][:, hs], Cbf[:, hs], False, True, [BqT[s_], BCbf], [Bbank[hb]], inc=False)
                self.mm(pD[:, h:h + 1], sTm[s_][:, hs], self.onescb[:, 0:1], True, n == 0, [BsTm[s_][h // 4], self.Bconst], [Bbank[7]],
                        inc=(n == 0 and h == 7))
                if n > 0:
                    self.mm(pD[:, h:h + 1], qT[s_][:, hs], nbf[:, h:h + 1], False, True, [BqT[s_], Bnbf], [Bbank[7]], inc=(h == 7))
            dm, rden, rstd = st8
            self.act(dm, pD[:, 0:8], AF.Abs, [Bbank[7]], [Bst[0]])
            self.ts("dve", dm, dm, 1.0, None, ALU.max, None, [Bst[0]], [Bst[0]])
            self.recip(rden, dm, [Bst[0]], [Bst[1]])
            for hb in range(2):
                self.tt("dve", hv[:, hb * 512:(hb + 1) * 512].rearrange("p (h d) -> p h d", h=4),
                        bank[5 + hb][:].rearrange("p (h d) -> p h d", h=4),
                        rden[:, hb * 4:(hb + 1) * 4].unsqueeze(2).broadcast_to([128, 4, 128]), ALU.mult,
                        [Bbank[5 + hb], Bbank[7], Bst[1]], [Bhv])
            if n + 1 < NT:
                pKn = bank[7]
                for h in range(8):
                    hb = 3 + h // 4
                    hs = slice(h * 128, (h + 1) * 128)
                    self.mm(bank[hb][0:64, (h % 4) * 128:(h % 4 + 1) * 128], L_["k"][:, h * 64:(h + 1) * 64], L_["v"][:, hs], True, True,
                            [L_["Bk"], L_["Bv"]], [Bbank[hb]], inc=(h % 4 == 3))
                for h in range(8):
                    self.mm(pKn[0:64, 8 + h:9 + h], L_["k"][:, h * 64:(h + 1) * 64], self.onescb[:, 0:1], True, True,
                            [L_["Bk"], self.Bconst], [BKn], inc=(h == 7))
                ealb = eal.unsqueeze(2).broadcast_to([64, 8, 128])
                for hb in range(2):
                    hsl = slice(hb * 512, (hb + 1) * 512)
                    if n == 0:
                        self.cp("dve", Aa[:, hsl], bank[3 + hb][0:64, :], [Bbank[3 + hb]], [BA])
                    else:
                        self.tt("dve", Aa[:, hsl], bank[3 + hb][0:64, :], C32[:, hsl], ALU.add, [Bbank[3 + hb], BC32], [BA])
                self.tt("pool", v4(C32), v4(Aa), ealb, ALU.mult, [BA, self.Bmtab], [BC32])
                self.cp("act", Cbf, C32, [BC32], [BCbf])
                if n == 0:
                    self.cp("dve", An, pKn[0:64, 8:16], [BKn], [BAn])
                else:
                    self.tt("dve", An, pKn[0:64, 8:16], n32, ALU.add, [BKn, Bn32], [BAn])
                self.tt("dve", n32, An, eal, ALU.mult, [BAn, self.Bmtab], [Bn32])
                self.cp("dve", nbf, n32, [Bn32], [Bnbf])

        def epi(n):
            L_ = ld[n % NL]
            dm, rden, rstd = st8
            self.act(sqt, hv, AF.Square, [Bhv], [Bsqt])
            self.red(rstd, v4(sqt), ALU.add, [Bsqt], [Bst[2]])
            self.act(rstd, rstd, AF.Sqrt, [Bst[2]], [Bst[2]], scale=1.0 / 128.0, bias=1e-6)
            self.recip(rstd, rstd, [Bst[2]], [Bst[2]])
            self.tt("pool", v4(hv), v4(hv), self.bcast_h(rstd, 128), ALU.mult, [Bhv, Bst[2]], [Bhv])
            self.tt("dve", yb[n % 2], hv, L_["o"], ALU.mult, [Bhv, L_["Bo"]], [Byb[n % 2]])

        def outT(n):
            for h in range(8):
                self.tr(pq[:, h * 128:(h + 1) * 128], yb[n % 2][:, h * 128:(h + 1) * 128], self.identb[:], [Byb[n % 2], self.Bconst], [Bbank[2]], inc=(h == 7))
            ys = yTs[n % 2]
            self.cp("act", ys.rearrange("p h t -> p (h t)"), pq, [Bbank[2]], [ByTs[n % 2]])
            S.dma("sp", yTb[:, :, n * 128:(n + 1) * 128], ys, [ByTs[n % 2]], [self.ByT[1][n]], self.ds[2 + n % 2])

        loads(0)
        loads(1)
        front(0)
        yield
        for n in range(NT):
            if n + 2 < NT:
                loads(n + 2)
            mid(n)
            yield
            if n + 1 < NT:
                front(n + 1)
                yield
            epi(n)
            yield
            if n > 0:
                outT(n - 1)
                yield
        outT(NT - 1)
        yield

    def phase_moba(self, l):
        S = self.S
        self.arena_reset()
        bank, Bbank = self.bank, self.Bbank
        qT_all = self.alloc([128, 8, SEQ], BF16)
        kT_all = self.alloc([128, 8, SEQ], BF16)
        v_all = self.alloc([128, NT, 1024], BF16)
        sel_all = self.alloc([128, NT, 8, 8], F32)
        BqTa = [Buf() for _ in range(NT)]
        BkTa = [Buf() for _ in range(NT)]
        Bva = [Buf() for _ in range(NT)]
        Bsel = [Buf() for _ in range(NT)]
        ksum = self.alloc([128, 8, NT], F32)
        kmean = self.alloc([128, 8, 8], F32)
        Bksum, Bkmean = Buf(), Buf()
        nbT = self.alloc([128, SEQ], BF16)
        BnbT = [Buf() for _ in range(NT)]
        Esel = self.alloc([128, 64, 128], BF16)
        BEsel = Buf()
        mark = self.aoff
        NL = 3
        ld = []
        for b in range(NL):
            ld.append(dict(q=self.alloc([128, 1024], F32), k=self.alloc([128, 1024], F32), Bq=Buf(), Bk=Buf()))
        nbt = [self.alloc([128, 64], F32) for _ in range(2)]
        Bnbt = self.bufs(2)
        self.cp("pool", Esel, self.identf[:, 0:64].unsqueeze(2).broadcast_to([128, 64, 128]), [self.Bconst], [BEsel])
        self.memset("pool", nbT, 0.0, [], BnbT)
        qT32 = [self.alloc([128, 1024], F32) for _ in range(2)]
        BqT32 = self.bufs(2)
        gm = self.alloc([128, 8, 8], F32)
        mx = self.alloc([128, 8, 8], F32)
        Bgm, Bmx = Buf(), Buf()
        zq, zk, zv = self.z["aq"], self.z["ak"], self.z["av"]
        Bz = self.Bz
        v4 = lambda ap: ap.rearrange("p (h d) -> p h d", h=8)
        self.memset("dve", kmean, 0.0, [], [Bkmean])
        self.memset("dve", sel_all, 0.0, [], Bsel)

        def loads(i):
            b = i % NL
            L_ = ld[b]
            rs = slice(i * 128, (i + 1) * 128)
            S.dma("sp", L_["q"], zq[rs, :], Bz["aq"][i], [L_["Bq"]], self.ds[0 + b])
            S.dma("sp", L_["k"], zk[rs, :], Bz["ak"][i], [L_["Bk"]], self.ds[3 + b])
            S.dma("sp", v_all[:, i, :], zv[rs, :], Bz["av"][i], [Bva[i]], self.ds[6 + b])

        loads(0)
        loads(1)
        for i in range(NT):
            if i + 2 < NT:
                loads(i + 2)
            L_ = ld[i % NL]
            qr, kr, Bqr, Bkr = L_["q"], L_["k"], L_["Bq"], L_["Bk"]
            q32 = qT32[i % 2]
            Bq32 = BqT32[i % 2]
            bq = i // 2
            for h in range(8):
                hb = h // 4
                self.tr(bank[hb][:, (h % 4) * 128:(h % 4 + 1) * 128], qr[:, h * 128:(h + 1) * 128], self.identf[:],
                        [Bqr, self.Bconst], [Bbank[hb]], inc=(h % 4 == 3))
            for h in range(8):
                hb = 2 + h // 4
                self.tr(bank[hb][:, (h % 4) * 128:(h % 4 + 1) * 128], kr[:, h * 128:(h + 1) * 128], self.identf[:],
                        [Bkr, self.Bconst], [Bbank[hb]], inc=(h % 4 == 3))
            for hb in range(2):
                self.cp("act", q32[:, hb * 512:(hb + 1) * 512], bank[hb][:], [Bbank[hb]], [Bq32])
                self.cp("dve", kT_all[:, hb * 4:(hb + 1) * 4, i * 128:(i + 1) * 128],
                        bank[2 + hb][:].rearrange("p (h t) -> p h t", h=4), [Bbank[2 + hb]], [BkTa[i]])
            self.cp("pool", qT_all[:, :, i * 128:(i + 1) * 128], v4(q32), [Bq32], [BqTa[i]])
            pks = bank[4 + (i % 2) * 2]
            Bpks = Bbank[4 + (i % 2) * 2]
            for h in range(8):
                self.mm(pks[:, h:h + 1], kr[:, h * 128:(h + 1) * 128], self.kmcol[:, 0:1], True, True, [Bkr, self.Bconst], [Bpks], inc=(h == 7))
            self.cp("act", ksum[:, :, i], pks[:, 0:8], [Bpks], [Bksum])
            if bq >= 1:
                pg = bank[5 + (i % 2) * 2]
                Bpg = Bbank[5 + (i % 2) * 2]
                for h in range(8):
                    self.mm(pg[:, h * 8:(h + 1) * 8], q32[:, h * 128:(h + 1) * 128], kmean[:, h, :], True, True,
                            [Bq32, Bkmean], [Bpg], inc=(h == 7))
                self.tt("dve", gm, pg[:, 0:64].rearrange("p (h n) -> p h n", h=8),
                        self.negm[:, bq, :].unsqueeze(1).broadcast_to([128, 8, 8]), ALU.add, [Bpg, self.Bconst], [Bgm])
                for h in range(8):
                    self.S.op("dve", lambda e, h=h: e.max(out=mx[:, h, :], in_=gm[:, h, :]), [Bgm], [Bmx])
                self.tt("dve", gm, gm, mx[:, :, 2:3].broadcast_to([128, 8, 8]), ALU.is_ge, [Bgm, Bmx], [Bgm])
                self.tt("dve", gm, gm, self.selfix[:, 0, bq, :].unsqueeze(1).broadcast_to([128, 8, 8]), ALU.mult, [Bgm, self.Bconst], [Bgm])
                self.tt("dve", sel_all[:, i], gm, self.selfix[:, 1, bq, :].unsqueeze(1).broadcast_to([128, 8, 8]), ALU.add,
                        [Bgm, self.Bconst], [Bsel[i]])
            else:
                self.cp("dve", sel_all[:, i], self.selfix[:, 1, 0:1, :].broadcast_to([128, 8, 8]), [self.Bconst], [Bsel[i]])
            if i % 2 == 1:
                self.tt("dve", kmean[:, :, bq], ksum[:, :, i - 1], ksum[:, :, i], ALU.add, [Bksum], [Bkmean])
            self.ts("dve", nbt[i % 2], sel_all[:, i].rearrange("p h n -> p (h n)"), -1.0, 30000.0, ALU.add, ALU.mult, [Bsel[i]], [Bnbt[i % 2]])
            self.tr(pks[0:64, 128:256], nbt[i % 2], self.identf[:], [Bnbt[i % 2], self.Bconst], [Bpks], inc=True)
            self.cp("act", nbT[0:64, i * 128:(i + 1) * 128], pks[0:64, 128:256], [Bpks], [BnbT[i]])
        S.barrier()
        self.aoff = mark
        NPT = 4
        LA = 2
        PT = [self.alloc([128, 512], BF16) for _ in range(NPT)]
        BPT = self.bufs(NPT)
        rd = [self.alloc([128, 512], F32) for _ in range(2)]
        Brd = self.bufs(2)
        dacc = [self.alloc([128, 512], F32) for _ in range(2)]
        Bdacc = self.bufs(2)
        yst = [self.alloc([128, 512], BF16) for _ in range(2)]
        Byst = self.bufs(2)
        items = []
        for h in range(8):
            for Q in range(4):
                for jt in range(4 * Q + 4):
                    items.append((h, Q, jt))

        def geom(Q, jt):
            c0 = 128 * max(0, jt - 4 * Q)
            return c0, 512 - c0, 4 * Q * 128 + c0, (4 * Q + 4) * 128

        def qk(idx):
            h, Q, jt = items[idx]
            n = jt // 2
            c0, N, qlo, qhi = geom(Q, jt)
            need_bias = n <= 2 * Q
            pS = bank[idx % 4]
            BpS = Bbank[idx % 4]
            pt = PT[idx % NPT]
            Bpt = BPT[idx % NPT]
            qdeps = [BqTa[x] for x in range(qlo // 128, qhi // 128)]
            self.mm(pS[:, 0:N], kT_all[:, h, jt * 128:(jt + 1) * 128], qT_all[:, h, qlo:qhi], True, not need_bias,
                    [BkTa[jt]] + qdeps, [BpS], inc=not need_bias)
            if need_bias:
                self.mm(pS[:, 0:N], Esel[:, h * 8 + n, :], nbT[:, qlo:qhi], False, True,
                        [BEsel] + [BnbT[x] for x in range(qlo // 128, qhi // 128)], [BpS], inc=True)
            self.act(pt[:, 0:N], pS[:, 0:N], AF.Exp, [BpS], [Bpt])
            if jt >= 4 * Q:
                self.tt("pool", pt[:, 0:128], pt[:, 0:128], self.maskb[:], ALU.mult, [Bpt, self.Bconst], [Bpt])

        def pv(idx):
            h, Q, jt = items[idx]
            gi = h * 4 + Q
            g2 = gi % 2
            hs = slice(h * 128, (h + 1) * 128)
            c0, N, qlo, qhi = geom(Q, jt)
            ntile = 4 * Q + 4
            pO, BpO = bank[4 + g2], Bbank[4 + g2]
            pDn, BpDn = bank[6 + g2], Bbank[6 + g2]
            pt = PT[idx % NPT]
            Bpt = BPT[idx % NPT]
            self.mm(pO[:, c0:512], v_all[:, jt, hs], pt[:, 0:N], jt == 0, jt == ntile - 1, [Bva[jt], Bpt], [BpO], inc=False)
            self.mm(pDn[:, c0:512], self.onesb[:], pt[:, 0:N], jt == 0, jt == ntile - 1, [self.Bconst, Bpt], [BpDn], inc=True)
            if jt == ntile - 1:
                self.recip(rd[g2], pDn[:], [BpDn], [Brd[g2]])
                self.tt("dve", yst[g2], pO[:], rd[g2], ALU.mult, [BpO, BpDn, Brd[g2]], [Byst[g2]])
                S.dma("sp", self.yT[2][h * 128:(h + 1) * 128, Q * 512:(Q + 1) * 512], yst[g2], [Byst[g2]],
                      [self.ByT[2][h * 4 + Q]], self.ds[12 + g2])

        for idx in range(len(items) + LA):
            if idx < len(items):
                qk(idx)
            if idx >= LA:
                pv(idx - LA)

    def phase_merge(self, l, xsrc):
        S = self.S
        self.arena_reset()
        bank, Bbank = self.bank, self.Bbank
        mT = self.alloc([128, 16, SEQ], BF16)
        BmT = [[Buf() for _ in range(4)] for _ in range(16)]
        yTb = [self.alloc([128, 8, SEQ], BF16) for _ in range(2)]
        ByTb = [self.bufs(4) for _ in range(2)]
        Wp = [self.alloc([128, 8, 128], BF16) for _ in range(4)]
        BWp = self.bufs(4)
        sgt = [self.alloc([128, 512], BF16) for _ in range(4)]
        Bsgt = self.bufs(4)
        prod = [self.alloc([128, 512], F32) for _ in range(2)]
        Bprod = self.bufs(2)
        Wo = [self.alloc([128, 16, 128], BF16) for _ in range(3)]
        BWo = self.bufs(3)
        xt = [self.alloc([128, 512], F32) for _ in range(4)]
        Bxt = self.bufs(4)
        ost = [self.alloc([128, 512], F32) for _ in range(4)]
        Bost = self.bufs(4)
        wps = [self.w_pa, self.w_pb, self.w_pc]

        def loady(b):
            yv = self.yT[b].rearrange("(c p) t -> p c t", p=128)
            for tb in range(4):
                if b < 2:
                    rd = [self.ByT[b][4 * tb + x] for x in range(4)]
                else:
                    rd = [self.ByT[2][h * 4 + tb] for h in range(8)]
                S.dma("sp", yTb[b % 2][:, :, tb * 512:(tb + 1) * 512], yv[:, :, tb * 512:(tb + 1) * 512], rd, [ByTb[b % 2][tb]], self.ds[0 + (b % 2) * 4 + tb])

        items = [(b, ncx, tb) for b in range(3) for ncx in range(16) for tb in range(4)]

        def loadw(b, ncx):
            wv = wps[b][l].rearrange("(c p) n -> p c n", p=128)
            s_ = (b * 16 + ncx) % 4
            S.dma("pool", Wp[s_], wv[:, :, ncx * 128:(ncx + 1) * 128], [], [BWp[s_]], self.ds[8 + s_])

        def loadsg(ii):
            b, ncx, tb = items[ii]
            k = ii % 4
            S.dma("sp", sgt[k], self.sgT[(b * 16 + ncx) * 128:(b * 16 + ncx + 1) * 128, tb * 512:(tb + 1) * 512],
                  [self.Bsg[b * 16 + ncx][tb]], [Bsgt[k]], self.ds[12 + k])

        wlist = [(b, ncx) for b in range(3) for ncx in range(16)]
        loady(0)
        loady(1)
        loadw(*wlist[0])
        loadw(*wlist[1])
        loadsg(0)
        loadsg(1)
        for ii, (b, ncx, tb) in enumerate(items):
            wi = b * 16 + ncx
            if tb == 0 and wi + 2 < len(wlist):
                loadw(*wlist[wi + 2])
            if ii + 2 < len(items):
                loadsg(ii + 2)
            if b == 1 and ncx == 0 and tb == 0:
                loady(2)
            s_ = wi % 4
            k = ii % 4
            ps = bank[k]
            Bps = Bbank[k]
            for c in range(8):
                self.mm(ps[:], Wp[s_][:, c, :], yTb[b % 2][:, c, tb * 512:(tb + 1) * 512], c == 0, c == 7,
                        [BWp[s_], ByTb[b % 2][tb]], [Bps], inc=(c == 7))
            msl = mT[:, ncx, tb * 512:(tb + 1) * 512]
            if b == 0:
                self.tt("dve", msl, ps[:], sgt[k], ALU.mult, [Bps, Bsgt[k]], [BmT[ncx][tb]])
            else:
                pr = prod[k % 2]
                self.tt("dve", pr, ps[:], sgt[k], ALU.mult, [Bps, Bsgt[k]], [Bprod[k % 2]])
                self.tt("pool", msl, pr, msl, ALU.add, [Bprod[k % 2], BmT[ncx][tb]], [BmT[ncx][tb]])
        wv = self.w_out[l].rearrange("(c p) n -> p c n", p=128)

        def loadwo(dcx):
            s_ = dcx % 3
            S.dma("pool", Wo[s_], wv[:, :, dcx * 128:(dcx + 1) * 128], [], [BWo[s_]], self.ds[16 + s_])

        oitems = [(dcx, tb) for dcx in range(16) for tb in range(4)]

        def loadx(ii):
            dcx, tb = oitems[ii]
            k = ii % 4
            S.dma("sp", xt[k], xsrc[dcx * 128:(dcx + 1) * 128, tb * 512:(tb + 1) * 512], [self.Bx[dcx][tb]], [Bxt[k]], self.ds[19 + k])

        loadwo(0)
        loadwo(1)
        loadx(0)
        loadx(1)
        for ii, (dcx, tb) in enumerate(oitems):
            if tb == 0 and dcx + 2 < 16:
                loadwo(dcx + 2)
            if ii + 2 < len(oitems):
                loadx(ii + 2)
            s_ = dcx % 3
            k = ii % 4
            ps = bank[4 + k]
            Bps = Bbank[4 + k]
            for c in range(16):
                self.mm(ps[:], Wo[s_][:, c, :], mT[:, c, tb * 512:(tb + 1) * 512], c == 0, c == 15,
                        [BWo[s_], BmT[c][tb]], [Bps], inc=(c == 15))
            self.tt("dve", ost[k], ps[:], xt[k], ALU.add, [Bps, Bxt[k]], [Bost[k]])
            S.dma("sp", self.xres[dcx * 128:(dcx + 1) * 128, tb * 512:(tb + 1) * 512], ost[k], [Bost[k]], [self.Bx[dcx][tb]], self.ds[23 + k])

    def phase_ffn_up(self, l):
        S = self.S
        hT, BhT = self.hT, self.BhT
        bank, Bbank = self.bank, self.Bbank
        NW = 3
        Wa = [self.alloc([128, 16, 128], BF16) for _ in range(NW)]
        Wb = [self.alloc([128, 16, 128], BF16) for _ in range(NW)]
        BWa, BWb = self.bufs(NW), self.bufs(NW)
        ua = [self.alloc([128, 2 + SEQ], F32) for _ in range(2)]
        ub = [self.alloc([128, 2 + SEQ], F32) for _ in range(2)]
        Bua = [[Buf() for _ in range(5)] for _ in range(2)]
        Bub = [[Buf() for _ in range(5)] for _ in range(2)]
        ta = [self.alloc([128, 512], F32) for _ in range(2)]
        tb_ = [self.alloc([128, 512], F32) for _ in range(2)]
        Bta, Btb = self.bufs(2), self.bufs(2)
        sl_ = [self.alloc([128, 512], F32) for _ in range(2)]
        Bsl = self.bufs(2)
        ast = [self.alloc([128, SEQ], BF16) for _ in range(2)]
        Bast = self.bufs(2)
        wv = self.w_up[l].rearrange("(c p) n -> p c n", p=128)
        for s_ in range(2):
            self.memset("dve", ua[s_][:, 0:2], 0.0, [], [Bua[s_][4]])
            self.memset("dve", ub[s_][:, 0:2], 0.0, [], [Bub[s_][4]])

        def loadw(fc):
            s = fc % NW
            S.dma("pool", Wa[s], wv[:, :, fc * 128:(fc + 1) * 128], [], [BWa[s]], self.ds[0 + s])
            S.dma("pool", Wb[s], wv[:, :, D_FF + fc * 128:D_FF + (fc + 1) * 128], [], [BWb[s]], self.ds[3 + s])
        loadw(0)
        loadw(1)
        cnt = 0
        cw, cb = self.convw, self.convb
        for fc in range(NFC):
            if fc + 2 < NFC:
                loadw(fc + 2)
            s = fc % NW
            u = fc % 2
            for tb in range(4):
                for (W_, BW_, uu, Buu, tt_, Btt, ch, pb) in ((Wa[s], BWa[s], ua[u], Bua[u], ta, Bta, fc, 0),
                                                            (Wb[s], BWb[s], ub[u], Bub[u], tb_, Btb, NFC + fc, 1)):
                    k = cnt % 4
                    cnt += 1
                    ps = bank[k]
                    Bps = Bbank[k]
                    for c in range(16):
                        self.mm(ps[:], W_[:, c, :], hT[:, c, tb * 512:(tb + 1) * 512], c == 0, c == 15,
                                [BW_, BhT[c][2 * tb], BhT[c][2 * tb + 1]], [Bps], inc=(c == 15))
                    self.cp("act", uu[:, 2 + tb * 512:2 + (tb + 1) * 512], ps[:], [Bps], [Buu[tb]])
                    prev = [Buu[tb - 1]] if tb > 0 else [Buu[4]]
                    t_ = tt_[tb % 2]
                    Bt_ = Btt[tb % 2]
                    self.ts("dve", t_, uu[:, 2 + tb * 512:2 + (tb + 1) * 512], cw[:, ch, 2:3], cb[:, ch:ch + 1], ALU.mult, ALU.add,
                            [Buu[tb], self.Blayer], [Bt_])
                    self.stt(t_, uu[:, 1 + tb * 512:1 + (tb + 1) * 512], cw[:, ch, 1:2], t_, ALU.mult, ALU.add,
                             [Buu[tb], self.Blayer, Bt_] + prev, [Bt_])
                    self.stt(t_, uu[:, tb * 512:(tb + 1) * 512], cw[:, ch, 0:1], t_, ALU.mult, ALU.add,
                             [Buu[tb], self.Blayer, Bt_] + prev, [Bt_])
                sx = sl_[tb % 2]
                self.act(sx, ta[tb % 2], AF.Silu, [Bta[tb % 2]], [Bsl[tb % 2]])
                self.tt("pool", ast[u][:, tb * 512:(tb + 1) * 512], sx, tb_[tb % 2], ALU.mult, [Bsl[tb % 2], Btb[tb % 2]], [Bast[u]])
            S.dma("sp", self.actT[fc * 128:(fc + 1) * 128, :], ast[u], [Bast[u]], self.Bact[fc], self.ds[6 + u])

    def phase_ffn_down(self, l, xdst):
        S = self.S
        self.arena_reset()
        bank, Bbank = self.bank, self.Bbank
        NSB = 11
        Wd = [self.alloc([128, NFC, 512], BF16) for _ in range(2)]
        BWd = [self.bufs(4) for _ in range(2)]
        At = self.alloc([128, NFC, 512], BF16)
        BAt = self.bufs(NSB)
        xt = [self.alloc([128, 512], F32) for _ in range(4)]
        Bxt = self.bufs(4)
        ost = [self.alloc([128, 512], F32) for _ in range(4)]
        Bost = self.bufs(4)
        wv = self.w_down[l].rearrange("(c p) n -> p c n", p=128)
        av = self.actT.rearrange("(c p) t -> p c t", p=128)

        def loadw(dg):
            for sblk in range(4):
                cs = slice(sblk * 11, (sblk + 1) * 11)
                S.dma("pool", Wd[dg % 2][:, cs, :], wv[:, cs, dg * 512:(dg + 1) * 512], [], [BWd[dg % 2][sblk]], self.ds[0 + (dg % 2) * 4 + sblk])

        its = [(dg, tb) for dg in range(4) for tb in range(4)]

        def loada(ii):
            dg, tb = its[ii]
            for sblk in range(NSB):
                cs = slice(sblk * 4, (sblk + 1) * 4)
                S.dma("sp", At[:, cs, :], av[:, cs, tb * 512:(tb + 1) * 512],
                      [self.Bact[c][tb] for c in range(sblk * 4, (sblk + 1) * 4)], [BAt[sblk]], self.ds[8 + sblk])

        loadw(0)
        loada(0)
        cnt = 0
        for ii, (dg, tb) in enumerate(its):
            if tb == 0 and dg + 1 < 4:
                loadw(dg + 1)
            for dcl in range(4):
                dcx = dg * 4 + dcl
                k = cnt % 4
                cnt += 1
                ps = bank[k]
                Bps = Bbank[k]
                S.dma("sp", xt[k], self.xres[dcx * 128:(dcx + 1) * 128, tb * 512:(tb + 1) * 512], [self.Bx[dcx][tb]], [Bxt[k]], self.ds[19 + k])
                for c in range(NFC):
                    self.mm(ps[:], Wd[dg % 2][:, c, dcl * 128:(dcl + 1) * 128], At[:, c, :], c == 0, c == NFC - 1,
                            [BWd[dg % 2][c // 11], BAt[c // 4]], [Bps], inc=(c == NFC - 1 or (dcl == 3 and c % 4 == 3)))
                if dcl == 3 and ii + 1 < len(its):
                    loada(ii + 1)
                self.tt("dve", ost[k], ps[:], xt[k], ALU.add, [Bps, Bxt[k]], [Bost[k]])
                S.dma("sp", xdst[dcx * 128:(dcx + 1) * 128, tb * 512:(tb + 1) * 512], ost[k], [Bost[k]], [self.Bx[dcx][tb]], self.ds[23 + k])


def _const_inputs():
    f32 = np.float32
    pos = np.arange(SEQ, dtype=f32)
    ret_freq = (1.0 / (10000.0 ** np.linspace(0.0, 1.0, 64, dtype=f32))).astype(f32)
    rope_freq = (1.0 / (10000.0 ** (np.arange(0, 128, 2, dtype=f32) / f32(128)))).astype(f32)
    tabs = np.zeros((128, 4, 16, 64), f32)
    for ti, fr in ((0, ret_freq), (2, rope_freq)):
        ang = (pos[:, None] * fr[None, :]).astype(f32)
        c = np.cos(ang).astype(f32).reshape(16, 128, 64).transpose(1, 0, 2)
        s = np.sin(ang).astype(f32).reshape(16, 128, 64).transpose(1, 0, 2)
        tabs[:, ti] = c
        tabs[:, ti + 1] = s
    h = np.arange(8, dtype=np.float64)
    log_gamma = np.log1p(-np.exp2(-5.0 - h))
    p = np.arange(128, dtype=np.float64)
    small = np.zeros((128, 24), f32)
    small[:, 0:8] = np.exp((p[:, None] + 1.0) * log_gamma[None, :])
    small[:, 8:16] = (128.0 ** -0.5) * np.exp(-(p[:, None] + 1.0) * log_gamma[None, :])
    small[:, 16:24] = np.exp(128.0 * log_gamma)[None, :]
    j = np.arange(128)
    mask = (j[None, :] >= j[:, None]).astype(f32)
    ident = np.eye(128, dtype=f32)
    negm = np.zeros((128, 8, 8), f32)
    selfix = np.zeros((128, 2, 8, 8), f32)
    for bq in range(8):
        for n in range(8):
            negm[:, bq, n] = 0.0 if n < bq else -1e30
            selfix[:, 0, bq, n] = 1.0 if n < bq else 0.0
            selfix[:, 1, bq, n] = 1.0 if n == bq else 0.0
    return dict(c_tabs=tabs, c_small=small, c_mask=mask, c_ident=ident, c_negm=negm, c_selfix=selfix)


def _layout_inputs(inputs):
    f32 = np.float32
    A = lambda k: np.ascontiguousarray(np.asarray(inputs[k], dtype=f32))
    shared = {}
    for k in ("w_in", "w_pa", "w_pb", "w_pc", "w_out", "w_up", "w_down"):
        shared[k] = A(k)
    shared["norm_mix_t"] = np.ascontiguousarray(A("norm_mix").reshape(DEPTH, 16, 128).transpose(0, 2, 1))
    shared["norm_ffn_t"] = np.ascontiguousarray(A("norm_ffn").reshape(DEPTH, 16, 128).transpose(0, 2, 1))
    shared["b_ig_t"] = A("b_ig").reshape(DEPTH, 8, 1)
    shared["b_fg_t"] = A("b_fg").reshape(DEPTH, 8, 1)
    shared["ret_gn_rep"] = np.ascontiguousarray(np.broadcast_to(A("ret_gn")[:, None, :], (DEPTH, 128, 1024)))
    shared["ml_norm_rep"] = np.ascontiguousarray(np.broadcast_to(A("ml_norm")[:, None, :], (DEPTH, 128, 1024)))
    shared["q_norm_rep"] = np.ascontiguousarray(np.broadcast_to(A("q_norm")[:, None, :], (DEPTH, 128, 128)))
    shared["k_norm_rep"] = np.ascontiguousarray(np.broadcast_to(A("k_norm")[:, None, :], (DEPTH, 128, 128)))
    cw = A("conv_w")
    shared["conv_w_t"] = np.ascontiguousarray(cw.reshape(DEPTH, 3, 88, 128).transpose(0, 3, 2, 1))
    shared["conv_b_t"] = np.ascontiguousarray(A("conv_b").reshape(DEPTH, 88, 128).transpose(0, 2, 1))
    shared.update(_const_inputs())
    return shared


_NC_CACHE = {}


def _get_nc(n_layers=DEPTH, dbg=False, stop_after=None):
    key = (n_layers, dbg, stop_after)
    if key not in _NC_CACHE:
        kb = KB(n_layers, dbg)
        kb.stop_after = stop_after
        _NC_CACHE[key] = (kb.build(), kb)
    return _NC_CACHE[key][0]


def kernel(**inputs):
    x = np.asarray(inputs["x"], dtype=np.float32)
    B = x.shape[0]
    shared = _layout_inputs(inputs)
    nc = _get_nc()
    in_maps = []
    for b in range(B):
        m = dict(shared)
        m["xT"] = np.ascontiguousarray(x[b].T)
        in_maps.append(m)
    res = run_bass_kernel_spmd(nc, in_maps, core_ids=list(range(B)))
    out = np.stack([np.ascontiguousarray(np.asarray(r["outT"]).T) for r in res.results], axis=0)
    return out.astype(np.float32)
```

```python
import math
from contextlib import ExitStack

import numpy as np
import concourse.bass as bass
import concourse.mybir as mybir
from concourse.bass_utils import run_bass_kernel_spmd

F32 = mybir.dt.float32
BF16 = mybir.dt.bfloat16
AF = mybir.ActivationFunctionType
ALU = mybir.AluOpType
AX = mybir.AxisListType

ENGS = ["pe", "act", "dve", "pool", "sp"]

DEPTH = 4
SEQ = 2048
DM = 2048
NT = 16
N_IN = 16400
D_FF = 5632
NFC = 44
C_RQ, C_RK, C_RV, C_RG = 0, 1024, 2048, 3072
C_MQ, C_MK, C_MV, C_MO, C_MI, C_MF = 4096, 4608, 5120, 6144, 7168, 7176
C_AQ, C_AK, C_AV = 7184, 8208, 9232
C_GA = 10256


class Buf:
    __slots__ = ("name", "last_w", "readers")

    def __init__(self, name=""):
        self.name = name
        self.last_w = None
        self.readers = {}


class DSem:
    __slots__ = ("name", "count")

    def __init__(self, name):
        self.name = name
        self.count = 0


class Sched:
    def __init__(self, nc):
        self.nc = nc
        self.streams = {e: [] for e in ENGS}
        self.cnt = {e: 0 for e in ENGS}
        self.waited = {e: {} for e in ENGS}
        self.semnames = list(ENGS)
        self.dsems = []

    def dsem(self):
        d = DSem("d%d" % len(self.dsems))
        self.dsems.append(d)
        self.semnames.append(d.name)
        return d

    def _collect(self, eng, reads, writes):
        deps = {}
        for b in reads:
            t = b.last_w
            if t is not None and deps.get(t[0], 0) < t[1]:
                deps[t[0]] = t[1]
        for b in writes:
            t = b.last_w
            if t is not None and deps.get(t[0], 0) < t[1]:
                deps[t[0]] = t[1]
            for s, v in b.readers.items():
                if deps.get(s, 0) < v:
                    deps[s] = v
        waits = []
        w = self.waited[eng]
        for s, v in deps.items():
            if s == "pe" and eng == "pe":
                continue
            if w.get(s, 0) >= v:
                continue
            w[s] = v
            waits.append((s, v))
        return waits

    def _commit(self, tok, reads, writes):
        s, v = tok
        for b in reads:
            if b.readers.get(s, 0) < v:
                b.readers[s] = v
        for b in writes:
            b.last_w = tok
            b.readers = {}

    def op(self, eng, fn, reads=(), writes=(), inc=True):
        waits = self._collect(eng, reads, writes)
        if inc:
            self.cnt[eng] += 1
            tok = (eng, self.cnt[eng])
        else:
            tok = (eng, self.cnt[eng] + 1)
        self._commit(tok, reads, writes)
        self.streams[eng].append((fn, waits, eng if inc else None, 1))

    def dma(self, q, out, in_, reads, writes, dsem):
        waits = self._collect(q, reads, writes)
        dsem.count += 16
        tok = (dsem.name, dsem.count)
        self._commit(tok, reads, writes)
        self.streams[q].append(
            (lambda e, out=out, in_=in_: e.dma_start(out=out, in_=in_), waits, dsem.name, 16))

    def barrier(self):
        cur = {e: self.cnt[e] for e in ENGS if self.cnt[e] > 0}
        for d in self.dsems:
            if d.count > 0:
                cur[d.name] = d.count
        for e in ENGS:
            w = self.waited[e]
            waits = []
            for s, v in cur.items():
                if s == e and e == "pe":
                    w[s] = v
                    continue
                if w.get(s, 0) >= v:
                    continue
                w[s] = v
                waits.append((s, v))
            if waits:
                self.streams[e].append((None, waits, None, 0))

    def emit(self, stack):
        nc = self.nc
        sems = {}
        for n in self.semnames:
            sems[n] = stack.enter_context(nc.semaphore("s_" + n))
        block = stack.enter_context(nc.Block())
        handles = {"pe": block.tensor, "act": block.scalar, "dve": block.vector,
                   "pool": block.gpsimd, "sp": block.sync}
        for e in ENGS:
            stream = self.streams[e]

            def body(eng, stream=stream):
                for fn, waits, incsem, incv in stream:
                    for s, v in waits:
                        eng.wait_ge(sems[s], v)
                    if fn is None:
                        continue
                    ins = fn(eng)
                    if incsem is not None:
                        ins.then_inc(sems[incsem], incv)
            handles[e](body)


def _dtsize(dt):
    return 2 if dt == BF16 else 4


class KB:
    def __init__(self, n_layers=DEPTH, dbg=False, wdepth=DEPTH):
        self.n_layers = n_layers
        self.wdepth = wdepth
        self.dbg = dbg
        self.nc = bass.Bass("TRN2", target_bir_lowering=False)
        self.S = Sched(self.nc)

    def tt(self, eng, out, in0, in1, op, R, W):
        self.S.op(eng, lambda e: e.tensor_tensor(out=out, in0=in0, in1=in1, op=op), R, W)

    def ts(self, eng, out, in0, s1, s2, op0, op1, R, W):
        if s2 is None:
            self.S.op(eng, lambda e: e.tensor_scalar(out=out, in0=in0, scalar1=s1, scalar2=None, op0=op0), R, W)
        else:
            self.S.op(eng, lambda e: e.tensor_scalar(out=out, in0=in0, scalar1=s1, scalar2=s2, op0=op0, op1=op1), R, W)

    def stt(self, out, in0, scalar, in1, op0, op1, R, W):
        self.S.op("dve", lambda e: e.scalar_tensor_tensor(out=out, in0=in0, scalar=scalar, in1=in1, op0=op0, op1=op1), R, W)

    def act(self, out, in_, func, R, W, scale=1.0, bias=0.0):
        self.S.op("act", lambda e: e.activation(out=out, in_=in_, func=func, bias=bias, scale=scale), R, W)

    def cp(self, eng, out, in_, R, W):
        if eng == "act":
            self.S.op("act", lambda e: e.activation(out=out, in_=in_, func=AF.Copy), R, W)
        else:
            self.S.op(eng, lambda e: e.tensor_copy(out=out, in_=in_), R, W)

    def mm(self, out, lhsT, rhs, start, stop, R, W, inc):
        self.S.op("pe", lambda e: e.matmul(out, lhsT=lhsT, rhs=rhs, start=start, stop=stop), R, W, inc=inc)

    def tr(self, out, in_, ident, R, W, inc):
        self.S.op("pe", lambda e: e.transpose(out=out, in_=in_, identity=ident), R, W, inc=inc)

    def red(self, out, in_, op, R, W):
        self.S.op("dve", lambda e: e.tensor_reduce(out=out, in_=in_, axis=AX.X, op=op), R, W)

    def recip(self, out, in_, R, W):
        self.S.op("dve", lambda e: e.reciprocal(out=out, in_=in_), R, W)

    def memset(self, eng, ap, val, R, W):
        self.S.op(eng, lambda e: e.memset(ap, val), R, W)

    def arena_reset(self):
        self.aoff = 0

    def alloc(self, shape, dt):
        nel = 1
        for s in shape[1:]:
            nel *= s
        nbytes = nel * _dtsize(dt)
        nbytes = (nbytes + 31) // 32 * 32
        off = self.aoff
        self.aoff += nbytes
        assert self.aoff <= self.arena_bytes, ("arena overflow", self.aoff, self.arena_bytes)
        w0 = off // 4
        ap = self.arena[0:shape[0], w0:w0 + nbytes // 4]
        if dt != F32:
            ap = ap.bitcast(dt)
        ap = ap[:, 0:nel]
        if len(shape) == 3:
            ap = ap.rearrange("p (a b) -> p a b", a=shape[1])
        elif len(shape) == 4:
            ap = ap.rearrange("p (a b c) -> p a b c", a=shape[1], b=shape[2])
        return ap

    def bufs(self, n):
        return [Buf() for _ in range(n)]

    def build(self):
        nc = self.nc
        S = self.S
        dbg = self.dbg
        L = self.n_layers
        self.stack = ExitStack()
        st = self.stack

        def din(name, shape, dt=F32):
            return nc.dram_tensor(name, list(shape), dt, kind="ExternalInput").ap()

        def dscr(name, shape, dt=F32):
            kind = "ExternalOutput" if dbg else "Internal"
            return nc.dram_tensor(name, list(shape), dt, kind=kind).ap()

        self.xT = din("xT", [DM, SEQ])
        self.w_in = din("w_in", [self.wdepth, DM, N_IN])
        self.w_pa = din("w_pa", [self.wdepth, 1024, DM])
        self.w_pb = din("w_pb", [self.wdepth, 1024, DM])
        self.w_pc = din("w_pc", [self.wdepth, 1024, DM])
        self.w_out = din("w_out", [self.wdepth, DM, DM])
        self.w_up = din("w_up", [self.wdepth, DM, 2 * D_FF])
        self.w_down = din("w_down", [self.wdepth, D_FF, DM])
        self.i_norm_mix = din("norm_mix_t", [self.wdepth, 128, 16])
        self.i_norm_ffn = din("norm_ffn_t", [self.wdepth, 128, 16])
        self.i_big = din("b_ig_t", [self.wdepth, 8, 1])
        self.i_bfg = din("b_fg_t", [self.wdepth, 8, 1])
        self.i_retgn = din("ret_gn_rep", [self.wdepth, 128, 1024])
        self.i_mlnorm = din("ml_norm_rep", [self.wdepth, 128, 1024])
        self.i_qnorm = din("q_norm_rep", [self.wdepth, 128, 128])
        self.i_knorm = din("k_norm_rep", [self.wdepth, 128, 128])
        self.i_convw = din("conv_w_t", [self.wdepth, 128, 88, 3])
        self.i_convb = din("conv_b_t", [self.wdepth, 128, 88])
        self.i_tabs = din("c_tabs", [128, 4, 16, 64])
        self.i_small = din("c_small", [128, 24])
        self.i_mask = din("c_mask", [128, 128])
        self.i_ident = din("c_ident", [128, 128])
        self.i_negm = din("c_negm", [128, 8, 8])
        self.i_selfix = din("c_selfix", [128, 2, 8, 8])
        self.outT = nc.dram_tensor("outT", [DM, SEQ], F32, kind="ExternalOutput").ap()
        self.xres = dscr("xres", [DM, SEQ])
        self.z = {}
        for nm, w in (("rq", 1024), ("rk", 1024), ("rv", 1024), ("rg", 1024), ("mq", 512), ("mk", 512),
                      ("mv", 1024), ("mo", 1024), ("aq", 1024), ("ak", 1024), ("av", 1024)):
            self.z[nm] = dscr("z_" + nm, [SEQ, w], F32 if nm in ("aq", "ak") else BF16)
        self.sgT = dscr("sgT", [3 * DM, SEQ], BF16)
        self.yT = [dscr("yT%d" % b, [1024, SEQ], BF16) for b in range(3)]
        self.actT = dscr("actT", [D_FF, SEQ], BF16)
        self.Bx = [[Buf() for _ in range(4)] for _ in range(16)]
        self.Bz = {nm: [[Buf() for _ in range(2)] for _ in range(NT)] for nm in self.z}
        self.Bsg = [[Buf() for _ in range(4)] for _ in range(48)]
        self.ByT = [[Buf() for _ in range(32)] for _ in range(3)]
        self.Bact = [[Buf() for _ in range(4)] for _ in range(NFC)]

        def sb(name, shape, dt):
            return st.enter_context(nc.sbuf_tensor(name, list(shape), dt))

        self.tabs = sb("tabs", [128, 4, 16, 64], F32)
        self.small = sb("small", [128, 24], F32)
        self.maskf = sb("maskf", [128, 128], F32)
        self.maskb = sb("maskb", [128, 128], BF16)
        self.identf = sb("identf", [128, 128], F32)
        self.identb = sb("identb", [128, 128], BF16)
        self.onesf = sb("onesf", [128, 128], F32)
        self.onescb = sb("onescb", [128, 2], BF16)
        self.onesb = sb("onesb", [128, 128], BF16)
        self.kmcol = sb("kmcol", [128, 2], F32)
        self.negm = sb("negm", [128, 8, 8], F32)
        self.selfix = sb("selfix", [128, 2, 8, 8], F32)
        self.gmix = sb("gmix", [128, 16], F32)
        self.gffn = sb("gffn", [128, 16], F32)
        self.big = sb("big", [8, 1], F32)
        self.bfg = sb("bfg", [8, 1], F32)
        self.retgn = sb("retgn", [128, 1024], F32)
        self.mlnorm = sb("mlnorm", [128, 1024], F32)
        self.qnw = sb("qnw", [128, 128], F32)
        self.knw = sb("knw", [128, 128], F32)
        self.convw = sb("convw", [128, 88, 3], F32)
        self.convb = sb("convb", [128, 88], F32)
        self.mtab = sb("mtab", [128, 3, 16, 8], F32)
        self.Bconst = Buf()
        self.Blayer = Buf()
        self.Bmtab = Buf()
        self.arena_bytes = (nc.sbuf_bytes_remaining // 32) * 32 - 64
        self.arena = sb("arena", [128, self.arena_bytes // 4], F32)
        self.bank = [st.enter_context(nc.psum_tensor("bank%d" % i, [128, 512], F32)) for i in range(8)]
        self.Bbank = [Buf() for _ in range(8)]
        self.ds = [S.dsem() for _ in range(40)]
        self.dconst = S.dsem()

        dc = self.dconst
        S.dma("sp", self.tabs[:], self.i_tabs[:, :, :, :], [], [self.Bconst], dc)
        S.dma("sp", self.small[:], self.i_small[:, :], [], [self.Bconst], dc)
        S.dma("sp", self.maskf[:], self.i_mask[:, :], [], [self.Bconst], dc)
        S.dma("sp", self.identf[:], self.i_ident[:, :], [], [self.Bconst], dc)
        S.dma("sp", self.negm[:], self.i_negm[:, :, :], [], [self.Bconst], dc)
        S.dma("sp", self.selfix[:], self.i_selfix[:, :, :, :], [], [self.Bconst], dc)
        S.barrier()
        self.cp("dve", self.maskb[:], self.maskf[:], [self.Bconst], [self.Bconst])
        self.cp("dve", self.identb[:], self.identf[:], [self.Bconst], [self.Bconst])
        self.memset("dve", self.onesf[:], 1.0, [], [self.Bconst])
        self.memset("dve", self.onescb[:], 1.0, [], [self.Bconst])
        self.memset("dve", self.onesb[:], 1.0, [], [self.Bconst])
        self.memset("dve", self.kmcol[:], 1.0 / 256.0, [], [self.Bconst])
        S.barrier()

        for l in range(L):
            self.layer(l)

        S.barrier()
        S.emit(st)
        return nc

    def layer(self, l):
        S = self.S
        dc = self.dconst
        xsrc = self.xT if l == 0 else self.xres
        xdst_final = self.outT if l == self.n_layers - 1 else self.xres
        for dst, src in ((self.gmix, self.i_norm_mix[l]), (self.gffn, self.i_norm_ffn[l]),
                         (self.big, self.i_big[l]), (self.bfg, self.i_bfg[l]),
                         (self.retgn, self.i_retgn[l]), (self.mlnorm, self.i_mlnorm[l]),
                         (self.qnw, self.i_qnorm[l]), (self.knw, self.i_knorm[l]),
                         (self.convw, self.i_convw[l]), (self.convb, self.i_convb[l])):
            S.dma("sp", dst[:], src, [], [self.Blayer], dc)
        S.barrier()
        self.ts("dve", self.qnw[:], self.qnw[:], 128.0 ** -0.5, None, ALU.mult, None, [self.Blayer], [self.Blayer])
        S.barrier()

        self.phase_norm(xsrc, self.gmix)
        self.phase_proj(l)
        S.barrier()
        self.phase_moba(l)
        S.barrier()
        if self.stop_after == "C":
            return
        self.phase_merge(l, xsrc)
        S.barrier()
        if self.stop_after == "G":
            return
        self.phase_norm(self.xres, self.gffn)
        self.phase_ffn_up(l)
        S.barrier()
        if self.stop_after == "F1":
            return
        self.phase_ffn_down(l, xdst_final)
        S.barrier()

    stop_after = None

    def phase_norm(self, xsrc, g):
        S = self.S
        self.arena_reset()
        self.hT = self.alloc([128, 16, SEQ], BF16)
        self.BhT = [[Buf() for _ in range(8)] for _ in range(16)]
        mark = self.aoff
        xb = [self.alloc([128, 16, 256], F32) for _ in range(2)]
        Bxb = self.bufs(2)
        sq = [self.alloc([128, 256], F32) for _ in range(4)]
        Bsq = self.bufs(4)
        sd = [self.alloc([128, 256], F32) for _ in range(2)]
        Bsd = self.bufs(2)
        xv = xsrc.rearrange("(c p) t -> p c t", p=128)
        for t in range(8):
            b = t % 2
            S.dma("sp", xb[b], xv[:, :, t * 256:(t + 1) * 256], [self.Bx[c][t // 2] for c in range(16)], [Bxb[b]], self.ds[b])
            ps = self.bank[b][:, 0:256]
            Bps = self.Bbank[b]
            for c in range(16):
                k = c % 4
                self.act(sq[k], xb[b][:, c, :], AF.Square, [Bxb[b]], [Bsq[k]])
                self.mm(ps, self.onesf[:], sq[k], c == 0, c == 15, [Bsq[k], self.Bconst], [Bps], inc=True)
            self.act(sd[b], ps, AF.Sqrt, [Bps], [Bsd[b]], scale=1.0 / DM, bias=1e-6)
            self.recip(sd[b], sd[b], [Bsd[b]], [Bsd[b]])
            for c in range(16):
                self.stt(self.hT[:, c, t * 256:(t + 1) * 256], xb[b][:, c, :], g[:, c:c + 1], sd[b],
                         ALU.mult, ALU.mult, [Bxb[b], Bsd[b], self.Blayer], [self.BhT[c][t]])
        self.aoff = mark
        S.barrier()

    def rot4(self, eng, dst, src, cos, sin, tmp, Btmp, R, Bsrc, Bdst, nh):
        sv = src.rearrange("p (h two d) -> p h two d", h=nh, two=2)
        dv = dst.rearrange("p (h two d) -> p h two d", h=nh, two=2)
        x1 = sv[:, :, 0, :]
        x2 = sv[:, :, 1, :]
        cb = cos.unsqueeze(1).broadcast_to([128, nh, 64])
        sbb = sin.unsqueeze(1).broadcast_to([128, nh, 64])
        t1v = tmp[0].rearrange("p (h d) -> p h d", h=nh)
        t2v = tmp[1].rearrange("p (h d) -> p h d", h=nh)
        self.tt(eng, t1v, x1, cb, ALU.mult, [Bsrc] + R, [Btmp[0]])
        self.tt(eng, t2v, x2, sbb, ALU.mult, [Bsrc] + R, [Btmp[1]])
        self.tt(eng, dv[:, :, 0, :], t1v, t2v, ALU.subtract, [Btmp[0], Btmp[1]], [Bdst])
        self.tt(eng, t1v, x1, sbb, ALU.mult, [Bsrc] + R, [Btmp[0]])
        self.tt(eng, t2v, x2, cb, ALU.mult, [Bsrc] + R, [Btmp[1]])
        self.tt(eng, dv[:, :, 1, :], t1v, t2v, ALU.add, [Btmp[0], Btmp[1]], [Bdst])

    def phase_proj(self, l):
        S = self.S
        hT = self.hT
        BhT = self.BhT
        bank, Bbank = self.bank, self.Bbank
        NW = 2
        Wt = [self.alloc([128, 16, 512], BF16) for _ in range(NW)]
        BW = self.bufs(NW)
        NR = 4
        stg = [self.alloc([128, 512], F32) for _ in range(NR)]
        Bstg = self.bufs(NR)
        stgb = [self.alloc([128, 512], BF16) for _ in range(NR)]
        Bstgb = self.bufs(NR)
        rt = {e: [self.alloc([128, 256], F32) for _ in range(2)] for e in ("dve", "pool")}
        Brt = {e: self.bufs(2) for e in ("dve", "pool")}
        r4 = [self.alloc([128, 4], F32) for _ in range(NR)]
        Br4 = self.bufs(NR)
        wv = self.w_in[l].rearrange("(c p) n -> p c n", p=128)
        self.pcnt = 0
        Wg = self.alloc([128, 16, 16], BF16)
        BWg = Buf()
        S.dma("pool", Wg, wv[:, :, C_MI:C_MI + 16], [], [BWg], self.ds[6])
        r8 = [self.alloc([128, 4], F32) for _ in range(8)]
        Br8 = self.bufs(8)
        mixer_base = self.aoff
        stg2 = stg3 = stg4 = None
        Bstg2 = self.bufs(8)
        Bstg3 = self.bufs(8)
        Bstg4 = self.bufs(8)
        A = self.alloc([8, SEQ], F32)
        Bm = self.alloc([8, SEQ], F32)
        Cc = self.alloc([8, SEQ], F32)
        Dd = self.alloc([8, SEQ], F32)
        BA, BB, BC, BD = self.bufs(4)
        blocks = []
        seg_of = []
        order = (("rq", C_RQ, 1024, 0), ("rk", C_RK, 1024, 0), ("rv", C_RV, 1024, 0), ("rg", C_RG, 1024, 0),
                 ("mq", C_MQ, 512, 1), ("mk", C_MK, 512, 1), ("mv", C_MV, 1024, 1), ("mo", C_MO, 1024, 1),
                 ("av", C_AV, 1024, 2))
        for nm, c0, w, seg in order:
            for j in range(w // 512):
                blocks.append(("tm", nm, c0 + j * 512, j))
                seg_of.append(seg)
        for gb in range(12):
            blocks.append(("fm", None, C_GA + gb * 512, gb))
            seg_of.append(2)
        for nm, c0, w, seg in (("aq", C_AQ, 1024, 3), ("ak", C_AK, 1024, 3)):
            for j in range(w // 512):
                blocks.append(("tm", nm, c0 + j * 512, j))
                seg_of.append(seg)
        nblk = len(blocks)

        def load(bi):
            kind, nm, c0, j = blocks[bi]
            s_ = bi % NW
            S.dma("pool", Wt[s_], wv[:, :, c0:c0 + 512], [], [BW[s_]], self.ds[0 + s_])

        load(0)
        load(1)
        for which, dstT, Bd in ((0, A, BA), (1, Bm, BB)):
            for tb in range(4):
                k = self.pcnt % 2
                self.pcnt += 1
                ps = bank[k]
                Bps = Bbank[k]
                for c in range(16):
                    self.mm(ps[0:8, :], Wg[:, c, which * 8:(which + 1) * 8], hT[:, c, tb * 512:(tb + 1) * 512],
                            c == 0, c == 15, [BhT[c][2 * tb], BhT[c][2 * tb + 1], BWg], [Bps], inc=(c == 15))
                self.cp("act", dstT[:, tb * 512:(tb + 1) * 512], ps[0:8, :], [Bps], [Bd])
        self.ts("dve", A, A, self.big[:, 0:1], 1.0 / 15.0, ALU.add, ALU.mult, [BA, self.Blayer], [BA])
        self.act(A, A, AF.Tanh, [BA], [BA])
        self.ts("dve", Bm, Bm, self.bfg[:, 0:1], 1.0 / 15.0, ALU.add, ALU.mult, [BB, self.Blayer], [BB])
        self.act(Bm, Bm, AF.Tanh, [BB], [BB])
        self.act(Bm, Bm, AF.Exp, [BB], [BB], scale=-15.0)
        self.act(Bm, Bm, AF.Ln, [BB], [BB], bias=1.0)
        for n in range(NT):
            sl = slice(n * 128, (n + 1) * 128)
            self.S.op("dve", lambda e, sl=sl: e.tensor_tensor_scan(out=Cc[:, sl], data0=self.onesf[0:8, :], data1=Bm[:, sl],
                                                                  initial=0.0, op0=ALU.mult, op1=ALU.subtract),
                      [BB, self.Bconst], [BC])
        self.act(Dd, Cc, AF.Exp, [BC], [BD])
        self.stt(A, A, 15.0, Cc, ALU.mult, ALU.subtract, [BA, BC], [BA])
        self.act(A, A, AF.Exp, [BA], [BA], bias=math.log(0.125))
        cl = Cc.rearrange("p (n t) -> p n t", t=128)[:, :, 127:128].broadcast_to([8, NT, 128])
        self.act(Bm.rearrange("p (n t) -> p n t", t=128), cl, AF.Exp, [BC], [BB])
        k = self.pcnt % 2
        self.pcnt += 1
        ps = bank[k]
        Bps = Bbank[k]
        idx = 0
        for wi, (src, Bs) in enumerate(((Dd, BD), (A, BA), (Bm, BB))):
            for n in range(NT):
                idx += 1
                col = (wi * NT + n) * 8
                self.tr(ps[:, col:col + 8], src[:, n * 128:(n + 1) * 128], self.identf[0:8, 0:8],
                        [Bs, self.Bconst], [Bps], inc=(idx == 48))
        self.cp("act", self.mtab[:].rearrange("p a n h -> p (a n h)"), ps[:, 0:384], [Bps], [self.Bmtab])
        S.barrier()
        self.aoff = mixer_base


        cosr, sinr = self.tabs[:, 0], self.tabs[:, 1]
        cosp, sinp = self.tabs[:, 2], self.tabs[:, 3]
        sq_r = self.small[:, 0:8]
        sk_r = self.small[:, 8:16]

        def evac(nm, j, i, ps, Bps, k):
            eng = "dve" if i % 2 == 0 else "pool"
            dst = self.z[nm]
            rows = slice(i * 128, (i + 1) * 128)
            cols = slice(j * 512, (j + 1) * 512)
            if nm in ("rv", "mv", "av"):
                self.cp("act", stgb[k], ps[:], [Bps], [Bstgb[k]])
                out_t, Bout = stgb[k], Bstgb[k]
            elif nm in ("rq", "rk"):
                sc = sq_r if nm == "rq" else sk_r
                for hh in range(4):
                    h = j * 4 + hh
                    self.S.op("act", lambda e, hh=hh, h=h, sc=sc: e.activation(out=stg[k][:, hh * 128:(hh + 1) * 128],
                                                                             in_=ps[:, hh * 128:(hh + 1) * 128], func=AF.Copy,
                                                                             scale=sc[:, h:h + 1]),
                              [Bps, self.Bconst], [Bstg[k]])
                self.rot4(eng, stgb[k], stg[k], cosr[:, i, :], sinr[:, i, :], rt[eng], Brt[eng], [self.Bconst], Bstg[k], Bstgb[k], 4)
                out_t, Bout = stgb[k], Bstgb[k]
            elif nm in ("rg", "mo"):
                fn = AF.Silu if nm == "rg" else AF.Sigmoid
                gw = self.retgn if nm == "rg" else self.mlnorm
                self.act(stg[k], ps[:], fn, [Bps], [Bstg[k]])
                self.tt(eng, stgb[k], stg[k], gw[:, cols], ALU.mult, [Bstg[k], self.Blayer], [Bstgb[k]])
                out_t, Bout = stgb[k], Bstgb[k]
            elif nm in ("mq", "mk"):
                tab = self.mtab[:, 0 if nm == "mq" else 1, i, :]
                tb_ = tab.unsqueeze(2).broadcast_to([128, 8, 64])
                v8 = lambda ap: ap.rearrange("p (h d) -> p h d", h=8)
                if i % 2 == 0:
                    self.tt("dve", v8(stgb[k]), v8(ps[:]), tb_, ALU.mult, [Bps, self.Bmtab], [Bstgb[k]])
                else:
                    self.cp("act", stg[k], ps[:], [Bps], [Bstg[k]])
                    self.tt("pool", v8(stgb[k]), v8(stg[k]), tb_, ALU.mult, [Bstg[k], self.Bmtab], [Bstgb[k]])
                out_t, Bout = stgb[k], Bstgb[k]
            else:
                gw = self.qnw if nm == "aq" else self.knw
                v4_ = lambda ap: ap.rearrange("p (h d) -> p h d", h=4)
                k8 = (j * NT + i) % 8
                self.act(stg2[k8], ps[:], AF.Square, [Bps], [Bstg2[k8]])
                self.cp("act", stg3[k8], ps[:], [Bps], [Bstg3[k8]])
                self.red(r8[k8], v4_(stg2[k8]), ALU.add, [Bstg2[k8]], [Br8[k8]])
                self.act(r8[k8], r8[k8], AF.Sqrt, [Br8[k8]], [Br8[k8]], scale=1.0 / 128.0, bias=1e-6)
                self.recip(r8[k8], r8[k8], [Br8[k8]], [Br8[k8]])
                self.tt(eng, v4_(stg3[k8]), v4_(stg3[k8]), r8[k8].unsqueeze(2).broadcast_to([128, 4, 128]), ALU.mult,
                        [Bstg3[k8], Br8[k8]], [Bstg3[k8]])
                self.tt(eng, v4_(stg3[k8]), v4_(stg3[k8]), gw[:].unsqueeze(1).broadcast_to([128, 4, 128]), ALU.mult,
                        [Bstg3[k8], self.Blayer], [Bstg3[k8]])
                self.rot4(eng, stg4[k8], stg3[k8], cosp[:, i, :], sinp[:, i, :], rt[eng], Brt[eng], [self.Bconst], Bstg3[k8], Bstg4[k8], 4)
                out_t, Bout = stg4[k8], Bstg4[k8]
                S.dma("sp", dst[rows, cols], out_t, [Bout], [self.Bz[nm][i][j]], self.ds[28 + k8])
                return
            S.dma("sp", dst[rows, cols], out_t, [Bout], [self.Bz[nm][i][j]], self.ds[28 + k])

        gen = None
        cur_seg = 0
        stepno = 0
        stride = {0: 1, 1: 1, 2: 2, 3: 1}

        def step(force=False):
            nonlocal gen, stepno
            stepno += 1
            if gen is not None and (force or stepno % stride[cur_seg] == 0):
                try:
                    next(gen)
                except StopIteration:
                    gen = None

        def drain():
            nonlocal gen
            while gen is not None:
                step(True)

        for bi, (kind, nm, c0, j) in enumerate(blocks):
            if seg_of[bi] != cur_seg:
                drain()
                cur_seg = seg_of[bi]
                S.barrier()
                self.aoff = mixer_base
                if cur_seg in (1, 2):
                    gen = self.ret_gen(l) if cur_seg == 1 else self.mlstm_gen(l)
                else:
                    stg2 = [self.alloc([128, 512], F32) for _ in range(8)]
                    stg3 = [self.alloc([128, 512], F32) for _ in range(8)]
                    stg4 = [self.alloc([128, 512], F32) for _ in range(8)]
            if bi + 1 < nblk and bi >= 1:
                load(bi + 1)
            s_ = bi % NW
            if kind == "tm":
                for i in range(NT):
                    k = self.pcnt % (4 if cur_seg in (0, 3) else 2)
                    self.pcnt += 1
                    ps = bank[k]
                    Bps = Bbank[k]
                    for c in range(16):
                        self.mm(ps[:], hT[:, c, i * 128:(i + 1) * 128], Wt[s_][:, c, :], c == 0, c == 15,
                                [BhT[c][i // 2], BW[s_]], [Bps], inc=(c == 15))
                    evac(nm, j, i, ps, Bps, k)
                    step()
            else:
                for nn in range(4):
                    for tb in range(4):
                        k = self.pcnt % 2
                        self.pcnt += 1
                        ps = bank[k]
                        Bps = Bbank[k]
                        for c in range(16):
                            self.mm(ps[:], Wt[s_][:, c, nn * 128:(nn + 1) * 128], hT[:, c, tb * 512:(tb + 1) * 512],
                                    c == 0, c == 15, [BhT[c][2 * tb], BhT[c][2 * tb + 1], BW[s_]], [Bps], inc=(c == 15))
                        self.act(stgb[k], ps[:], AF.Sigmoid, [Bps], [Bstgb[k]])
                        row = (j * 4 + nn) * 128
                        S.dma("sp", self.sgT[row:row + 128, tb * 512:(tb + 1) * 512], stgb[k], [Bstgb[k]],
                              [self.Bsg[j * 4 + nn][tb]], self.ds[32 + k])
                        step()
        drain()

    def bcast_h(self, ap8, d):
        return ap8.unsqueeze(2).broadcast_to([ap8.shape[0], 8, d])

    def ret_gen(self, l):
        S = self.S
        bank, Bbank = self.bank, self.Bbank
        NL = 3
        ld = []
        for b in range(NL):
            ld.append(dict(q=self.alloc([128, 1024], BF16), k=self.alloc([128, 1024], BF16),
                           g=self.alloc([128, 1024], BF16), v=self.alloc([128, 1024], BF16),
                           Bq=Buf(), Bk=Buf(), Bg=Buf(), Bv=Buf()))
        qT = [self.alloc([128, 1024], BF16) for _ in range(2)]
        kT = [self.alloc([128, 1024], BF16) for _ in range(2)]
        BqT, BkT = self.bufs(2), self.bufs(2)
        sTm = [self.alloc([128, 1024], BF16) for _ in range(2)]
        BsTm = [self.bufs(2) for _ in range(2)]
        R32 = self.alloc([128, 1024], F32)
        Rbf = self.alloc([128, 1024], BF16)
        Aa = self.alloc([128, 1024], F32)
        BR32, BRbf, BA = Buf(), Buf(), Buf()
        osb = self.alloc([128, 1024], F32)
        sqt = self.alloc([128, 1024], F32)
        cc = sqt
        Bosb, Bsqt, Bcc = self.bufs(3)
        yb = [self.alloc([128, 1024], BF16) for _ in range(2)]
        Byb = self.bufs(2)
        yTs = [self.alloc([128, 8, 128], BF16) for _ in range(2)]
        ByTs = self.bufs(2)
        st8 = [self.alloc([128, 8], F32) for _ in range(6)]
        Bst = self.bufs(6)
        gC = self.small[:, 16:24]
        zq, zk, zv, zg = self.z["rq"], self.z["rk"], self.z["rv"], self.z["rg"]
        Bz = self.Bz
        yTa = self.yT[0].rearrange("(h e) t -> e h t", e=128)
        v4 = lambda ap: ap.rearrange("p (h d) -> p h d", h=8)
        maskb4 = self.maskb[:].unsqueeze(1).broadcast_to([128, 4, 128])
        pq = bank[2][:].bitcast(BF16)
        pk = bank[3][:].bitcast(BF16)

        def loads(n):
            b = n % NL
            L_ = ld[b]
            rs = slice(n * 128, (n + 1) * 128)
            S.dma("sp", L_["q"], zq[rs, :], Bz["rq"][n], [L_["Bq"]], self.ds[4 + b])
            S.dma("sp", L_["k"], zk[rs, :], Bz["rk"][n], [L_["Bk"]], self.ds[7 + b])
            S.dma("sp", L_["g"], zg[rs, :], Bz["rg"][n], [L_["Bg"]], self.ds[10 + b])
            S.dma("sp", L_["v"], zv[rs, :], Bz["rv"][n], [L_["Bv"]], self.ds[13 + b])

        def front(n):
            L_ = ld[n % NL]
            s_ = n % 2
            for h in range(8):
                self.tr(pq[:, h * 128:(h + 1) * 128], L_["q"][:, h * 128:(h + 1) * 128], self.identb[:], [L_["Bq"], self.Bconst], [Bbank[2]], inc=(h == 7))
            for h in range(8):
                self.tr(pk[:, h * 128:(h + 1) * 128], L_["k"][:, h * 128:(h + 1) * 128], self.identb[:], [L_["Bk"], self.Bconst], [Bbank[3]], inc=(h == 7))
            self.cp("act", qT[s_], pq, [Bbank[2]], [BqT[s_]])
            self.cp("dve", kT[s_], pk, [Bbank[3]], [BkT[s_]])
            for h in range(8):
                hb = 4 + h // 4
                self.mm(bank[hb][:, (h % 4) * 128:(h % 4 + 1) * 128], kT[s_][:, h * 128:(h + 1) * 128], qT[s_][:, h * 128:(h + 1) * 128],
                        True, True, [BkT[s_], BqT[s_]], [Bbank[hb]], inc=(h % 4 == 3))
            for hb in range(2):
                self.tt("dve", sTm[s_][:, hb * 512:(hb + 1) * 512].rearrange("p (h d) -> p h d", h=4),
                        bank[4 + hb][:].rearrange("p (h d) -> p h d", h=4), maskb4, ALU.mult,
                        [Bbank[4 + hb], self.Bconst], [BsTm[s_][hb]])

        def mid(n):
            L_ = ld[n % NL]
            s_ = n % 2
            for h in range(8):
                hb = 6 + h // 4
                osl = bank[hb][:, (h % 4) * 128:(h % 4 + 1) * 128]
                hs = slice(h * 128, (h + 1) * 128)
                self.mm(osl, sTm[s_][:, hs], L_["v"][:, hs], True, n == 0, [BsTm[s_][h // 4], L_["Bv"]], [Bbank[hb]],
                        inc=(n == 0 and h % 4 == 3))
                if n > 0:
                    self.mm(osl, qT[s_][:, hs], Rbf[:, hs], False, True, [BqT[s_], BRbf], [Bbank[hb]], inc=(h % 4 == 3))
            if n + 1 < NT:
                for h in range(8):
                    hb = 4 + h // 4
                    hs = slice(h * 128, (h + 1) * 128)
                    self.mm(bank[hb][:, (h % 4) * 128:(h % 4 + 1) * 128], L_["k"][:, hs], L_["v"][:, hs], True, True,
                            [L_["Bk"], L_["Bv"]], [Bbank[hb]], inc=(h % 4 == 3))
                for hb in range(2):
                    hsl = slice(hb * 512, (hb + 1) * 512)
                    if n == 0:
                        self.cp("dve", Aa[:, hsl], bank[4 + hb][:], [Bbank[4 + hb]], [BA])
                    else:
                        self.tt("dve", Aa[:, hsl], bank[4 + hb][:], R32[:, hsl], ALU.add, [Bbank[4 + hb], BR32], [BA])
                self.tt("pool", v4(R32), v4(Aa), self.bcast_h(gC, 128), ALU.mult, [BA, self.Bconst], [BR32])
                self.cp("act", Rbf, R32, [BR32], [BRbf])

        def epi(n):
            L_ = ld[n % NL]
            for hb in range(2):
                self.cp("act", osb[:, hb * 512:(hb + 1) * 512], bank[6 + hb][:], [Bbank[6 + hb]], [Bosb])
            self.act(sqt, osb, AF.Square, [Bosb], [Bsqt])
            s1, s2, mean, msq, var, rstd = st8
            self.red(s1, v4(osb), ALU.add, [Bosb], [Bst[0]])
            self.red(s2, v4(sqt), ALU.add, [Bsqt], [Bst[1]])
            self.ts("dve", mean, s1, 1.0 / 128.0, None, ALU.mult, None, [Bst[0]], [Bst[2]])
            self.tt("dve", msq, mean, mean, ALU.mult, [Bst[2]], [Bst[3]])
            self.stt(var, s2, 1.0 / 128.0, msq, ALU.mult, ALU.subtract, [Bst[1], Bst[3]], [Bst[4]])
            self.act(rstd, var, AF.Sqrt, [Bst[4]], [Bst[5]], bias=1e-5)
            self.recip(rstd, rstd, [Bst[5]], [Bst[5]])
            self.tt("pool", v4(cc), v4(osb), self.bcast_h(mean, 128), ALU.subtract, [Bosb, Bst[2]], [Bcc])
            self.tt("pool", v4(cc), v4(cc), self.bcast_h(rstd, 128), ALU.mult, [Bcc, Bst[5]], [Bcc])
            self.tt("dve", yb[n % 2], cc, L_["g"], ALU.mult, [Bcc, L_["Bg"]], [Byb[n % 2]])

        def outT(n):
            for h in range(8):
                self.tr(pq[:, h * 128:(h + 1) * 128], yb[n % 2][:, h * 128:(h + 1) * 128], self.identb[:], [Byb[n % 2], self.Bconst], [Bbank[2]], inc=(h == 7))
            ys = yTs[n % 2]
            self.cp("act", ys.rearrange("p h t -> p (h t)"), pq, [Bbank[2]], [ByTs[n % 2]])
            S.dma("sp", yTa[:, :, n * 128:(n + 1) * 128], ys, [ByTs[n % 2]], [self.ByT[0][n]], self.ds[16 + n % 2])

        loads(0)
        loads(1)
        front(0)
        yield
        for n in range(NT):
            if n + 2 < NT:
                loads(n + 2)
            mid(n)
            yield
            if n + 1 < NT:
                front(n + 1)
                yield
            epi(n)
            yield
            if n > 0:
                outT(n - 1)
                yield
        outT(NT - 1)
        yield

    def mlstm_gen(self, l):
        S = self.S
        bank, Bbank = self.bank, self.Bbank
        NL = 3
        ld = []
        for b in range(NL):
            ld.append(dict(q=self.alloc([128, 512], BF16), k=self.alloc([128, 512], BF16),
                           o=self.alloc([128, 1024], BF16), v=self.alloc([128, 1024], BF16),
                           Bq=Buf(), Bk=Buf(), Bo=Buf(), Bv=Buf()))
        qT = [self.alloc([64, 1024], BF16) for _ in range(2)]
        kT = [self.alloc([64, 1024], BF16) for _ in range(2)]
        BqT, BkT = self.bufs(2), self.bufs(2)
        sTm = [self.alloc([128, 1024], BF16) for _ in range(2)]
        BsTm = [self.bufs(2) for _ in range(2)]
        C32 = self.alloc([64, 1024], F32)
        Cbf = self.alloc([64, 1024], BF16)
        Aa = self.alloc([64, 1024], F32)
        n32 = self.alloc([64, 8], F32)
        nbf = self.alloc([64, 8], BF16)
        An = self.alloc([64, 8], F32)
        BC32, BCbf, BA, Bn32, Bnbf, BAn = self.bufs(6)
        hv = self.alloc([128, 1024], F32)
        sqt = self.alloc([128, 1024], F32)
        Bhv, Bsqt = self.bufs(2)
        yb = [self.alloc([128, 1024], BF16) for _ in range(2)]
        Byb = self.bufs(2)
        yTs = [self.alloc([128, 8, 128], BF16) for _ in range(2)]
        ByTs = self.bufs(2)
        st8 = [self.alloc([128, 8], F32) for _ in range(3)]
        Bst = self.bufs(3)
        zq, zk, zv, zo = self.z["mq"], self.z["mk"], self.z["mv"], self.z["mo"]
        Bz = self.Bz
        yTb = self.yT[1].rearrange("(h e) t -> e h t", e=128)
        v4 = lambda ap: ap.rearrange("p (h d) -> p h d", h=8)
        maskb4 = self.maskb[:].unsqueeze(1).broadcast_to([128, 4, 128])
        mtab = self.mtab
        pq = bank[2][:].bitcast(BF16)
        pk = pq
        pD = bank[7]
        BKn = Buf()

        def loads(n):
            b = n % NL
            L_ = ld[b]
            rs = slice(n * 128, (n + 1) * 128)
            S.dma("sp", L_["q"], zq[rs, :], [Bz["mq"][n][0]], [L_["Bq"]], self.ds[36 + b])
            S.dma("sp", L_["k"], zk[rs, :], [Bz["mk"][n][0]], [L_["Bk"]], self.ds[18 + b])
            S.dma("sp", L_["o"], zo[rs, :], Bz["mo"][n], [L_["Bo"]], self.ds[21 + b])
            S.dma("sp", L_["v"], zv[rs, :], Bz["mv"][n], [L_["Bv"]], self.ds[24 + b])

        def front(n):
            L_ = ld[n % NL]
            s_ = n % 2
            for h in range(8):
                self.tr(pq[0:64, h * 128:(h + 1) * 128], L_["q"][:, h * 64:(h + 1) * 64], self.identb[:], [L_["Bq"], self.Bconst], [Bbank[2]], inc=(h == 7))
            self.cp("act", qT[s_], pq[0:64, :], [Bbank[2]], [BqT[s_]])
            for h in range(8):
                self.tr(pk[0:64, h * 128:(h + 1) * 128], L_["k"][:, h * 64:(h + 1) * 64], self.identb[:], [L_["Bk"], self.Bconst], [Bbank[2]], inc=(h == 7))
            self.cp("dve", kT[s_], pk[0:64, :], [Bbank[2]], [BkT[s_]])
            for h in range(8):
                hb = 3 + h // 4
                hs = slice(h * 128, (h + 1) * 128)
                self.mm(bank[hb][:, (h % 4) * 128:(h % 4 + 1) * 128], kT[s_][:, hs], qT[s_][:, hs], True, True, [BkT[s_], BqT[s_]], [Bbank[hb]], inc=(h % 4 == 3))
            for hb in range(2):
                self.tt("dve", sTm[s_][:, hb * 512:(hb + 1) * 512].rearrange("p (h d) -> p h d", h=4),
                        bank[3 + hb][:].rearrange("p (h d) -> p h d", h=4), maskb4, ALU.mult,
                        [Bbank[3 + hb], self.Bconst], [BsTm[s_][hb]])

        def mid(n):
            L_ = ld[n % NL]
            s_ = n % 2
            eal = mtab[0:64, 2, n, :]
            for h in range(8):
                hb = 5 + h // 4
                osl = bank[hb][:, (h % 4) * 128:(h % 4 + 1) * 128]
                hs = slice(h * 128, (h + 1) * 128)
                self.mm(osl, sTm[s_][:, hs], L_["v"][:, hs], True, n == 0, [BsTm[s_][h // 4], L_["Bv"]], [Bbank[hb]], inc=False)
                if n > 0:
                    self.mm(osl, qT[s_][:, hs], Cbf[:, hs], False, True, [BqT[s_], BCbf], [Bbank[hb]], inc=False)
                self.mm(pD[:, h:h + 1], sTm[s_][:, hs], self.onescb[:, 0:1], True, n == 0, [BsTm[s_][h // 4], self.Bconst], [Bbank[7]],
                        inc=(n == 0 and h == 7))
                if n > 0:
                    self.mm(pD[:, h:h + 1], qT[s_][:, hs], nbf[:, h:h + 1], False, True, [BqT[s_], Bnbf], [Bbank[7]], inc=(h == 7))
            dm, rden, rstd = st8
            self.act(dm, pD[:, 0:8], AF.Abs, [Bbank[7]], [Bst[0]])
            self.ts("dve", dm, dm, 1.0, None, ALU.max, None, [Bst[0]], [Bst[0]])
            self.recip(rden, dm, [Bst[0]], [Bst[1]])
            for hb in range(2):
                self.tt("dve", hv[:, hb * 512:(hb + 1) * 512].rearrange("p (h d) -> p h d", h=4),
                        bank[5 + hb][:].rearrange("p (h d) -> p h d", h=4),
                        rden[:, hb * 4:(hb + 1) * 4].unsqueeze(2).broadcast_to([128, 4, 128]), ALU.mult,
                        [Bbank[5 + hb], Bbank[7], Bst[1]], [Bhv])
            if n + 1 < NT:
                pKn = bank[7]
                for h in range(8):
                    hb = 3 + h // 4
                    hs = slice(h * 128, (h + 1) * 128)
                    self.mm(bank[hb][0:64, (h % 4) * 128:(h % 4 + 1) * 128], L_["k"][:, h * 64:(h + 1) * 64], L_["v"][:, hs], True, True,
                            [L_["Bk"], L_["Bv"]], [Bbank[hb]], inc=(h % 4 == 3))
                for h in range(8):
                    self.mm(pKn[0:64, 8 + h:9 + h], L_["k"][:, h * 64:(h + 1) * 64], self.onescb[:, 0:1], True, True,
                            [L_["Bk"], self.Bconst], [BKn], inc=(h == 7))
                ealb = eal.unsqueeze(2).broadcast_to([64, 8, 128])
                for hb in range(2):
                    hsl = slice(hb * 512, (hb + 1) * 512)
                    if n == 0:
                        self.cp("dve", Aa[:, hsl], bank[3 + hb][0:64, :], [Bbank[3 + hb]], [BA])
                    else:
                        self.tt("dve", Aa[:, hsl], bank[3 + hb][0:64, :], C32[:, hsl], ALU.add, [Bbank[3 + hb], BC32], [BA])
                self.tt("pool", v4(C32), v4(Aa), ealb, ALU.mult, [BA, self.Bmtab], [BC32])
                self.cp("act", Cbf, C32, [BC32], [BCbf])
                if n == 0:
                    self.cp("dve", An, pKn[0:64, 8:16], [BKn], [BAn])
                else:
                    self.tt("dve", An, pKn[0:64, 8:16], n32, ALU.add, [BKn, Bn32], [BAn])
                self.tt("dve", n32, An, eal, ALU.mult, [BAn, self.Bmtab], [Bn32])
                self.cp("dve", nbf, n32, [Bn32], [Bnbf])

        def epi(n):
            L_ = ld[n % NL]
            dm, rden, rstd = st8
            self.act(sqt, hv, AF.Square, [Bhv], [Bsqt])
            self.red(rstd, v4(sqt), ALU.add, [Bsqt], [Bst[2]])
            self.act(rstd, rstd, AF.Sqrt, [Bst[2]], [Bst[2]], scale=1.0 / 128.0, bias=1e-6)
            self.recip(rstd, rstd, [Bst[2]], [Bst[2]])
            self.tt("pool", v4(hv), v4(hv), self.bcast_h(rstd, 128), ALU.mult, [Bhv, Bst[2]], [Bhv])
            self.tt("dve", yb[n % 2], hv, L_["o"], ALU.mult, [Bhv, L_["Bo"]], [Byb[n % 2]])

        def outT(n):
            for h in range(8):
                self.tr(pq[:, h * 128:(h + 1) * 128], yb[n % 2][:, h * 128:(h + 1) * 128], self.identb[:], [Byb[n % 2], self.Bconst], [Bbank[2]], inc=(h == 7))
            ys = yTs[n % 2]
            self.cp("act", ys.rearrange("p h t -> p (h t)"), pq, [Bbank[2]], [ByTs[n % 2]])
            S.dma("sp", yTb[:, :, n * 128:(n + 1) * 128], ys, [ByTs[n % 2]], [self.ByT[1][n]], self.ds[2 + n % 2])

        loads(0)
        loads(1)
        front(0)
        yield
        for n in range(NT):
            if n + 2 < NT:
                loads(n + 2)
            mid(n)
            yield
            if n + 1 < NT:
                front(n + 1)
                yield
            epi(n)
            yield
            if n > 0:
                outT(n - 1)
                yield
        outT(NT - 1)
        yield

    def phase_moba(self, l):
        S = self.S
        self.arena_reset()
        bank, Bbank = self.bank, self.Bbank
        qT_all = self.alloc([128, 8, SEQ], BF16)
        kT_all = self.alloc([128, 8, SEQ], BF16)
        v_all = self.alloc([128, NT, 1024], BF16)
        sel_all = self.alloc([128, NT, 8, 8], F32)
        BqTa = [Buf() for _ in range(NT)]
        BkTa = [Buf() for _ in range(NT)]
        Bva = [Buf() for _ in range(NT)]
        Bsel = [Buf() for _ in range(NT)]
        ksum = self.alloc([128, 8, NT], F32)
        kmean = self.alloc([128, 8, 8], F32)
        Bksum, Bkmean = Buf(), Buf()
        nbT = self.alloc([128, SEQ], BF16)
        BnbT = [Buf() for _ in range(NT)]
        Esel = self.alloc([128, 64, 128], BF16)
        BEsel = Buf()
        mark = self.aoff
        NL = 3
        ld = []
        for b in range(NL):
            ld.append(dict(q=self.alloc([128, 1024], F32), k=self.alloc([128, 1024], F32), Bq=Buf(), Bk=Buf()))
        nbt = [self.alloc([128, 64], F32) for _ in range(2)]
        Bnbt = self.bufs(2)
        self.cp("pool", Esel, self.identf[:, 0:64].unsqueeze(2).broadcast_to([128, 64, 128]), [self.Bconst], [BEsel])
        self.memset("pool", nbT, 0.0, [], BnbT)
        qT32 = [self.alloc([128, 1024], F32) for _ in range(2)]
        BqT32 = self.bufs(2)
        gm = self.alloc([128, 8, 8], F32)
        mx = self.alloc([128, 8, 8], F32)
        Bgm, Bmx = Buf(), Buf()
        zq, zk, zv = self.z["aq"], self.z["ak"], self.z["av"]
        Bz = self.Bz
        v4 = lambda ap: ap.rearrange("p (h d) -> p h d", h=8)
        self.memset("dve", kmean, 0.0, [], [Bkmean])
        self.memset("dve", sel_all, 0.0, [], Bsel)

        def loads(i):
            b = i % NL
            L_ = ld[b]
            rs = slice(i * 128, (i + 1) * 128)
            S.dma("sp", L_["q"], zq[rs, :], Bz["aq"][i], [L_["Bq"]], self.ds[0 + b])
            S.dma("sp", L_["k"], zk[rs, :], Bz["ak"][i], [L_["Bk"]], self.ds[3 + b])
            S.dma("sp", v_all[:, i, :], zv[rs, :], Bz["av"][i], [Bva[i]], self.ds[6 + b])

        loads(0)
        loads(1)
        for i in range(NT):
            if i + 2 < NT:
                loads(i + 2)
            L_ = ld[i % NL]
            qr, kr, Bqr, Bkr = L_["q"], L_["k"], L_["Bq"], L_["Bk"]
            q32 = qT32[i % 2]
            Bq32 = BqT32[i % 2]
            bq = i // 2
            for h in range(8):
                hb = h // 4
                self.tr(bank[hb][:, (h % 4) * 128:(h % 4 + 1) * 128], qr[:, h * 128:(h + 1) * 128], self.identf[:],
                        [Bqr, self.Bconst], [Bbank[hb]], inc=(h % 4 == 3))
            for h in range(8):
                hb = 2 + h // 4
                self.tr(bank[hb][:, (h % 4) * 128:(h % 4 + 1) * 128], kr[:, h * 128:(h + 1) * 128], self.identf[:],
                        [Bkr, self.Bconst], [Bbank[hb]], inc=(h % 4 == 3))
            for hb in range(2):
                self.cp("act", q32[:, hb * 512:(hb + 1) * 512], bank[hb][:], [Bbank[hb]], [Bq32])
                self.cp("dve", kT_all[:, hb * 4:(hb + 1) * 4, i * 128:(i + 1) * 128],
                        bank[2 + hb][:].rearrange("p (h t) -> p h t", h=4), [Bbank[2 + hb]], [BkTa[i]])
            self.cp("pool", qT_all[:, :, i * 128:(i + 1) * 128], v4(q32), [Bq32], [BqTa[i]])
            pks = bank[4 + (i % 2) * 2]
            Bpks = Bbank[4 + (i % 2) * 2]
            for h in range(8):
                self.mm(pks[:, h:h + 1], kr[:, h * 128:(h + 1) * 128], self.kmcol[:, 0:1], True, True, [Bkr, self.Bconst], [Bpks], inc=(h == 7))
            self.cp("act", ksum[:, :, i], pks[:, 0:8], [Bpks], [Bksum])
            if bq >= 1:
                pg = bank[5 + (i % 2) * 2]
                Bpg = Bbank[5 + (i % 2) * 2]
                for h in range(8):
                    self.mm(pg[:, h * 8:(h + 1) * 8], q32[:, h * 128:(h + 1) * 128], kmean[:, h, :], True, True,
                            [Bq32, Bkmean], [Bpg], inc=(h == 7))
                self.tt("dve", gm, pg[:, 0:64].rearrange("p (h n) -> p h n", h=8),
                        self.negm[:, bq, :].unsqueeze(1).broadcast_to([128, 8, 8]), ALU.add, [Bpg, self.Bconst], [Bgm])
                for h in range(8):
                    self.S.op("dve", lambda e, h=h: e.max(out=mx[:, h, :], in_=gm[:, h, :]), [Bgm], [Bmx])
                self.tt("dve", gm, gm, mx[:, :, 2:3].broadcast_to([128, 8, 8]), ALU.is_ge, [Bgm, Bmx], [Bgm])
                self.tt("dve", gm, gm, self.selfix[:, 0, bq, :].unsqueeze(1).broadcast_to([128, 8, 8]), ALU.mult, [Bgm, self.Bconst], [Bgm])
                self.tt("dve", sel_all[:, i], gm, self.selfix[:, 1, bq, :].unsqueeze(1).broadcast_to([128, 8, 8]), ALU.add,
                        [Bgm, self.Bconst], [Bsel[i]])
            else:
                self.cp("dve", sel_all[:, i], self.selfix[:, 1, 0:1, :].broadcast_to([128, 8, 8]), [self.Bconst], [Bsel[i]])
            if i % 2 == 1:
                self.tt("dve", kmean[:, :, bq], ksum[:, :, i - 1], ksum[:, :, i], ALU.add, [Bksum], [Bkmean])
            self.ts("dve", nbt[i % 2], sel_all[:, i].rearrange("p h n -> p (h n)"), -1.0, 30000.0, ALU.add, ALU.mult, [Bsel[i]], [Bnbt[i % 2]])
            self.tr(pks[0:64, 128:256], nbt[i % 2], self.identf[:], [Bnbt[i % 2], self.Bconst], [Bpks], inc=True)
            self.cp("act", nbT[0:64, i * 128:(i + 1) * 128], pks[0:64, 128:256], [Bpks], [BnbT[i]])
        S.barrier()
        self.aoff = mark
        NPT = 4
        LA = 2
        PT = [self.alloc([128, 512], BF16) for _ in range(NPT)]
        BPT = self.bufs(NPT)
        rd = [self.alloc([128, 512], F32) for _ in range(2)]
        Brd = self.bufs(2)
        dacc = [self.alloc([128, 512], F32) for _ in range(2)]
        Bdacc = self.bufs(2)
        yst = [self.alloc([128, 512], BF16) for _ in range(2)]
        Byst = self.bufs(2)
        items = []
        for h in range(8):
            for Q in range(4):
                for jt in range(4 * Q + 4):
                    items.append((h, Q, jt))

        def geom(Q, jt):
            c0 = 128 * max(0, jt - 4 * Q)
            return c0, 512 - c0, 4 * Q * 128 + c0, (4 * Q + 4) * 128

        def qk(idx):
            h, Q, jt = items[idx]
            n = jt // 2
            c0, N, qlo, qhi = geom(Q, jt)
            need_bias = n <= 2 * Q
            pS = bank[idx % 4]
            BpS = Bbank[idx % 4]
            pt = PT[idx % NPT]
            Bpt = BPT[idx % NPT]
            qdeps = [BqTa[x] for x in range(qlo // 128, qhi // 128)]
            self.mm(pS[:, 0:N], kT_all[:, h, jt * 128:(jt + 1) * 128], qT_all[:, h, qlo:qhi], True, not need_bias,
                    [BkTa[jt]] + qdeps, [BpS], inc=not need_bias)
            if need_bias:
                self.mm(pS[:, 0:N], Esel[:, h * 8 + n, :], nbT[:, qlo:qhi], False, True,
                        [BEsel] + [BnbT[x] for x in range(qlo // 128, qhi // 128)], [BpS], inc=True)
            self.act(pt[:, 0:N], pS[:, 0:N], AF.Exp, [BpS], [Bpt])
            if jt >= 4 * Q:
                self.tt("pool", pt[:, 0:128], pt[:, 0:128], self.maskb[:], ALU.mult, [Bpt, self.Bconst], [Bpt])

        def pv(idx):
            h, Q, jt = items[idx]
            gi = h * 4 + Q
            g2 = gi % 2
            hs = slice(h * 128, (h + 1) * 128)
            c0, N, qlo, qhi = geom(Q, jt)
            ntile = 4 * Q + 4
            pO, BpO = bank[4 + g2], Bbank[4 + g2]
            pDn, BpDn = bank[6 + g2], Bbank[6 + g2]
            pt = PT[idx % NPT]
            Bpt = BPT[idx % NPT]
            self.mm(pO[:, c0:512], v_all[:, jt, hs], pt[:, 0:N], jt == 0, jt == ntile - 1, [Bva[jt], Bpt], [BpO], inc=False)
            self.mm(pDn[:, c0:512], self.onesb[:], pt[:, 0:N], jt == 0, jt == ntile - 1, [self.Bconst, Bpt], [BpDn], inc=True)
            if jt == ntile - 1:
                self.recip(rd[g2], pDn[:], [BpDn], [Brd[g2]])
                self.tt("dve", yst[g2], pO[:], rd[g2], ALU.mult, [BpO, BpDn, Brd[g2]], [Byst[g2]])
                S.dma("sp", self.yT[2][h * 128:(h + 1) * 128, Q * 512:(Q + 1) * 512], yst[g2], [Byst[g2]],
                      [self.ByT[2][h * 4 + Q]], self.ds[12 + g2])

        for idx in range(len(items) + LA):
            if idx < len(items):
                qk(idx)
            if idx >= LA:
                pv(idx - LA)

    def phase_merge(self, l, xsrc):
        S = self.S
        self.arena_reset()
        bank, Bbank = self.bank, self.Bbank
        mT = self.alloc([128, 16, SEQ], BF16)
        BmT = [[Buf() for _ in range(4)] for _ in range(16)]
        yTb = [self.alloc([128, 8, SEQ], BF16) for _ in range(2)]
        ByTb = [self.bufs(4) for _ in range(2)]
        Wp = [self.alloc([128, 8, 128], BF16) for _ in range(4)]
        BWp = self.bufs(4)
        sgt = [self.alloc([128, 512], BF16) for _ in range(4)]
        Bsgt = self.bufs(4)
        prod = [self.alloc([128, 512], F32) for _ in range(2)]
        Bprod = self.bufs(2)
        Wo = [self.alloc([128, 16, 128], BF16) for _ in range(3)]
        BWo = self.bufs(3)
        xt = [self.alloc([128, 512], F32) for _ in range(4)]
        Bxt = self.bufs(4)
        ost = [self.alloc([128, 512], F32) for _ in range(4)]
        Bost = self.bufs(4)
        wps = [self.w_pa, self.w_pb, self.w_pc]

        def loady(b):
            yv = self.yT[b].rearrange("(c p) t -> p c t", p=128)
            for tb in range(4):
                if b < 2:
                    rd = [self.ByT[b][4 * tb + x] for x in range(4)]
                else:
                    rd = [self.ByT[2][h * 4 + tb] for h in range(8)]
                S.dma("sp", yTb[b % 2][:, :, tb * 512:(tb + 1) * 512], yv[:, :, tb * 512:(tb + 1) * 512], rd, [ByTb[b % 2][tb]], self.ds[0 + (b % 2) * 4 + tb])

        items = [(b, ncx, tb) for b in range(3) for ncx in range(16) for tb in range(4)]

        def loadw(b, ncx):
            wv = wps[b][l].rearrange("(c p) n -> p c n", p=128)
            s_ = (b * 16 + ncx) % 4
            S.dma("pool", Wp[s_], wv[:, :, ncx * 128:(ncx + 1) * 128], [], [BWp[s_]], self.ds[8 + s_])

        def loadsg(ii):
            b, ncx, tb = items[ii]
            k = ii % 4
            S.dma("sp", sgt[k], self.sgT[(b * 16 + ncx) * 128:(b * 16 + ncx + 1) * 128, tb * 512:(tb + 1) * 512],
                  [self.Bsg[b * 16 + ncx][tb]], [Bsgt[k]], self.ds[12 + k])

        wlist = [(b, ncx) for b in range(3) for ncx in range(16)]
        loady(0)
        loady(1)
        loadw(*wlist[0])
        loadw(*wlist[1])
        loadsg(0)
        loadsg(1)
        for ii, (b, ncx, tb) in enumerate(items):
            wi = b * 16 + ncx
            if tb == 0 and wi + 2 < len(wlist):
                loadw(*wlist[wi + 2])
            if ii + 2 < len(items):
                loadsg(ii + 2)
            if b == 1 and ncx == 0 and tb == 0:
                loady(2)
            s_ = wi % 4
            k = ii % 4
            ps = bank[k]
            Bps = Bbank[k]
            for c in range(8):
                self.mm(ps[:], Wp[s_][:, c, :], yTb[b % 2][:, c, tb * 512:(tb + 1) * 512], c == 0, c == 7,
                        [BWp[s_], ByTb[b % 2][tb]], [Bps], inc=(c == 7))
            msl = mT[:, ncx, tb * 512:(tb + 1) * 512]
            if b == 0:
                self.tt("dve", msl, ps[:], sgt[k], ALU.mult, [Bps, Bsgt[k]], [BmT[ncx][tb]])
            else:
                pr = prod[k % 2]
                self.tt("dve", pr, ps[:], sgt[k], ALU.mult, [Bps, Bsgt[k]], [Bprod[k % 2]])
                self.tt("pool", msl, pr, msl, ALU.add, [Bprod[k % 2], BmT[ncx][tb]], [BmT[ncx][tb]])
        wv = self.w_out[l].rearrange("(c p) n -> p c n", p=128)

        def loadwo(dcx):
            s_ = dcx % 3
            S.dma("pool", Wo[s_], wv[:, :, dcx * 128:(dcx + 1) * 128], [], [BWo[s_]], self.ds[16 + s_])

        oitems = [(dcx, tb) for dcx in range(16) for tb in range(4)]

        def loadx(ii):
            dcx, tb = oitems[ii]
            k = ii % 4
            S.dma("sp", xt[k], xsrc[dcx * 128:(dcx + 1) * 128, tb * 512:(tb + 1) * 512], [self.Bx[dcx][tb]], [Bxt[k]], self.ds[19 + k])

        loadwo(0)
        loadwo(1)
        loadx(0)
        loadx(1)
        for ii, (dcx, tb) in enumerate(oitems):
            if tb == 0 and dcx + 2 < 16:
                loadwo(dcx + 2)
            if ii + 2 < len(oitems):
                loadx(ii + 2)
            s_ = dcx % 3
            k = ii % 4
            ps = bank[4 + k]
            Bps = Bbank[4 + k]
            for c in range(16):
                self.mm(ps[:], Wo[s_][:, c, :], mT[:, c, tb * 512:(tb + 1) * 512], c == 0, c == 15,
                        [BWo[s_], BmT[c][tb]], [Bps], inc=(c == 15))
            self.tt("dve", ost[k], ps[:], xt[k], ALU.add, [Bps, Bxt[k]], [Bost[k]])
            S.dma("sp", self.xres[dcx * 128:(dcx + 1) * 128, tb * 512:(tb + 1) * 512], ost[k], [Bost[k]], [self.Bx[dcx][tb]], self.ds[23 + k])

    def phase_ffn_up(self, l):
        S = self.S
        hT, BhT = self.hT, self.BhT
        bank, Bbank = self.bank, self.Bbank
        NW = 3
        Wa = [self.alloc([128, 16, 128], BF16) for _ in range(NW)]
        Wb = [self.alloc([128, 16, 128], BF16) for _ in range(NW)]
        BWa, BWb = self.bufs(NW), self.bufs(NW)
        ua = [self.alloc([128, 2 + SEQ], F32) for _ in range(2)]
        ub = [self.alloc([128, 2 + SEQ], F32) for _ in range(2)]
        Bua = [[Buf() for _ in range(5)] for _ in range(2)]
        Bub = [[Buf() for _ in range(5)] for _ in range(2)]
        ta = [self.alloc([128, 512], F32) for _ in range(2)]
        tb_ = [self.alloc([128, 512], F32) for _ in range(2)]
        Bta, Btb = self.bufs(2), self.bufs(2)
        sl_ = [self.alloc([128, 512], F32) for _ in range(2)]
        Bsl = self.bufs(2)
        ast = [self.alloc([128, SEQ], BF16) for _ in range(2)]
        Bast = self.bufs(2)
        wv = self.w_up[l].rearrange("(c p) n -> p c n", p=128)
        for s_ in range(2):
            self.memset("dve", ua[s_][:, 0:2], 0.0, [], [Bua[s_][4]])
            self.memset("dve", ub[s_][:, 0:2], 0.0, [], [Bub[s_][4]])

        def loadw(fc):
            s = fc % NW
            S.dma("pool", Wa[s], wv[:, :, fc * 128:(fc + 1) * 128], [], [BWa[s]], self.ds[0 + s])
            S.dma("pool", Wb[s], wv[:, :, D_FF + fc * 128:D_FF + (fc + 1) * 128], [], [BWb[s]], self.ds[3 + s])
        loadw(0)
        loadw(1)
        cnt = 0
        cw, cb = self.convw, self.convb
        for fc in range(NFC):
            if fc + 2 < NFC:
                loadw(fc + 2)
            s = fc % NW
            u = fc % 2
            for tb in range(4):
                for (W_, BW_, uu, Buu, tt_, Btt, ch, pb) in ((Wa[s], BWa[s], ua[u], Bua[u], ta, Bta, fc, 0),
                                                            (Wb[s], BWb[s], ub[u], Bub[u], tb_, Btb, NFC + fc, 1)):
                    k = cnt % 4
                    cnt += 1
                    ps = bank[k]
                    Bps = Bbank[k]
                    for c in range(16):
                        self.mm(ps[:], W_[:, c, :], hT[:, c, tb * 512:(tb + 1) * 512], c == 0, c == 15,
                                [BW_, BhT[c][2 * tb], BhT[c][2 * tb + 1]], [Bps], inc=(c == 15))
                    self.cp("act", uu[:, 2 + tb * 512:2 + (tb + 1) * 512], ps[:], [Bps], [Buu[tb]])
                    prev = [Buu[tb - 1]] if tb > 0 else [Buu[4]]
                    t_ = tt_[tb % 2]
                    Bt_ = Btt[tb % 2]
                    self.ts("dve", t_, uu[:, 2 + tb * 512:2 + (tb + 1) * 512], cw[:, ch, 2:3], cb[:, ch:ch + 1], ALU.mult, ALU.add,
                            [Buu[tb], self.Blayer], [Bt_])
                    self.stt(t_, uu[:, 1 + tb * 512:1 + (tb + 1) * 512], cw[:, ch, 1:2], t_, ALU.mult, ALU.add,
                             [Buu[tb], self.Blayer, Bt_] + prev, [Bt_])
                    self.stt(t_, uu[:, tb * 512:(tb + 1) * 512], cw[:, ch, 0:1], t_, ALU.mult, ALU.add,
                             [Buu[tb], self.Blayer, Bt_] + prev, [Bt_])
                sx = sl_[tb % 2]
                self.act(sx, ta[tb % 2], AF.Silu, [Bta[tb % 2]], [Bsl[tb % 2]])
                self.tt("pool", ast[u][:, tb * 512:(tb + 1) * 512], sx, tb_[tb % 2], ALU.mult, [Bsl[tb % 2], Btb[tb % 2]], [Bast[u]])
            S.dma("sp", self.actT[fc * 128:(fc + 1) * 128, :], ast[u], [Bast[u]], self.Bact[fc], self.ds[6 + u])

    def phase_ffn_down(self, l, xdst):
        S = self.S
        self.arena_reset()
        bank, Bbank = self.bank, self.Bbank
        NSB = 11
        Wd = [self.alloc([128, NFC, 512], BF16) for _ in range(2)]
        BWd = [self.bufs(4) for _ in range(2)]
        At = self.alloc([128, NFC, 512], BF16)
        BAt = self.bufs(NSB)
        xt = [self.alloc([128, 512], F32) for _ in range(8)]
        Bxt = self.bufs(8)
        ost = [self.alloc([128, 512], F32) for _ in range(4)]
        Bost = self.bufs(4)
        wv = self.w_down[l].rearrange("(c p) n -> p c n", p=128)
        av = self.actT.rearrange("(c p) t -> p c t", p=128)

        def loadw(dg):
            for sblk in range(4):
                cs = slice(sblk * 11, (sblk + 1) * 11)
                S.dma("pool", Wd[dg % 2][:, cs, :], wv[:, cs, dg * 512:(dg + 1) * 512], [], [BWd[dg % 2][sblk]], self.ds[0 + (dg % 2) * 4 + sblk])

        its = [(dg, tb) for dg in range(4) for tb in range(4)]

        def loada(ii, sblk):
            dg, tb = its[ii]
            cs = slice(sblk * 4, (sblk + 1) * 4)
            S.dma("sp", At[:, cs, :], av[:, cs, tb * 512:(tb + 1) * 512],
                  [self.Bact[c][tb] for c in range(sblk * 4, (sblk + 1) * 4)], [BAt[sblk]], self.ds[8 + sblk])

        def loadx(ii):
            dg, tb = its[ii]
            for dcl in range(4):
                dcx = dg * 4 + dcl
                k = (ii % 2) * 4 + dcl
                S.dma("sp", xt[k], self.xres[dcx * 128:(dcx + 1) * 128, tb * 512:(tb + 1) * 512], [self.Bx[dcx][tb]], [Bxt[k]], self.ds[19 + k])

        loadw(0)
        for sblk in range(NSB):
            loada(0, sblk)
        loadx(0)
        for ii, (dg, tb) in enumerate(its):
            if tb == 0 and dg + 1 < 4:
                loadw(dg + 1)
            if ii + 1 < len(its):
                loadx(ii + 1)
            pb = (ii % 2) * 4
            for sblk in range(NSB):
                for dcl in range(4):
                    for c in range(sblk * 4, (sblk + 1) * 4):
                        self.mm(bank[pb + dcl][:], Wd[dg % 2][:, c, dcl * 128:(dcl + 1) * 128], At[:, c, :], c == 0, c == NFC - 1,
                                [BWd[dg % 2][c // 11], BAt[sblk]], [Bbank[pb + dcl]],
                                inc=(c == NFC - 1 or (dcl == 3 and c == sblk * 4 + 3)))
                if ii + 1 < len(its):
                    loada(ii + 1, sblk)
            for dcl in range(4):
                dcx = dg * 4 + dcl
                k = pb + dcl
                ko = dcl
                self.tt("dve", ost[ko], bank[pb + dcl][:], xt[k], ALU.add, [Bbank[pb + dcl], Bxt[k]], [Bost[ko]])
                S.dma("sp", xdst[dcx * 128:(dcx + 1) * 128, tb * 512:(tb + 1) * 512], ost[ko], [Bost[ko]], [self.Bx[dcx][tb]], self.ds[27 + ko])


def _const_inputs():
    f32 = np.float32
    pos = np.arange(SEQ, dtype=f32)
    ret_freq = (1.0 / (10000.0 ** np.linspace(0.0, 1.0, 64, dtype=f32))).astype(f32)
    rope_freq = (1.0 / (10000.0 ** (np.arange(0, 128, 2, dtype=f32) / f32(128)))).astype(f32)
    tabs = np.zeros((128, 4, 16, 64), f32)
    for ti, fr in ((0, ret_freq), (2, rope_freq)):
        ang = (pos[:, None] * fr[None, :]).astype(f32)
        c = np.cos(ang).astype(f32).reshape(16, 128, 64).transpose(1, 0, 2)
        s = np.sin(ang).astype(f32).reshape(16, 128, 64).transpose(1, 0, 2)
        tabs[:, ti] = c
        tabs[:, ti + 1] = s
    h = np.arange(8, dtype=np.float64)
    log_gamma = np.log1p(-np.exp2(-5.0 - h))
    p = np.arange(128, dtype=np.float64)
    small = np.zeros((128, 24), f32)
    small[:, 0:8] = np.exp((p[:, None] + 1.0) * log_gamma[None, :])
    small[:, 8:16] = (128.0 ** -0.5) * np.exp(-(p[:, None] + 1.0) * log_gamma[None, :])
    small[:, 16:24] = np.exp(128.0 * log_gamma)[None, :]
    j = np.arange(128)
    mask = (j[None, :] >= j[:, None]).astype(f32)
    ident = np.eye(128, dtype=f32)
    negm = np.zeros((128, 8, 8), f32)
    selfix = np.zeros((128, 2, 8, 8), f32)
    for bq in range(8):
        for n in range(8):
            negm[:, bq, n] = 0.0 if n < bq else -1e30
            selfix[:, 0, bq, n] = 1.0 if n < bq else 0.0
            selfix[:, 1, bq, n] = 1.0 if n == bq else 0.0
    return dict(c_tabs=tabs, c_small=small, c_mask=mask, c_ident=ident, c_negm=negm, c_selfix=selfix)


def _layout_inputs(inputs):
    f32 = np.float32
    A = lambda k: np.ascontiguousarray(np.asarray(inputs[k], dtype=f32))
    shared = {}
    for k in ("w_in", "w_pa", "w_pb", "w_pc", "w_out", "w_up", "w_down"):
        shared[k] = A(k)
    shared["norm_mix_t"] = np.ascontiguousarray(A("norm_mix").reshape(DEPTH, 16, 128).transpose(0, 2, 1))
    shared["norm_ffn_t"] = np.ascontiguousarray(A("norm_ffn").reshape(DEPTH, 16, 128).transpose(0, 2, 1))
    shared["b_ig_t"] = A("b_ig").reshape(DEPTH, 8, 1)
    shared["b_fg_t"] = A("b_fg").reshape(DEPTH, 8, 1)
    shared["ret_gn_rep"] = np.ascontiguousarray(np.broadcast_to(A("ret_gn")[:, None, :], (DEPTH, 128, 1024)))
    shared["ml_norm_rep"] = np.ascontiguousarray(np.broadcast_to(A("ml_norm")[:, None, :], (DEPTH, 128, 1024)))
    shared["q_norm_rep"] = np.ascontiguousarray(np.broadcast_to(A("q_norm")[:, None, :], (DEPTH, 128, 128)))
    shared["k_norm_rep"] = np.ascontiguousarray(np.broadcast_to(A("k_norm")[:, None, :], (DEPTH, 128, 128)))
    cw = A("conv_w")
    shared["conv_w_t"] = np.ascontiguousarray(cw.reshape(DEPTH, 3, 88, 128).transpose(0, 3, 2, 1))
    shared["conv_b_t"] = np.ascontiguousarray(A("conv_b").reshape(DEPTH, 88, 128).transpose(0, 2, 1))
    shared.update(_const_inputs())
    return shared


_NC_CACHE = {}


def _get_nc(n_layers=DEPTH, dbg=False, stop_after=None):
    key = (n_layers, dbg, stop_after)
    if key not in _NC_CACHE:
        kb = KB(n_layers, dbg)
        kb.stop_after = stop_after
        _NC_CACHE[key] = (kb.build(), kb)
    return _NC_CACHE[key][0]


def kernel(**inputs):
    x = np.asarray(inputs["x"], dtype=np.float32)
    B = x.shape[0]
    shared = _layout_inputs(inputs)
    nc = _get_nc()
    in_maps = []
    for b in range(B):
        m = dict(shared)
        m["xT"] = np.ascontiguousarray(x[b].T)
        in_maps.append(m)
    res = run_bass_kernel_spmd(nc, in_maps, core_ids=list(range(B)))
    out = np.stack([np.ascontiguousarray(np.asarray(r["outT"]).T) for r in res.results], axis=0)
    return out.astype(np.float32)
```

```python
import math
from contextlib import ExitStack

import numpy as np
import concourse.bass as bass
import concourse.mybir as mybir
from concourse.bass_utils import run_bass_kernel_spmd

F32 = mybir.dt.float32
BF16 = mybir.dt.bfloat16
AF = mybir.ActivationFunctionType
ALU = mybir.AluOpType
AX = mybir.AxisListType

ENGS = ["pe", "act", "dve", "pool", "sp"]

DEPTH = 4
SEQ = 2048
DM = 2048
NT = 16
N_IN = 16400
D_FF = 5632
NFC = 44
C_RQ, C_RK, C_RV, C_RG = 0, 1024, 2048, 3072
C_MQ, C_MK, C_MV, C_MO, C_MI, C_MF = 4096, 4608, 5120, 6144, 7168, 7176
C_AQ, C_AK, C_AV = 7184, 8208, 9232
C_GA = 10256


class Buf:
    __slots__ = ("name", "last_w", "readers")

    def __init__(self, name=""):
        self.name = name
        self.last_w = None
        self.readers = {}


class DSem:
    __slots__ = ("name", "count")

    def __init__(self, name):
        self.name = name
        self.count = 0


class Sched:
    def __init__(self, nc):
        self.nc = nc
        self.streams = {e: [] for e in ENGS}
        self.cnt = {e: 0 for e in ENGS}
        self.waited = {e: {} for e in ENGS}
        self.semnames = list(ENGS)
        self.dsems = []

    def dsem(self):
        d = DSem("d%d" % len(self.dsems))
        self.dsems.append(d)
        self.semnames.append(d.name)
        return d

    def _collect(self, eng, reads, writes):
        deps = {}
        for b in reads:
            t = b.last_w
            if t is not None and deps.get(t[0], 0) < t[1]:
                deps[t[0]] = t[1]
        for b in writes:
            t = b.last_w
            if t is not None and deps.get(t[0], 0) < t[1]:
                deps[t[0]] = t[1]
            for s, v in b.readers.items():
                if deps.get(s, 0) < v:
                    deps[s] = v
        waits = []
        w = self.waited[eng]
        for s, v in deps.items():
            if s == "pe" and eng == "pe":
                continue
            if w.get(s, 0) >= v:
                continue
            w[s] = v
            waits.append((s, v))
        return waits

    def _commit(self, tok, reads, writes):
        s, v = tok
        for b in reads:
            if b.readers.get(s, 0) < v:
                b.readers[s] = v
        for b in writes:
            b.last_w = tok
            b.readers = {}

    def op(self, eng, fn, reads=(), writes=(), inc=True):
        waits = self._collect(eng, reads, writes)
        if inc:
            self.cnt[eng] += 1
            tok = (eng, self.cnt[eng])
        else:
            tok = (eng, self.cnt[eng] + 1)
        self._commit(tok, reads, writes)
        self.streams[eng].append((fn, waits, eng if inc else None, 1))

    def dma(self, q, out, in_, reads, writes, dsem):
        waits = self._collect(q, reads, writes)
        dsem.count += 16
        tok = (dsem.name, dsem.count)
        self._commit(tok, reads, writes)
        self.streams[q].append(
            (lambda e, out=out, in_=in_: e.dma_start(out=out, in_=in_), waits, dsem.name, 16))

    def barrier(self):
        cur = {e: self.cnt[e] for e in ENGS if self.cnt[e] > 0}
        for d in self.dsems:
            if d.count > 0:
                cur[d.name] = d.count
        for e in ENGS:
            w = self.waited[e]
            waits = []
            for s, v in cur.items():
                if s == e and e == "pe":
                    w[s] = v
                    continue
                if w.get(s, 0) >= v:
                    continue
                w[s] = v
                waits.append((s, v))
            if waits:
                self.streams[e].append((None, waits, None, 0))

    def emit(self, stack):
        nc = self.nc
        sems = {}
        for n in self.semnames:
            sems[n] = stack.enter_context(nc.semaphore("s_" + n))
        block = stack.enter_context(nc.Block())
        handles = {"pe": block.tensor, "act": block.scalar, "dve": block.vector,
                   "pool": block.gpsimd, "sp": block.sync}
        for e in ENGS:
            stream = self.streams[e]

            def body(eng, stream=stream):
                for fn, waits, incsem, incv in stream:
                    for s, v in waits:
                        eng.wait_ge(sems[s], v)
                    if fn is None:
                        continue
                    ins = fn(eng)
                    if incsem is not None:
                        ins.then_inc(sems[incsem], incv)
            handles[e](body)


def _dtsize(dt):
    return 2 if dt == BF16 else 4


class KB:
    def __init__(self, n_layers=DEPTH, dbg=False, wdepth=DEPTH):
        self.n_layers = n_layers
        self.wdepth = wdepth
        self.dbg = dbg
        self.nc = bass.Bass("TRN2", target_bir_lowering=False)
        self.S = Sched(self.nc)

    def tt(self, eng, out, in0, in1, op, R, W):
        self.S.op(eng, lambda e: e.tensor_tensor(out=out, in0=in0, in1=in1, op=op), R, W)

    def ts(self, eng, out, in0, s1, s2, op0, op1, R, W):
        if s2 is None:
            self.S.op(eng, lambda e: e.tensor_scalar(out=out, in0=in0, scalar1=s1, scalar2=None, op0=op0), R, W)
        else:
            self.S.op(eng, lambda e: e.tensor_scalar(out=out, in0=in0, scalar1=s1, scalar2=s2, op0=op0, op1=op1), R, W)

    def stt(self, out, in0, scalar, in1, op0, op1, R, W):
        self.S.op("dve", lambda e: e.scalar_tensor_tensor(out=out, in0=in0, scalar=scalar, in1=in1, op0=op0, op1=op1), R, W)

    def act(self, out, in_, func, R, W, scale=1.0, bias=0.0):
        self.S.op("act", lambda e: e.activation(out=out, in_=in_, func=func, bias=bias, scale=scale), R, W)

    def cp(self, eng, out, in_, R, W):
        if eng == "act":
            self.S.op("act", lambda e: e.activation(out=out, in_=in_, func=AF.Copy), R, W)
        else:
            self.S.op(eng, lambda e: e.tensor_copy(out=out, in_=in_), R, W)

    def mm(self, out, lhsT, rhs, start, stop, R, W, inc):
        self.S.op("pe", lambda e: e.matmul(out, lhsT=lhsT, rhs=rhs, start=start, stop=stop), R, W, inc=inc)

    def tr(self, out, in_, ident, R, W, inc):
        self.S.op("pe", lambda e: e.transpose(out=out, in_=in_, identity=ident), R, W, inc=inc)

    def red(self, out, in_, op, R, W):
        self.S.op("dve", lambda e: e.tensor_reduce(out=out, in_=in_, axis=AX.X, op=op), R, W)

    def recip(self, out, in_, R, W):
        self.S.op("dve", lambda e: e.reciprocal(out=out, in_=in_), R, W)

    def memset(self, eng, ap, val, R, W):
        self.S.op(eng, lambda e: e.memset(ap, val), R, W)

    def arena_reset(self):
        self.aoff = 0

    def alloc(self, shape, dt):
        nel = 1
        for s in shape[1:]:
            nel *= s
        nbytes = nel * _dtsize(dt)
        nbytes = (nbytes + 31) // 32 * 32
        off = self.aoff
        self.aoff += nbytes
        assert self.aoff <= self.arena_bytes, ("arena overflow", self.aoff, self.arena_bytes)
        w0 = off // 4
        ap = self.arena[0:shape[0], w0:w0 + nbytes // 4]
        if dt != F32:
            ap = ap.bitcast(dt)
        ap = ap[:, 0:nel]
        if len(shape) == 3:
            ap = ap.rearrange("p (a b) -> p a b", a=shape[1])
        elif len(shape) == 4:
            ap = ap.rearrange("p (a b c) -> p a b c", a=shape[1], b=shape[2])
        return ap

    def bufs(self, n):
        return [Buf() for _ in range(n)]

    def build(self):
        nc = self.nc
        S = self.S
        dbg = self.dbg
        L = self.n_layers
        self.stack = ExitStack()
        st = self.stack

        def din(name, shape, dt=F32):
            return nc.dram_tensor(name, list(shape), dt, kind="ExternalInput").ap()

        def dscr(name, shape, dt=F32):
            kind = "ExternalOutput" if dbg else "Internal"
            return nc.dram_tensor(name, list(shape), dt, kind=kind).ap()

        self.xT = din("xT", [DM, SEQ])
        self.w_in = din("w_in", [self.wdepth, DM, N_IN])
        self.w_pa = din("w_pa", [self.wdepth, 1024, DM])
        self.w_pb = din("w_pb", [self.wdepth, 1024, DM])
        self.w_pc = din("w_pc", [self.wdepth, 1024, DM])
        self.w_out = din("w_out", [self.wdepth, DM, DM])
        self.w_up = din("w_up", [self.wdepth, DM, 2 * D_FF])
        self.w_down = din("w_down", [self.wdepth, D_FF, DM])
        self.i_norm_mix = din("norm_mix_t", [self.wdepth, 128, 16])
        self.i_norm_ffn = din("norm_ffn_t", [self.wdepth, 128, 16])
        self.i_big = din("b_ig_t", [self.wdepth, 8, 1])
        self.i_bfg = din("b_fg_t", [self.wdepth, 8, 1])
        self.i_retgn = din("ret_gn_rep", [self.wdepth, 128, 1024])
        self.i_mlnorm = din("ml_norm_rep", [self.wdepth, 128, 1024])
        self.i_qnorm = din("q_norm_rep", [self.wdepth, 128, 128])
        self.i_knorm = din("k_norm_rep", [self.wdepth, 128, 128])
        self.i_convw = din("conv_w_t", [self.wdepth, 128, 88, 3])
        self.i_convb = din("conv_b_t", [self.wdepth, 128, 88])
        self.i_tabs = din("c_tabs", [128, 4, 16, 64])
        self.i_small = din("c_small", [128, 24])
        self.i_mask = din("c_mask", [128, 128])
        self.i_ident = din("c_ident", [128, 128])
        self.i_negm = din("c_negm", [128, 8, 8])
        self.i_selfix = din("c_selfix", [128, 2, 8, 8])
        self.outT = nc.dram_tensor("outT", [DM, SEQ], F32, kind="ExternalOutput").ap()
        self.xres = dscr("xres", [DM, SEQ])
        self.z = {}
        for nm, w in (("rq", 1024), ("rk", 1024), ("rv", 1024), ("rg", 1024), ("mq", 512), ("mk", 512),
                      ("mv", 1024), ("mo", 1024), ("aq", 1024), ("ak", 1024), ("av", 1024)):
            self.z[nm] = dscr("z_" + nm, [SEQ, w], F32 if nm in ("aq", "ak") else BF16)
        self.sgT = dscr("sgT", [3 * DM, SEQ], BF16)
        self.yT = [dscr("yT%d" % b, [1024, SEQ], BF16) for b in range(3)]
        self.actT = dscr("actT", [D_FF, SEQ], BF16)
        self.Bx = [[Buf() for _ in range(4)] for _ in range(16)]
        self.Bz = {nm: [[Buf() for _ in range(2)] for _ in range(NT)] for nm in self.z}
        self.Bsg = [[Buf() for _ in range(4)] for _ in range(48)]
        self.ByT = [[Buf() for _ in range(32)] for _ in range(3)]
        self.Bact = [[Buf() for _ in range(4)] for _ in range(NFC)]

        def sb(name, shape, dt):
            return st.enter_context(nc.sbuf_tensor(name, list(shape), dt))

        self.tabs = sb("tabs", [128, 4, 16, 64], F32)
        self.small = sb("small", [128, 24], F32)
        self.maskf = sb("maskf", [128, 128], F32)
        self.maskb = sb("maskb", [128, 128], BF16)
        self.identf = sb("identf", [128, 128], F32)
        self.identb = sb("identb", [128, 128], BF16)
        self.onesf = sb("onesf", [128, 128], F32)
        self.onescb = sb("onescb", [128, 2], BF16)
        self.onesb = sb("onesb", [128, 128], BF16)
        self.kmcol = sb("kmcol", [128, 2], F32)
        self.negm = sb("negm", [128, 8, 8], F32)
        self.selfix = sb("selfix", [128, 2, 8, 8], F32)
        self.gmix = sb("gmix", [128, 16], F32)
        self.gffn = sb("gffn", [128, 16], F32)
        self.big = sb("big", [8, 1], F32)
        self.bfg = sb("bfg", [8, 1], F32)
        self.retgn = sb("retgn", [128, 1024], F32)
        self.mlnorm = sb("mlnorm", [128, 1024], F32)
        self.qnw = sb("qnw", [128, 128], F32)
        self.knw = sb("knw", [128, 128], F32)
        self.convw = sb("convw", [128, 88, 3], F32)
        self.convb = sb("convb", [128, 88], F32)
        self.mtab = sb("mtab", [128, 3, 16, 8], F32)
        self.Bconst = Buf()
        self.Blayer = Buf()
        self.Bmtab = Buf()
        self.arena_bytes = (nc.sbuf_bytes_remaining // 32) * 32 - 64
        self.arena = sb("arena", [128, self.arena_bytes // 4], F32)
        self.bank = [st.enter_context(nc.psum_tensor("bank%d" % i, [128, 512], F32)) for i in range(8)]
        self.Bbank = [Buf() for _ in range(8)]
        self.ds = [S.dsem() for _ in range(40)]
        self.dconst = S.dsem()

        dc = self.dconst
        S.dma("sp", self.tabs[:], self.i_tabs[:, :, :, :], [], [self.Bconst], dc)
        S.dma("sp", self.small[:], self.i_small[:, :], [], [self.Bconst], dc)
        S.dma("sp", self.maskf[:], self.i_mask[:, :], [], [self.Bconst], dc)
        S.dma("sp", self.identf[:], self.i_ident[:, :], [], [self.Bconst], dc)
        S.dma("sp", self.negm[:], self.i_negm[:, :, :], [], [self.Bconst], dc)
        S.dma("sp", self.selfix[:], self.i_selfix[:, :, :, :], [], [self.Bconst], dc)
        S.barrier()
        self.cp("dve", self.maskb[:], self.maskf[:], [self.Bconst], [self.Bconst])
        self.cp("dve", self.identb[:], self.identf[:], [self.Bconst], [self.Bconst])
        self.memset("dve", self.onesf[:], 1.0, [], [self.Bconst])
        self.memset("dve", self.onescb[:], 1.0, [], [self.Bconst])
        self.memset("dve", self.onesb[:], 1.0, [], [self.Bconst])
        self.memset("dve", self.kmcol[:], 1.0 / 256.0, [], [self.Bconst])
        S.barrier()

        for l in range(L):
            self.layer(l)

        S.barrier()
        S.emit(st)
        return nc

    def layer(self, l):
        S = self.S
        dc = self.dconst
        xsrc = self.xT if l == 0 else self.xres
        xdst_final = self.outT if l == self.n_layers - 1 else self.xres
        for dst, src in ((self.gmix, self.i_norm_mix[l]), (self.gffn, self.i_norm_ffn[l]),
                         (self.big, self.i_big[l]), (self.bfg, self.i_bfg[l]),
                         (self.retgn, self.i_retgn[l]), (self.mlnorm, self.i_mlnorm[l]),
                         (self.qnw, self.i_qnorm[l]), (self.knw, self.i_knorm[l]),
                         (self.convw, self.i_convw[l]), (self.convb, self.i_convb[l])):
            S.dma("sp", dst[:], src, [], [self.Blayer], dc)
        S.barrier()
        self.ts("dve", self.qnw[:], self.qnw[:], 128.0 ** -0.5, None, ALU.mult, None, [self.Blayer], [self.Blayer])
        S.barrier()

        self.phase_norm(xsrc, self.gmix)
        self.phase_proj(l)
        S.barrier()
        self.phase_moba(l)
        S.barrier()
        if self.stop_after == "C":
            return
        self.phase_merge(l, xsrc)
        S.barrier()
        if self.stop_after == "G":
            return
        self.phase_norm(self.xres, self.gffn)
        self.phase_ffn_up(l)
        S.barrier()
        if self.stop_after == "F1":
            return
        self.phase_ffn_down(l, xdst_final)
        S.barrier()

    stop_after = None

    def phase_norm(self, xsrc, g):
        S = self.S
        self.arena_reset()
        self.hT = self.alloc([128, 16, SEQ], BF16)
        self.BhT = [[Buf() for _ in range(8)] for _ in range(16)]
        mark = self.aoff
        NXB = 3
        xb = [self.alloc([128, 16, 256], F32) for _ in range(NXB)]
        Bxb = self.bufs(NXB)
        sq = [self.alloc([128, 256], F32) for _ in range(4)]
        Bsq = self.bufs(4)
        sd = [self.alloc([128, 256], F32) for _ in range(NXB)]
        Bsd = self.bufs(NXB)
        xv = xsrc.rearrange("(c p) t -> p c t", p=128)
        def loadx(t):
            b = t % NXB
            S.dma("sp", xb[b], xv[:, :, t * 256:(t + 1) * 256], [self.Bx[c][t // 2] for c in range(16)], [Bxb[b]], self.ds[b])

        loadx(0)
        loadx(1)
        for t in range(8):
            b = t % NXB
            if t + 2 < 8:
                loadx(t + 2)
            ps = self.bank[b][:, 0:256]
            Bps = self.Bbank[b]
            for c in range(16):
                k = c % 4
                self.act(sq[k], xb[b][:, c, :], AF.Square, [Bxb[b]], [Bsq[k]])
                self.mm(ps, self.onesf[:], sq[k], c == 0, c == 15, [Bsq[k], self.Bconst], [Bps], inc=True)
            self.act(sd[b], ps, AF.Sqrt, [Bps], [Bsd[b]], scale=1.0 / DM, bias=1e-6)
            self.recip(sd[b], sd[b], [Bsd[b]], [Bsd[b]])
            for c in range(16):
                self.stt(self.hT[:, c, t * 256:(t + 1) * 256], xb[b][:, c, :], g[:, c:c + 1], sd[b],
                         ALU.mult, ALU.mult, [Bxb[b], Bsd[b], self.Blayer], [self.BhT[c][t]])
        self.aoff = mark
        S.barrier()

    def rot4(self, eng, dst, src, cos, sin, tmp, Btmp, R, Bsrc, Bdst, nh):
        sv = src.rearrange("p (h two d) -> p h two d", h=nh, two=2)
        dv = dst.rearrange("p (h two d) -> p h two d", h=nh, two=2)
        x1 = sv[:, :, 0, :]
        x2 = sv[:, :, 1, :]
        cb = cos.unsqueeze(1).broadcast_to([128, nh, 64])
        sbb = sin.unsqueeze(1).broadcast_to([128, nh, 64])
        t1v = tmp[0].rearrange("p (h d) -> p h d", h=nh)
        t2v = tmp[1].rearrange("p (h d) -> p h d", h=nh)
        self.tt(eng, t1v, x1, cb, ALU.mult, [Bsrc] + R, [Btmp[0]])
        self.tt(eng, t2v, x2, sbb, ALU.mult, [Bsrc] + R, [Btmp[1]])
        self.tt(eng, dv[:, :, 0, :], t1v, t2v, ALU.subtract, [Btmp[0], Btmp[1]], [Bdst])
        self.tt(eng, t1v, x1, sbb, ALU.mult, [Bsrc] + R, [Btmp[0]])
        self.tt(eng, t2v, x2, cb, ALU.mult, [Bsrc] + R, [Btmp[1]])
        self.tt(eng, dv[:, :, 1, :], t1v, t2v, ALU.add, [Btmp[0], Btmp[1]], [Bdst])

    def phase_proj(self, l):
        S = self.S
        hT = self.hT
        BhT = self.BhT
        bank, Bbank = self.bank, self.Bbank
        NW = 2
        Wt = [self.alloc([128, 16, 512], BF16) for _ in range(NW)]
        BW = self.bufs(NW)
        NR = 4
        stg = [self.alloc([128, 512], F32) for _ in range(NR)]
        Bstg = self.bufs(NR)
        stgb = [self.alloc([128, 512], BF16) for _ in range(NR)]
        Bstgb = self.bufs(NR)
        rt = {e: [self.alloc([128, 256], F32) for _ in range(2)] for e in ("dve", "pool")}
        Brt = {e: self.bufs(2) for e in ("dve", "pool")}
        r4 = [self.alloc([128, 4], F32) for _ in range(NR)]
        Br4 = self.bufs(NR)
        wv = self.w_in[l].rearrange("(c p) n -> p c n", p=128)
        self.pcnt = 0
        Wg = self.alloc([128, 16, 16], BF16)
        BWg = Buf()
        S.dma("pool", Wg, wv[:, :, C_MI:C_MI + 16], [], [BWg], self.ds[6])
        r8 = [self.alloc([128, 4], F32) for _ in range(8)]
        Br8 = self.bufs(8)
        mixer_base = self.aoff
        stg2 = stg3 = stg4 = None
        Bstg2 = self.bufs(8)
        Bstg3 = self.bufs(8)
        Bstg4 = self.bufs(8)
        A = self.alloc([8, SEQ], F32)
        Bm = self.alloc([8, SEQ], F32)
        Cc = self.alloc([8, SEQ], F32)
        Dd = self.alloc([8, SEQ], F32)
        BA, BB, BC, BD = self.bufs(4)
        blocks = []
        seg_of = []
        order = (("rq", C_RQ, 1024, 0), ("rk", C_RK, 1024, 0), ("rv", C_RV, 1024, 0), ("rg", C_RG, 1024, 0),
                 ("mq", C_MQ, 512, 1), ("mk", C_MK, 512, 1), ("mv", C_MV, 1024, 1), ("mo", C_MO, 1024, 1),
                 ("av", C_AV, 1024, 2))
        for nm, c0, w, seg in order:
            for j in range(w // 512):
                blocks.append(("tm", nm, c0 + j * 512, j))
                seg_of.append(seg)
        for gb in range(12):
            blocks.append(("fm", None, C_GA + gb * 512, gb))
            seg_of.append(2)
        for nm, c0, w, seg in (("aq", C_AQ, 1024, 3), ("ak", C_AK, 1024, 3)):
            for j in range(w // 512):
                blocks.append(("tm", nm, c0 + j * 512, j))
                seg_of.append(seg)
        nblk = len(blocks)

        def load(bi):
            kind, nm, c0, j = blocks[bi]
            s_ = bi % NW
            S.dma("pool", Wt[s_], wv[:, :, c0:c0 + 512], [], [BW[s_]], self.ds[0 + s_])

        load(0)
        load(1)
        for which, dstT, Bd in ((0, A, BA), (1, Bm, BB)):
            for tb in range(4):
                k = self.pcnt % 2
                self.pcnt += 1
                ps = bank[k]
                Bps = Bbank[k]
                for c in range(16):
                    self.mm(ps[0:8, :], Wg[:, c, which * 8:(which + 1) * 8], hT[:, c, tb * 512:(tb + 1) * 512],
                            c == 0, c == 15, [BhT[c][2 * tb], BhT[c][2 * tb + 1], BWg], [Bps], inc=(c == 15))
                self.cp("act", dstT[:, tb * 512:(tb + 1) * 512], ps[0:8, :], [Bps], [Bd])
        self.ts("dve", A, A, self.big[:, 0:1], 1.0 / 15.0, ALU.add, ALU.mult, [BA, self.Blayer], [BA])
        self.act(A, A, AF.Tanh, [BA], [BA])
        self.ts("dve", Bm, Bm, self.bfg[:, 0:1], 1.0 / 15.0, ALU.add, ALU.mult, [BB, self.Blayer], [BB])
        self.act(Bm, Bm, AF.Tanh, [BB], [BB])
        self.act(Bm, Bm, AF.Exp, [BB], [BB], scale=-15.0)
        self.act(Bm, Bm, AF.Ln, [BB], [BB], bias=1.0)
        for n in range(NT):
            sl = slice(n * 128, (n + 1) * 128)
            self.S.op("dve", lambda e, sl=sl: e.tensor_tensor_scan(out=Cc[:, sl], data0=self.onesf[0:8, :], data1=Bm[:, sl],
                                                                  initial=0.0, op0=ALU.mult, op1=ALU.subtract),
                      [BB, self.Bconst], [BC])
        self.act(Dd, Cc, AF.Exp, [BC], [BD])
        self.stt(A, A, 15.0, Cc, ALU.mult, ALU.subtract, [BA, BC], [BA])
        self.act(A, A, AF.Exp, [BA], [BA], bias=math.log(0.125))
        cl = Cc.rearrange("p (n t) -> p n t", t=128)[:, :, 127:128].broadcast_to([8, NT, 128])
        self.act(Bm.rearrange("p (n t) -> p n t", t=128), cl, AF.Exp, [BC], [BB])
        k = self.pcnt % 2
        self.pcnt += 1
        ps = bank[k]
        Bps = Bbank[k]
        idx = 0
        for wi, (src, Bs) in enumerate(((Dd, BD), (A, BA), (Bm, BB))):
            for n in range(NT):
                idx += 1
                col = (wi * NT + n) * 8
                self.tr(ps[:, col:col + 8], src[:, n * 128:(n + 1) * 128], self.identf[0:8, 0:8],
                        [Bs, self.Bconst], [Bps], inc=(idx == 48))
        self.cp("act", self.mtab[:].rearrange("p a n h -> p (a n h)"), ps[:, 0:384], [Bps], [self.Bmtab])
        S.barrier()
        self.aoff = mixer_base


        cosr, sinr = self.tabs[:, 0], self.tabs[:, 1]
        cosp, sinp = self.tabs[:, 2], self.tabs[:, 3]
        sq_r = self.small[:, 0:8]
        sk_r = self.small[:, 8:16]

        def evac(nm, j, i, ps, Bps, k):
            eng = "dve" if i % 2 == 0 else "pool"
            dst = self.z[nm]
            rows = slice(i * 128, (i + 1) * 128)
            cols = slice(j * 512, (j + 1) * 512)
            if nm in ("rv", "mv", "av"):
                self.cp("act", stgb[k], ps[:], [Bps], [Bstgb[k]])
                out_t, Bout = stgb[k], Bstgb[k]
            elif nm in ("rq", "rk"):
                sc = sq_r if nm == "rq" else sk_r
                for hh in range(4):
                    h = j * 4 + hh
                    self.S.op("act", lambda e, hh=hh, h=h, sc=sc: e.activation(out=stg[k][:, hh * 128:(hh + 1) * 128],
                                                                             in_=ps[:, hh * 128:(hh + 1) * 128], func=AF.Copy,
                                                                             scale=sc[:, h:h + 1]),
                              [Bps, self.Bconst], [Bstg[k]])
                self.rot4(eng, stgb[k], stg[k], cosr[:, i, :], sinr[:, i, :], rt[eng], Brt[eng], [self.Bconst], Bstg[k], Bstgb[k], 4)
                out_t, Bout = stgb[k], Bstgb[k]
            elif nm in ("rg", "mo"):
                fn = AF.Silu if nm == "rg" else AF.Sigmoid
                gw = self.retgn if nm == "rg" else self.mlnorm
                self.act(stg[k], ps[:], fn, [Bps], [Bstg[k]])
                self.tt(eng, stgb[k], stg[k], gw[:, cols], ALU.mult, [Bstg[k], self.Blayer], [Bstgb[k]])
                out_t, Bout = stgb[k], Bstgb[k]
            elif nm in ("mq", "mk"):
                tab = self.mtab[:, 0 if nm == "mq" else 1, i, :]
                tb_ = tab.unsqueeze(2).broadcast_to([128, 8, 64])
                v8 = lambda ap: ap.rearrange("p (h d) -> p h d", h=8)
                if i % 2 == 0:
                    self.tt("dve", v8(stgb[k]), v8(ps[:]), tb_, ALU.mult, [Bps, self.Bmtab], [Bstgb[k]])
                else:
                    self.cp("act", stg[k], ps[:], [Bps], [Bstg[k]])
                    self.tt("pool", v8(stgb[k]), v8(stg[k]), tb_, ALU.mult, [Bstg[k], self.Bmtab], [Bstgb[k]])
                out_t, Bout = stgb[k], Bstgb[k]
            else:
                gw = self.qnw if nm == "aq" else self.knw
                v4_ = lambda ap: ap.rearrange("p (h d) -> p h d", h=4)
                k8 = (j * NT + i) % 8
                self.act(stg2[k8], ps[:], AF.Square, [Bps], [Bstg2[k8]])
                self.cp("act", stg3[k8], ps[:], [Bps], [Bstg3[k8]])
                self.red(r8[k8], v4_(stg2[k8]), ALU.add, [Bstg2[k8]], [Br8[k8]])
                self.act(r8[k8], r8[k8], AF.Sqrt, [Br8[k8]], [Br8[k8]], scale=1.0 / 128.0, bias=1e-6)
                self.recip(r8[k8], r8[k8], [Br8[k8]], [Br8[k8]])
                self.tt(eng, v4_(stg3[k8]), v4_(stg3[k8]), r8[k8].unsqueeze(2).broadcast_to([128, 4, 128]), ALU.mult,
                        [Bstg3[k8], Br8[k8]], [Bstg3[k8]])
                self.tt(eng, v4_(stg3[k8]), v4_(stg3[k8]), gw[:].unsqueeze(1).broadcast_to([128, 4, 128]), ALU.mult,
                        [Bstg3[k8], self.Blayer], [Bstg3[k8]])
                self.rot4(eng, stg4[k8], stg3[k8], cosp[:, i, :], sinp[:, i, :], rt[eng], Brt[eng], [self.Bconst], Bstg3[k8], Bstg4[k8], 4)
                out_t, Bout = stg4[k8], Bstg4[k8]
                S.dma("sp", dst[rows, cols], out_t, [Bout], [self.Bz[nm][i][j]], self.ds[28 + k8])
                return
            S.dma("sp", dst[rows, cols], out_t, [Bout], [self.Bz[nm][i][j]], self.ds[28 + k])

        gen = None
        cur_seg = 0
        stepno = 0
        stride = {0: 1, 1: 1, 2: 2, 3: 1}

        def step(force=False):
            nonlocal gen, stepno
            stepno += 1
            if gen is not None and (force or stepno % stride[cur_seg] == 0):
                try:
                    next(gen)
                except StopIteration:
                    gen = None

        def drain():
            nonlocal gen
            while gen is not None:
                step(True)

        for bi, (kind, nm, c0, j) in enumerate(blocks):
            if seg_of[bi] != cur_seg:
                drain()
                cur_seg = seg_of[bi]
                S.barrier()
                self.aoff = mixer_base
                if cur_seg in (1, 2):
                    gen = self.ret_gen(l) if cur_seg == 1 else self.mlstm_gen(l)
                else:
                    stg2 = [self.alloc([128, 512], F32) for _ in range(8)]
                    stg3 = [self.alloc([128, 512], F32) for _ in range(8)]
                    stg4 = [self.alloc([128, 512], F32) for _ in range(8)]
            if bi + 1 < nblk and bi >= 1:
                load(bi + 1)
            s_ = bi % NW
            if kind == "tm":
                for i in range(NT):
                    k = self.pcnt % (4 if cur_seg in (0, 3) else 2)
                    self.pcnt += 1
                    ps = bank[k]
                    Bps = Bbank[k]
                    for c in range(16):
                        self.mm(ps[:], hT[:, c, i * 128:(i + 1) * 128], Wt[s_][:, c, :], c == 0, c == 15,
                                [BhT[c][i // 2], BW[s_]], [Bps], inc=(c == 15))
                    evac(nm, j, i, ps, Bps, k)
                    step()
            else:
                for nn in range(4):
                    for tb in range(4):
                        k = self.pcnt % 2
                        self.pcnt += 1
                        ps = bank[k]
                        Bps = Bbank[k]
                        for c in range(16):
                            self.mm(ps[:], Wt[s_][:, c, nn * 128:(nn + 1) * 128], hT[:, c, tb * 512:(tb + 1) * 512],
                                    c == 0, c == 15, [BhT[c][2 * tb], BhT[c][2 * tb + 1], BW[s_]], [Bps], inc=(c == 15))
                        self.act(stgb[k], ps[:], AF.Sigmoid, [Bps], [Bstgb[k]])
                        row = (j * 4 + nn) * 128
                        S.dma("sp", self.sgT[row:row + 128, tb * 512:(tb + 1) * 512], stgb[k], [Bstgb[k]],
                              [self.Bsg[j * 4 + nn][tb]], self.ds[32 + k])
                        step()
        drain()

    def bcast_h(self, ap8, d):
        return ap8.unsqueeze(2).broadcast_to([ap8.shape[0], 8, d])

    def ret_gen(self, l):
        S = self.S
        bank, Bbank = self.bank, self.Bbank
        NL = 3
        ld = []
        for b in range(NL):
            ld.append(dict(q=self.alloc([128, 1024], BF16), k=self.alloc([128, 1024], BF16),
                           g=self.alloc([128, 1024], BF16), v=self.alloc([128, 1024], BF16),
                           Bq=Buf(), Bk=Buf(), Bg=Buf(), Bv=Buf()))
        qT = [self.alloc([128, 1024], BF16) for _ in range(2)]
        kT = [self.alloc([128, 1024], BF16) for _ in range(2)]
        BqT, BkT = self.bufs(2), self.bufs(2)
        sTm = [self.alloc([128, 1024], BF16) for _ in range(2)]
        BsTm = [self.bufs(2) for _ in range(2)]
        R32 = self.alloc([128, 1024], F32)
        Rbf = self.alloc([128, 1024], BF16)
        Aa = self.alloc([128, 1024], F32)
        BR32, BRbf, BA = Buf(), Buf(), Buf()
        osb = self.alloc([128, 1024], F32)
        sqt = self.alloc([128, 1024], F32)
        cc = sqt
        Bosb, Bsqt, Bcc = self.bufs(3)
        yb = [self.alloc([128, 1024], BF16) for _ in range(2)]
        Byb = self.bufs(2)
        yTs = [self.alloc([128, 8, 128], BF16) for _ in range(2)]
        ByTs = self.bufs(2)
        st8 = [self.alloc([128, 8], F32) for _ in range(6)]
        Bst = self.bufs(6)
        gC = self.small[:, 16:24]
        zq, zk, zv, zg = self.z["rq"], self.z["rk"], self.z["rv"], self.z["rg"]
        Bz = self.Bz
        yTa = self.yT[0].rearrange("(h e) t -> e h t", e=128)
        v4 = lambda ap: ap.rearrange("p (h d) -> p h d", h=8)
        maskb4 = self.maskb[:].unsqueeze(1).broadcast_to([128, 4, 128])
        pq = bank[2][:].bitcast(BF16)
        pk = bank[3][:].bitcast(BF16)

        def loads(n):
            b = n % NL
            L_ = ld[b]
            rs = slice(n * 128, (n + 1) * 128)
            S.dma("sp", L_["q"], zq[rs, :], Bz["rq"][n], [L_["Bq"]], self.ds[4 + b])
            S.dma("sp", L_["k"], zk[rs, :], Bz["rk"][n], [L_["Bk"]], self.ds[7 + b])
            S.dma("sp", L_["g"], zg[rs, :], Bz["rg"][n], [L_["Bg"]], self.ds[10 + b])
            S.dma("sp", L_["v"], zv[rs, :], Bz["rv"][n], [L_["Bv"]], self.ds[13 + b])

        def front(n):
            L_ = ld[n % NL]
            s_ = n % 2
            for h in range(8):
                self.tr(pq[:, h * 128:(h + 1) * 128], L_["q"][:, h * 128:(h + 1) * 128], self.identb[:], [L_["Bq"], self.Bconst], [Bbank[2]], inc=(h == 7))
            for h in range(8):
                self.tr(pk[:, h * 128:(h + 1) * 128], L_["k"][:, h * 128:(h + 1) * 128], self.identb[:], [L_["Bk"], self.Bconst], [Bbank[3]], inc=(h == 7))
            self.cp("act", qT[s_], pq, [Bbank[2]], [BqT[s_]])
            self.cp("dve", kT[s_], pk, [Bbank[3]], [BkT[s_]])
            for h in range(8):
                hb = 4 + h // 4
                self.mm(bank[hb][:, (h % 4) * 128:(h % 4 + 1) * 128], kT[s_][:, h * 128:(h + 1) * 128], qT[s_][:, h * 128:(h + 1) * 128],
                        True, True, [BkT[s_], BqT[s_]], [Bbank[hb]], inc=(h % 4 == 3))
            for hb in range(2):
                self.tt("dve", sTm[s_][:, hb * 512:(hb + 1) * 512].rearrange("p (h d) -> p h d", h=4),
                        bank[4 + hb][:].rearrange("p (h d) -> p h d", h=4), maskb4, ALU.mult,
                        [Bbank[4 + hb], self.Bconst], [BsTm[s_][hb]])

        def mid(n):
            L_ = ld[n % NL]
            s_ = n % 2
            for h in range(8):
                hb = 6 + h // 4
                osl = bank[hb][:, (h % 4) * 128:(h % 4 + 1) * 128]
                hs = slice(h * 128, (h + 1) * 128)
                self.mm(osl, sTm[s_][:, hs], L_["v"][:, hs], True, n == 0, [BsTm[s_][h // 4], L_["Bv"]], [Bbank[hb]],
                        inc=(n == 0 and h % 4 == 3))
                if n > 0:
                    self.mm(osl, qT[s_][:, hs], Rbf[:, hs], False, True, [BqT[s_], BRbf], [Bbank[hb]], inc=(h % 4 == 3))
            if n + 1 < NT:
                for h in range(8):
                    hb = 4 + h // 4
                    hs = slice(h * 128, (h + 1) * 128)
                    self.mm(bank[hb][:, (h % 4) * 128:(h % 4 + 1) * 128], L_["k"][:, hs], L_["v"][:, hs], True, True,
                            [L_["Bk"], L_["Bv"]], [Bbank[hb]], inc=(h % 4 == 3))
                for hb in range(2):
                    hsl = slice(hb * 512, (hb + 1) * 512)
                    if n == 0:
                        self.cp("dve", Aa[:, hsl], bank[4 + hb][:], [Bbank[4 + hb]], [BA])
                    else:
                        self.tt("dve", Aa[:, hsl], bank[4 + hb][:], R32[:, hsl], ALU.add, [Bbank[4 + hb], BR32], [BA])
                self.tt("pool", v4(R32), v4(Aa), self.bcast_h(gC, 128), ALU.mult, [BA, self.Bconst], [BR32])
                self.cp("act", Rbf, R32, [BR32], [BRbf])

        def epi(n):
            L_ = ld[n % NL]
            for hb in range(2):
                self.cp("act", osb[:, hb * 512:(hb + 1) * 512], bank[6 + hb][:], [Bbank[6 + hb]], [Bosb])
            self.act(sqt, osb, AF.Square, [Bosb], [Bsqt])
            s1, s2, mean, msq, var, rstd = st8
            self.red(s1, v4(osb), ALU.add, [Bosb], [Bst[0]])
            self.red(s2, v4(sqt), ALU.add, [Bsqt], [Bst[1]])
            self.ts("dve", mean, s1, 1.0 / 128.0, None, ALU.mult, None, [Bst[0]], [Bst[2]])
            self.tt("dve", msq, mean, mean, ALU.mult, [Bst[2]], [Bst[3]])
            self.stt(var, s2, 1.0 / 128.0, msq, ALU.mult, ALU.subtract, [Bst[1], Bst[3]], [Bst[4]])
            self.act(rstd, var, AF.Sqrt, [Bst[4]], [Bst[5]], bias=1e-5)
            self.recip(rstd, rstd, [Bst[5]], [Bst[5]])
            self.tt("pool", v4(cc), v4(osb), self.bcast_h(mean, 128), ALU.subtract, [Bosb, Bst[2]], [Bcc])
            self.tt("pool", v4(cc), v4(cc), self.bcast_h(rstd, 128), ALU.mult, [Bcc, Bst[5]], [Bcc])
            self.tt("dve", yb[n % 2], cc, L_["g"], ALU.mult, [Bcc, L_["Bg"]], [Byb[n % 2]])

        def outT(n):
            for h in range(8):
                self.tr(pq[:, h * 128:(h + 1) * 128], yb[n % 2][:, h * 128:(h + 1) * 128], self.identb[:], [Byb[n % 2], self.Bconst], [Bbank[2]], inc=(h == 7))
            ys = yTs[n % 2]
            self.cp("act", ys.rearrange("p h t -> p (h t)"), pq, [Bbank[2]], [ByTs[n % 2]])
            S.dma("sp", yTa[:, :, n * 128:(n + 1) * 128], ys, [ByTs[n % 2]], [self.ByT[0][n]], self.ds[16 + n % 2])

        loads(0)
        loads(1)
        front(0)
        yield
        for n in range(NT):
            if n + 2 < NT:
                loads(n + 2)
            mid(n)
            yield
            if n + 1 < NT:
                front(n + 1)
                yield
            epi(n)
            yield
            if n > 0:
                outT(n - 1)
                yield
        outT(NT - 1)
        yield

    def mlstm_gen(self, l):
        S = self.S
        bank, Bbank = self.bank, self.Bbank
        NL = 3
        ld = []
        for b in range(NL):
            ld.append(dict(q=self.alloc([128, 512], BF16), k=self.alloc([128, 512], BF16),
                           o=self.alloc([128, 1024], BF16), v=self.alloc([128, 1024], BF16),
                           Bq=Buf(), Bk=Buf(), Bo=Buf(), Bv=Buf()))
        qT = [self.alloc([64, 1024], BF16) for _ in range(2)]
        kT = [self.alloc([64, 1024], BF16) for _ in range(2)]
        BqT, BkT = self.bufs(2), self.bufs(2)
        sTm = [self.alloc([128, 1024], BF16) for _ in range(2)]
        BsTm = [self.bufs(2) for _ in range(2)]
        C32 = self.alloc([64, 1024], F32)
        Cbf = self.alloc([64, 1024], BF16)
        Aa = self.alloc([64, 1024], F32)
        n32 = self.alloc([64, 8], F32)
        nbf = self.alloc([64, 8], BF16)
        An = self.alloc([64, 8], F32)
        BC32, BCbf, BA, Bn32, Bnbf, BAn = self.bufs(6)
        hv = self.alloc([128, 1024], F32)
        sqt = self.alloc([128, 1024], F32)
        Bhv, Bsqt = self.bufs(2)
        yb = [self.alloc([128, 1024], BF16) for _ in range(2)]
        Byb = self.bufs(2)
        yTs = [self.alloc([128, 8, 128], BF16) for _ in range(2)]
        ByTs = self.bufs(2)
        st8 = [self.alloc([128, 8], F32) for _ in range(3)]
        Bst = self.bufs(3)
        zq, zk, zv, zo = self.z["mq"], self.z["mk"], self.z["mv"], self.z["mo"]
        Bz = self.Bz
        yTb = self.yT[1].rearrange("(h e) t -> e h t", e=128)
        v4 = lambda ap: ap.rearrange("p (h d) -> p h d", h=8)
        maskb4 = self.maskb[:].unsqueeze(1).broadcast_to([128, 4, 128])
        mtab = self.mtab
        pq = bank[2][:].bitcast(BF16)
        pk = pq
        pD = bank[7]
        BKn = Buf()

        def loads(n):
            b = n % NL
            L_ = ld[b]
            rs = slice(n * 128, (n + 1) * 128)
            S.dma("sp", L_["q"], zq[rs, :], [Bz["mq"][n][0]], [L_["Bq"]], self.ds[36 + b])
            S.dma("sp", L_["k"], zk[rs, :], [Bz["mk"][n][0]], [L_["Bk"]], self.ds[18 + b])
            S.dma("sp", L_["o"], zo[rs, :], Bz["mo"][n], [L_["Bo"]], self.ds[21 + b])
            S.dma("sp", L_["v"], zv[rs, :], Bz["mv"][n], [L_["Bv"]], self.ds[24 + b])

        def front(n):
            L_ = ld[n % NL]
            s_ = n % 2
            for h in range(8):
                self.tr(pq[0:64, h * 128:(h + 1) * 128], L_["q"][:, h * 64:(h + 1) * 64], self.identb[:], [L_["Bq"], self.Bconst], [Bbank[2]], inc=(h == 7))
            self.cp("act", qT[s_], pq[0:64, :], [Bbank[2]], [BqT[s_]])
            for h in range(8):
                self.tr(pk[0:64, h * 128:(h + 1) * 128], L_["k"][:, h * 64:(h + 1) * 64], self.identb[:], [L_["Bk"], self.Bconst], [Bbank[2]], inc=(h == 7))
            self.cp("dve", kT[s_], pk[0:64, :], [Bbank[2]], [BkT[s_]])
            for h in range(8):
                hb = 3 + h // 4
                hs = slice(h * 128, (h + 1) * 128)
                self.mm(bank[hb][:, (h % 4) * 128:(h % 4 + 1) * 128], kT[s_][:, hs], qT[s_][:, hs], True, True, [BkT[s_], BqT[s_]], [Bbank[hb]], inc=(h % 4 == 3))
            for hb in range(2):
                self.tt("dve", sTm[s_][:, hb * 512:(hb + 1) * 512].rearrange("p (h d) -> p h d", h=4),
                        bank[3 + hb][:].rearrange("p (h d) -> p h d", h=4), maskb4, ALU.mult,
                        [Bbank[3 + hb], self.Bconst], [BsTm[s_][hb]])

        def mid(n):
            L_ = ld[n % NL]
            s_ = n % 2
            eal = mtab[0:64, 2, n, :]
            for h in range(8):
                hb = 5 + h // 4
                osl = bank[hb][:, (h % 4) * 128:(h % 4 + 1) * 128]
                hs = slice(h * 128, (h + 1) * 128)
                self.mm(osl, sTm[s_][:, hs], L_["v"][:, hs], True, n == 0, [BsTm[s_][h // 4], L_["Bv"]], [Bbank[hb]], inc=False)
                if n > 0:
                    self.mm(osl, qT[s_][:, hs], Cbf[:, hs], False, True, [BqT[s_], BCbf], [Bbank[hb]], inc=False)
                self.mm(pD[:, h:h + 1], sTm[s_][:, hs], self.onescb[:, 0:1], True, n == 0, [BsTm[s_][h // 4], self.Bconst], [Bbank[7]],
                        inc=(n == 0 and h == 7))
                if n > 0:
                    self.mm(pD[:, h:h + 1], qT[s_][:, hs], nbf[:, h:h + 1], False, True, [BqT[s_], Bnbf], [Bbank[7]], inc=(h == 7))
            dm, rden, rstd = st8
            self.act(dm, pD[:, 0:8], AF.Abs, [Bbank[7]], [Bst[0]])
            self.ts("dve", dm, dm, 1.0, None, ALU.max, None, [Bst[0]], [Bst[0]])
            self.recip(rden, dm, [Bst[0]], [Bst[1]])
            for hb in range(2):
                self.tt("dve", hv[:, hb * 512:(hb + 1) * 512].rearrange("p (h d) -> p h d", h=4),
                        bank[5 + hb][:].rearrange("p (h d) -> p h d", h=4),
                        rden[:, hb * 4:(hb + 1) * 4].unsqueeze(2).broadcast_to([128, 4, 128]), ALU.mult,
                        [Bbank[5 + hb], Bbank[7], Bst[1]], [Bhv])
            if n + 1 < NT:
                pKn = bank[7]
                for h in range(8):
                    hb = 3 + h // 4
                    hs = slice(h * 128, (h + 1) * 128)
                    self.mm(bank[hb][0:64, (h % 4) * 128:(h % 4 + 1) * 128], L_["k"][:, h * 64:(h + 1) * 64], L_["v"][:, hs], True, True,
                            [L_["Bk"], L_["Bv"]], [Bbank[hb]], inc=(h % 4 == 3))
                for h in range(8):
                    self.mm(pKn[0:64, 8 + h:9 + h], L_["k"][:, h * 64:(h + 1) * 64], self.onescb[:, 0:1], True, True,
                            [L_["Bk"], self.Bconst], [BKn], inc=(h == 7))
                ealb = eal.unsqueeze(2).broadcast_to([64, 8, 128])
                for hb in range(2):
                    hsl = slice(hb * 512, (hb + 1) * 512)
                    if n == 0:
                        self.cp("dve", Aa[:, hsl], bank[3 + hb][0:64, :], [Bbank[3 + hb]], [BA])
                    else:
                        self.tt("dve", Aa[:, hsl], bank[3 + hb][0:64, :], C32[:, hsl], ALU.add, [Bbank[3 + hb], BC32], [BA])
                self.tt("pool", v4(C32), v4(Aa), ealb, ALU.mult, [BA, self.Bmtab], [BC32])
                self.cp("act", Cbf, C32, [BC32], [BCbf])
                if n == 0:
                    self.cp("dve", An, pKn[0:64, 8:16], [BKn], [BAn])
                else:
                    self.tt("dve", An, pKn[0:64, 8:16], n32, ALU.add, [BKn, Bn32], [BAn])
                self.tt("dve", n32, An, eal, ALU.mult, [BAn, self.Bmtab], [Bn32])
                self.cp("dve", nbf, n32, [Bn32], [Bnbf])

        def epi(n):
            L_ = ld[n % NL]
            dm, rden, rstd = st8
            self.act(sqt, hv, AF.Square, [Bhv], [Bsqt])
            self.red(rstd, v4(sqt), ALU.add, [Bsqt], [Bst[2]])
            self.act(rstd, rstd, AF.Sqrt, [Bst[2]], [Bst[2]], scale=1.0 / 128.0, bias=1e-6)
            self.recip(rstd, rstd, [Bst[2]], [Bst[2]])
            self.tt("pool", v4(hv), v4(hv), self.bcast_h(rstd, 128), ALU.mult, [Bhv, Bst[2]], [Bhv])
            self.tt("dve", yb[n % 2], hv, L_["o"], ALU.mult, [Bhv, L_["Bo"]], [Byb[n % 2]])

        def outT(n):
            for h in range(8):
                self.tr(pq[:, h * 128:(h + 1) * 128], yb[n % 2][:, h * 128:(h + 1) * 128], self.identb[:], [Byb[n % 2], self.Bconst], [Bbank[2]], inc=(h == 7))
            ys = yTs[n % 2]
            self.cp("act", ys.rearrange("p h t -> p (h t)"), pq, [Bbank[2]], [ByTs[n % 2]])
            S.dma("sp", yTb[:, :, n * 128:(n + 1) * 128], ys, [ByTs[n % 2]], [self.ByT[1][n]], self.ds[2 + n % 2])

        loads(0)
        loads(1)
        front(0)
        yield
        for n in range(NT):
            if n + 2 < NT:
                loads(n + 2)
            mid(n)
            yield
            if n + 1 < NT:
                front(n + 1)
                yield
            epi(n)
            yield
            if n > 0:
                outT(n - 1)
                yield
        outT(NT - 1)
        yield

    def phase_moba(self, l):
        S = self.S
        self.arena_reset()
        bank, Bbank = self.bank, self.Bbank
        qT_all = self.alloc([128, 8, SEQ], BF16)
        kT_all = self.alloc([128, 8, SEQ], BF16)
        v_all = self.alloc([128, NT, 1024], BF16)
        sel_all = self.alloc([128, NT, 8, 8], F32)
        BqTa = [Buf() for _ in range(NT)]
        BkTa = [Buf() for _ in range(NT)]
        Bva = [Buf() for _ in range(NT)]
        Bsel = [Buf() for _ in range(NT)]
        ksum = self.alloc([128, 8, NT], F32)
        kmean = self.alloc([128, 8, 8], F32)
        Bksum, Bkmean = Buf(), Buf()
        nbT = self.alloc([128, SEQ], BF16)
        BnbT = [Buf() for _ in range(NT)]
        Esel = self.alloc([128, 64, 128], BF16)
        BEsel = Buf()
        mark = self.aoff
        NL = 3
        ld = []
        for b in range(NL):
            ld.append(dict(q=self.alloc([128, 1024], F32), k=self.alloc([128, 1024], F32), Bq=Buf(), Bk=Buf()))
        nbt = [self.alloc([128, 64], F32) for _ in range(2)]
        Bnbt = self.bufs(2)
        self.cp("pool", Esel, self.identf[:, 0:64].unsqueeze(2).broadcast_to([128, 64, 128]), [self.Bconst], [BEsel])
        self.memset("pool", nbT, 0.0, [], BnbT)
        qT32 = [self.alloc([128, 1024], F32) for _ in range(2)]
        BqT32 = self.bufs(2)
        gm = self.alloc([128, 8, 8], F32)
        mx = self.alloc([128, 8, 8], F32)
        Bgm, Bmx = Buf(), Buf()
        zq, zk, zv = self.z["aq"], self.z["ak"], self.z["av"]
        Bz = self.Bz
        v4 = lambda ap: ap.rearrange("p (h d) -> p h d", h=8)
        self.memset("dve", kmean, 0.0, [], [Bkmean])
        self.memset("dve", sel_all, 0.0, [], Bsel)

        def loads(i):
            b = i % NL
            L_ = ld[b]
            rs = slice(i * 128, (i + 1) * 128)
            S.dma("sp", L_["q"], zq[rs, :], Bz["aq"][i], [L_["Bq"]], self.ds[0 + b])
            S.dma("sp", L_["k"], zk[rs, :], Bz["ak"][i], [L_["Bk"]], self.ds[3 + b])
            S.dma("sp", v_all[:, i, :], zv[rs, :], Bz["av"][i], [Bva[i]], self.ds[6 + b])

        loads(0)
        loads(1)
        for i in range(NT):
            if i + 2 < NT:
                loads(i + 2)
            L_ = ld[i % NL]
            qr, kr, Bqr, Bkr = L_["q"], L_["k"], L_["Bq"], L_["Bk"]
            q32 = qT32[i % 2]
            Bq32 = BqT32[i % 2]
            bq = i // 2
            for h in range(8):
                hb = h // 4
                self.tr(bank[hb][:, (h % 4) * 128:(h % 4 + 1) * 128], qr[:, h * 128:(h + 1) * 128], self.identf[:],
                        [Bqr, self.Bconst], [Bbank[hb]], inc=(h % 4 == 3))
            for h in range(8):
                hb = 2 + h // 4
                self.tr(bank[hb][:, (h % 4) * 128:(h % 4 + 1) * 128], kr[:, h * 128:(h + 1) * 128], self.identf[:],
                        [Bkr, self.Bconst], [Bbank[hb]], inc=(h % 4 == 3))
            for hb in range(2):
                self.cp("act", q32[:, hb * 512:(hb + 1) * 512], bank[hb][:], [Bbank[hb]], [Bq32])
                self.cp("dve", kT_all[:, hb * 4:(hb + 1) * 4, i * 128:(i + 1) * 128],
                        bank[2 + hb][:].rearrange("p (h t) -> p h t", h=4), [Bbank[2 + hb]], [BkTa[i]])
            self.cp("pool", qT_all[:, :, i * 128:(i + 1) * 128], v4(q32), [Bq32], [BqTa[i]])
            pks = bank[4 + (i % 2) * 2]
            Bpks = Bbank[4 + (i % 2) * 2]
            for h in range(8):
                self.mm(pks[:, h:h + 1], kr[:, h * 128:(h + 1) * 128], self.kmcol[:, 0:1], True, True, [Bkr, self.Bconst], [Bpks], inc=(h == 7))
            self.cp("act", ksum[:, :, i], pks[:, 0:8], [Bpks], [Bksum])
            if bq >= 1:
                pg = bank[5 + (i % 2) * 2]
                Bpg = Bbank[5 + (i % 2) * 2]
                for h in range(8):
                    self.mm(pg[:, h * 8:(h + 1) * 8], q32[:, h * 128:(h + 1) * 128], kmean[:, h, :], True, True,
                            [Bq32, Bkmean], [Bpg], inc=(h == 7))
                self.tt("dve", gm, pg[:, 0:64].rearrange("p (h n) -> p h n", h=8),
                        self.negm[:, bq, :].unsqueeze(1).broadcast_to([128, 8, 8]), ALU.add, [Bpg, self.Bconst], [Bgm])
                for h in range(8):
                    self.S.op("dve", lambda e, h=h: e.max(out=mx[:, h, :], in_=gm[:, h, :]), [Bgm], [Bmx])
                self.tt("dve", gm, gm, mx[:, :, 2:3].broadcast_to([128, 8, 8]), ALU.is_ge, [Bgm, Bmx], [Bgm])
                self.tt("dve", gm, gm, self.selfix[:, 0, bq, :].unsqueeze(1).broadcast_to([128, 8, 8]), ALU.mult, [Bgm, self.Bconst], [Bgm])
                self.tt("dve", sel_all[:, i], gm, self.selfix[:, 1, bq, :].unsqueeze(1).broadcast_to([128, 8, 8]), ALU.add,
                        [Bgm, self.Bconst], [Bsel[i]])
            else:
                self.cp("dve", sel_all[:, i], self.selfix[:, 1, 0:1, :].broadcast_to([128, 8, 8]), [self.Bconst], [Bsel[i]])
            if i % 2 == 1:
                self.tt("dve", kmean[:, :, bq], ksum[:, :, i - 1], ksum[:, :, i], ALU.add, [Bksum], [Bkmean])
            self.ts("dve", nbt[i % 2], sel_all[:, i].rearrange("p h n -> p (h n)"), -1.0, 30000.0, ALU.add, ALU.mult, [Bsel[i]], [Bnbt[i % 2]])
            self.tr(pks[0:64, 128:256], nbt[i % 2], self.identf[:], [Bnbt[i % 2], self.Bconst], [Bpks], inc=True)
            self.cp("act", nbT[0:64, i * 128:(i + 1) * 128], pks[0:64, 128:256], [Bpks], [BnbT[i]])
        S.barrier()
        self.aoff = mark
        NPT = 4
        LA = 2
        PT = [self.alloc([128, 512], BF16) for _ in range(NPT)]
        BPT = self.bufs(NPT)
        rd = [self.alloc([128, 512], F32) for _ in range(2)]
        Brd = self.bufs(2)
        dacc = [self.alloc([128, 512], F32) for _ in range(2)]
        Bdacc = self.bufs(2)
        yst = [self.alloc([128, 512], BF16) for _ in range(2)]
        Byst = self.bufs(2)
        items = []
        for h in range(8):
            for Q in range(4):
                for jt in range(4 * Q + 4):
                    items.append((h, Q, jt))

        def geom(Q, jt):
            c0 = 128 * max(0, jt - 4 * Q)
            return c0, 512 - c0, 4 * Q * 128 + c0, (4 * Q + 4) * 128

        def qk(idx):
            h, Q, jt = items[idx]
            n = jt // 2
            c0, N, qlo, qhi = geom(Q, jt)
            need_bias = n <= 2 * Q
            pS = bank[idx % 4]
            BpS = Bbank[idx % 4]
            pt = PT[idx % NPT]
            Bpt = BPT[idx % NPT]
            qdeps = [BqTa[x] for x in range(qlo // 128, qhi // 128)]
            self.mm(pS[:, 0:N], kT_all[:, h, jt * 128:(jt + 1) * 128], qT_all[:, h, qlo:qhi], True, not need_bias,
                    [BkTa[jt]] + qdeps, [BpS], inc=not need_bias)
            if need_bias:
                self.mm(pS[:, 0:N], Esel[:, h * 8 + n, :], nbT[:, qlo:qhi], False, True,
                        [BEsel] + [BnbT[x] for x in range(qlo // 128, qhi // 128)], [BpS], inc=True)
            self.act(pt[:, 0:N], pS[:, 0:N], AF.Exp, [BpS], [Bpt])
            if jt >= 4 * Q:
                self.tt("pool", pt[:, 0:128], pt[:, 0:128], self.maskb[:], ALU.mult, [Bpt, self.Bconst], [Bpt])

        def pv(idx):
            h, Q, jt = items[idx]
            gi = h * 4 + Q
            g2 = gi % 2
            hs = slice(h * 128, (h + 1) * 128)
            c0, N, qlo, qhi = geom(Q, jt)
            ntile = 4 * Q + 4
            pO, BpO = bank[4 + g2], Bbank[4 + g2]
            pDn, BpDn = bank[6 + g2], Bbank[6 + g2]
            pt = PT[idx % NPT]
            Bpt = BPT[idx % NPT]
            self.mm(pO[:, c0:512], v_all[:, jt, hs], pt[:, 0:N], jt == 0, jt == ntile - 1, [Bva[jt], Bpt], [BpO], inc=False)
            self.mm(pDn[:, c0:512], self.onesb[:], pt[:, 0:N], jt == 0, jt == ntile - 1, [self.Bconst, Bpt], [BpDn], inc=True)
            if jt == ntile - 1:
                self.recip(rd[g2], pDn[:], [BpDn], [Brd[g2]])
                self.tt("dve", yst[g2], pO[:], rd[g2], ALU.mult, [BpO, BpDn, Brd[g2]], [Byst[g2]])
                S.dma("sp", self.yT[2][h * 128:(h + 1) * 128, Q * 512:(Q + 1) * 512], yst[g2], [Byst[g2]],
                      [self.ByT[2][h * 4 + Q]], self.ds[12 + g2])

        for idx in range(len(items) + LA):
            if idx < len(items):
                qk(idx)
            if idx >= LA:
                pv(idx - LA)

    def phase_merge(self, l, xsrc):
        S = self.S
        self.arena_reset()
        bank, Bbank = self.bank, self.Bbank
        mT = self.alloc([128, 16, SEQ], BF16)
        BmT = [[Buf() for _ in range(4)] for _ in range(16)]
        yTb = [self.alloc([128, 8, SEQ], BF16) for _ in range(2)]
        ByTb = [self.bufs(4) for _ in range(2)]
        Wp = [self.alloc([128, 8, 128], BF16) for _ in range(4)]
        BWp = self.bufs(4)
        sgt = [self.alloc([128, 512], BF16) for _ in range(4)]
        Bsgt = self.bufs(4)
        prod = [self.alloc([128, 512], F32) for _ in range(2)]
        Bprod = self.bufs(2)
        Wo = [self.alloc([128, 16, 128], BF16) for _ in range(3)]
        BWo = self.bufs(3)
        xt = [self.alloc([128, 512], F32) for _ in range(4)]
        Bxt = self.bufs(4)
        ost = [self.alloc([128, 512], F32) for _ in range(4)]
        Bost = self.bufs(4)
        wps = [self.w_pa, self.w_pb, self.w_pc]

        def loady(b):
            yv = self.yT[b].rearrange("(c p) t -> p c t", p=128)
            for tb in range(4):
                if b < 2:
                    rd = [self.ByT[b][4 * tb + x] for x in range(4)]
                else:
                    rd = [self.ByT[2][h * 4 + tb] for h in range(8)]
                S.dma("sp", yTb[b % 2][:, :, tb * 512:(tb + 1) * 512], yv[:, :, tb * 512:(tb + 1) * 512], rd, [ByTb[b % 2][tb]], self.ds[0 + (b % 2) * 4 + tb])

        items = [(b, ncx, tb) for b in range(3) for ncx in range(16) for tb in range(4)]

        def loadw(b, ncx):
            wv = wps[b][l].rearrange("(c p) n -> p c n", p=128)
            s_ = (b * 16 + ncx) % 4
            S.dma("pool", Wp[s_], wv[:, :, ncx * 128:(ncx + 1) * 128], [], [BWp[s_]], self.ds[8 + s_])

        def loadsg(ii):
            b, ncx, tb = items[ii]
            k = ii % 4
            S.dma("sp", sgt[k], self.sgT[(b * 16 + ncx) * 128:(b * 16 + ncx + 1) * 128, tb * 512:(tb + 1) * 512],
                  [self.Bsg[b * 16 + ncx][tb]], [Bsgt[k]], self.ds[12 + k])

        wlist = [(b, ncx) for b in range(3) for ncx in range(16)]
        loady(0)
        loady(1)
        loadw(*wlist[0])
        loadw(*wlist[1])
        loadsg(0)
        loadsg(1)
        for ii, (b, ncx, tb) in enumerate(items):
            wi = b * 16 + ncx
            if tb == 0 and wi + 2 < len(wlist):
                loadw(*wlist[wi + 2])
            if ii + 2 < len(items):
                loadsg(ii + 2)
            if b == 1 and ncx == 0 and tb == 0:
                loady(2)
            s_ = wi % 4
            k = ii % 4
            ps = bank[k]
            Bps = Bbank[k]
            for c in range(8):
                self.mm(ps[:], Wp[s_][:, c, :], yTb[b % 2][:, c, tb * 512:(tb + 1) * 512], c == 0, c == 7,
                        [BWp[s_], ByTb[b % 2][tb]], [Bps], inc=(c == 7))
            msl = mT[:, ncx, tb * 512:(tb + 1) * 512]
            if b == 0:
                self.tt("dve", msl, ps[:], sgt[k], ALU.mult, [Bps, Bsgt[k]], [BmT[ncx][tb]])
            else:
                pr = prod[k % 2]
                self.tt("dve", pr, ps[:], sgt[k], ALU.mult, [Bps, Bsgt[k]], [Bprod[k % 2]])
                self.tt("pool", msl, pr, msl, ALU.add, [Bprod[k % 2], BmT[ncx][tb]], [BmT[ncx][tb]])
        wv = self.w_out[l].rearrange("(c p) n -> p c n", p=128)

        def loadwo(dcx):
            s_ = dcx % 3
            S.dma("pool", Wo[s_], wv[:, :, dcx * 128:(dcx + 1) * 128], [], [BWo[s_]], self.ds[16 + s_])

        oitems = [(dcx, tb) for dcx in range(16) for tb in range(4)]

        def loadx(ii):
            dcx, tb = oitems[ii]
            k = ii % 4
            S.dma("sp", xt[k], xsrc[dcx * 128:(dcx + 1) * 128, tb * 512:(tb + 1) * 512], [self.Bx[dcx][tb]], [Bxt[k]], self.ds[19 + k])

        loadwo(0)
        loadwo(1)
        loadx(0)
        loadx(1)
        for ii, (dcx, tb) in enumerate(oitems):
            if tb == 0 and dcx + 2 < 16:
                loadwo(dcx + 2)
            if ii + 2 < len(oitems):
                loadx(ii + 2)
            s_ = dcx % 3
            k = ii % 4
            ps = bank[4 + k]
            Bps = Bbank[4 + k]
            for c in range(16):
                self.mm(ps[:], Wo[s_][:, c, :], mT[:, c, tb * 512:(tb + 1) * 512], c == 0, c == 15,
                        [BWo[s_], BmT[c][tb]], [Bps], inc=(c == 15))
            self.tt("dve", ost[k], ps[:], xt[k], ALU.add, [Bps, Bxt[k]], [Bost[k]])
            S.dma("sp", self.xres[dcx * 128:(dcx + 1) * 128, tb * 512:(tb + 1) * 512], ost[k], [Bost[k]], [self.Bx[dcx][tb]], self.ds[23 + k])

    def phase_ffn_up(self, l):
        S = self.S
        hT, BhT = self.hT, self.BhT
        bank, Bbank = self.bank, self.Bbank
        NW = 3
        Wa = [self.alloc([128, 16, 128], BF16) for _ in range(NW)]
        Wb = [self.alloc([128, 16, 128], BF16) for _ in range(NW)]
        BWa, BWb = self.bufs(NW), self.bufs(NW)
        ua = [self.alloc([128, 2 + SEQ], F32) for _ in range(2)]
        ub = [self.alloc([128, 2 + SEQ], F32) for _ in range(2)]
        Bua = [[Buf() for _ in range(5)] for _ in range(2)]
        Bub = [[Buf() for _ in range(5)] for _ in range(2)]
        ta = [self.alloc([128, 512], F32) for _ in range(2)]
        tb_ = [self.alloc([128, 512], F32) for _ in range(2)]
        Bta, Btb = self.bufs(2), self.bufs(2)
        sl_ = [self.alloc([128, 512], F32) for _ in range(2)]
        Bsl = self.bufs(2)
        ast = [self.alloc([128, SEQ], BF16) for _ in range(2)]
        Bast = self.bufs(2)
        wv = self.w_up[l].rearrange("(c p) n -> p c n", p=128)
        for s_ in range(2):
            self.memset("dve", ua[s_][:, 0:2], 0.0, [], [Bua[s_][4]])
            self.memset("dve", ub[s_][:, 0:2], 0.0, [], [Bub[s_][4]])

        def loadw(fc):
            s = fc % NW
            S.dma("pool", Wa[s], wv[:, :, fc * 128:(fc + 1) * 128], [], [BWa[s]], self.ds[0 + s])
            S.dma("pool", Wb[s], wv[:, :, D_FF + fc * 128:D_FF + (fc + 1) * 128], [], [BWb[s]], self.ds[3 + s])
        loadw(0)
        loadw(1)
        cnt = 0
        cw, cb = self.convw, self.convb
        for fc in range(NFC):
            if fc + 2 < NFC:
                loadw(fc + 2)
            s = fc % NW
            u = fc % 2
            for tb in range(4):
                for (W_, BW_, uu, Buu, tt_, Btt, ch, pb) in ((Wa[s], BWa[s], ua[u], Bua[u], ta, Bta, fc, 0),
                                                            (Wb[s], BWb[s], ub[u], Bub[u], tb_, Btb, NFC + fc, 1)):
                    k = cnt % 4
                    cnt += 1
                    ps = bank[k]
                    Bps = Bbank[k]
                    for c in range(16):
                        self.mm(ps[:], W_[:, c, :], hT[:, c, tb * 512:(tb + 1) * 512], c == 0, c == 15,
                                [BW_, BhT[c][2 * tb], BhT[c][2 * tb + 1]], [Bps], inc=(c == 15))
                    self.cp("act", uu[:, 2 + tb * 512:2 + (tb + 1) * 512], ps[:], [Bps], [Buu[tb]])
                    prev = [Buu[tb - 1]] if tb > 0 else [Buu[4]]
                    t_ = tt_[tb % 2]
                    Bt_ = Btt[tb % 2]
                    self.ts("dve", t_, uu[:, 2 + tb * 512:2 + (tb + 1) * 512], cw[:, ch, 2:3], cb[:, ch:ch + 1], ALU.mult, ALU.add,
                            [Buu[tb], self.Blayer], [Bt_])
                    self.stt(t_, uu[:, 1 + tb * 512:1 + (tb + 1) * 512], cw[:, ch, 1:2], t_, ALU.mult, ALU.add,
                             [Buu[tb], self.Blayer, Bt_] + prev, [Bt_])
                    self.stt(t_, uu[:, tb * 512:(tb + 1) * 512], cw[:, ch, 0:1], t_, ALU.mult, ALU.add,
                             [Buu[tb], self.Blayer, Bt_] + prev, [Bt_])
                sx = sl_[tb % 2]
                self.act(sx, ta[tb % 2], AF.Silu, [Bta[tb % 2]], [Bsl[tb % 2]])
                self.tt("pool", ast[u][:, tb * 512:(tb + 1) * 512], sx, tb_[tb % 2], ALU.mult, [Bsl[tb % 2], Btb[tb % 2]], [Bast[u]])
            S.dma("sp", self.actT[fc * 128:(fc + 1) * 128, :], ast[u], [Bast[u]], self.Bact[fc], self.ds[6 + u])

    def phase_ffn_down(self, l, xdst):
        S = self.S
        self.arena_reset()
        bank, Bbank = self.bank, self.Bbank
        NSB = 11
        Wd = [self.alloc([128, NFC, 512], BF16) for _ in range(2)]
        BWd = [self.bufs(4) for _ in range(2)]
        At = self.alloc([128, NFC, 512], BF16)
        BAt = self.bufs(NSB)
        xt = [self.alloc([128, 512], F32) for _ in range(8)]
        Bxt = self.bufs(8)
        ost = [self.alloc([128, 512], F32) for _ in range(4)]
        Bost = self.bufs(4)
        wv = self.w_down[l].rearrange("(c p) n -> p c n", p=128)
        av = self.actT.rearrange("(c p) t -> p c t", p=128)

        def loadw(dg):
            for sblk in range(4):
                cs = slice(sblk * 11, (sblk + 1) * 11)
                S.dma("pool", Wd[dg % 2][:, cs, :], wv[:, cs, dg * 512:(dg + 1) * 512], [], [BWd[dg % 2][sblk]], self.ds[0 + (dg % 2) * 4 + sblk])

        its = [(dg, tb) for dg in range(4) for tb in range(4)]

        def loada(ii, sblk):
            dg, tb = its[ii]
            cs = slice(sblk * 4, (sblk + 1) * 4)
            S.dma("sp", At[:, cs, :], av[:, cs, tb * 512:(tb + 1) * 512],
                  [self.Bact[c][tb] for c in range(sblk * 4, (sblk + 1) * 4)], [BAt[sblk]], self.ds[8 + sblk])

        def loadx(ii):
            dg, tb = its[ii]
            for dcl in range(4):
                dcx = dg * 4 + dcl
                k = (ii % 2) * 4 + dcl
                S.dma("sp", xt[k], self.xres[dcx * 128:(dcx + 1) * 128, tb * 512:(tb + 1) * 512], [self.Bx[dcx][tb]], [Bxt[k]], self.ds[19 + k])

        loadw(0)
        for sblk in range(NSB):
            loada(0, sblk)
        loadx(0)
        for ii, (dg, tb) in enumerate(its):
            if tb == 0 and dg + 1 < 4:
                loadw(dg + 1)
            if ii + 1 < len(its):
                loadx(ii + 1)
            pb = (ii % 2) * 4
            for sblk in range(NSB):
                for dcl in range(4):
                    for c in range(sblk * 4, (sblk + 1) * 4):
                        self.mm(bank[pb + dcl][:], Wd[dg % 2][:, c, dcl * 128:(dcl + 1) * 128], At[:, c, :], c == 0, c == NFC - 1,
                                [BWd[dg % 2][c // 11], BAt[sblk]], [Bbank[pb + dcl]],
                                inc=(c == NFC - 1 or (dcl == 3 and c == sblk * 4 + 3)))
                if ii + 1 < len(its):
                    loada(ii + 1, sblk)
            for dcl in range(4):
                dcx = dg * 4 + dcl
                k = pb + dcl
                ko = dcl
                self.tt("dve", ost[ko], bank[pb + dcl][:], xt[k], ALU.add, [Bbank[pb + dcl], Bxt[k]], [Bost[ko]])
                S.dma("sp", xdst[dcx * 128:(dcx + 1) * 128, tb * 512:(tb + 1) * 512], ost[ko], [Bost[ko]], [self.Bx[dcx][tb]], self.ds[27 + ko])


def _const_inputs():
    f32 = np.float32
    pos = np.arange(SEQ, dtype=f32)
    ret_freq = (1.0 / (10000.0 ** np.linspace(0.0, 1.0, 64, dtype=f32))).astype(f32)
    rope_freq = (1.0 / (10000.0 ** (np.arange(0, 128, 2, dtype=f32) / f32(128)))).astype(f32)
    tabs = np.zeros((128, 4, 16, 64), f32)
    for ti, fr in ((0, ret_freq), (2, rope_freq)):
        ang = (pos[:, None] * fr[None, :]).astype(f32)
        c = np.cos(ang).astype(f32).reshape(16, 128, 64).transpose(1, 0, 2)
        s = np.sin(ang).astype(f32).reshape(16, 128, 64).transpose(1, 0, 2)
        tabs[:, ti] = c
        tabs[:, ti + 1] = s
    h = np.arange(8, dtype=np.float64)
    log_gamma = np.log1p(-np.exp2(-5.0 - h))
    p = np.arange(128, dtype=np.float64)
    small = np.zeros((128, 24), f32)
    small[:, 0:8] = np.exp((p[:, None] + 1.0) * log_gamma[None, :])
    small[:, 8:16] = (128.0 ** -0.5) * np.exp(-(p[:, None] + 1.0) * log_gamma[None, :])
    small[:, 16:24] = np.exp(128.0 * log_gamma)[None, :]
    j = np.arange(128)
    mask = (j[None, :] >= j[:, None]).astype(f32)
    ident = np.eye(128, dtype=f32)
    negm = np.zeros((128, 8, 8), f32)
    selfix = np.zeros((128, 2, 8, 8), f32)
    for bq in range(8):
        for n in range(8):
            negm[:, bq, n] = 0.0 if n < bq else -1e30
            selfix[:, 0, bq, n] = 1.0 if n < bq else 0.0
            selfix[:, 1, bq, n] = 1.0 if n == bq else 0.0
    return dict(c_tabs=tabs, c_small=small, c_mask=mask, c_ident=ident, c_negm=negm, c_selfix=selfix)


def _layout_inputs(inputs):
    f32 = np.float32
    A = lambda k: np.ascontiguousarray(np.asarray(inputs[k], dtype=f32))
    shared = {}
    for k in ("w_in", "w_pa", "w_pb", "w_pc", "w_out", "w_up", "w_down"):
        shared[k] = A(k)
    shared["norm_mix_t"] = np.ascontiguousarray(A("norm_mix").reshape(DEPTH, 16, 128).transpose(0, 2, 1))
    shared["norm_ffn_t"] = np.ascontiguousarray(A("norm_ffn").reshape(DEPTH, 16, 128).transpose(0, 2, 1))
    shared["b_ig_t"] = A("b_ig").reshape(DEPTH, 8, 1)
    shared["b_fg_t"] = A("b_fg").reshape(DEPTH, 8, 1)
    shared["ret_gn_rep"] = np.ascontiguousarray(np.broadcast_to(A("ret_gn")[:, None, :], (DEPTH, 128, 1024)))
    shared["ml_norm_rep"] = np.ascontiguousarray(np.broadcast_to(A("ml_norm")[:, None, :], (DEPTH, 128, 1024)))
    shared["q_norm_rep"] = np.ascontiguousarray(np.broadcast_to(A("q_norm")[:, None, :], (DEPTH, 128, 128)))
    shared["k_norm_rep"] = np.ascontiguousarray(np.broadcast_to(A("k_norm")[:, None, :], (DEPTH, 128, 128)))
    cw = A("conv_w")
    shared["conv_w_t"] = np.ascontiguousarray(cw.reshape(DEPTH, 3, 88, 128).transpose(0, 3, 2, 1))
    shared["conv_b_t"] = np.ascontiguousarray(A("conv_b").reshape(DEPTH, 88, 128).transpose(0, 2, 1))
    shared.update(_const_inputs())
    return shared


_NC_CACHE = {}


def _get_nc(n_layers=DEPTH, dbg=False, stop_after=None):
    key = (n_layers, dbg, stop_after)
    if key not in _NC_CACHE:
        kb = KB(n_layers, dbg)
        kb.stop_after = stop_after
        _NC_CACHE[key] = (kb.build(), kb)
    return _NC_CACHE[key][0]


def kernel(**inputs):
    x = np.asarray(inputs["x"], dtype=np.float32)
    B = x.shape[0]
    shared = _layout_inputs(inputs)
    nc = _get_nc()
    in_maps = []
    for b in range(B):
        m = dict(shared)
        m["xT"] = np.ascontiguousarray(x[b].T)
        in_maps.append(m)
    res = run_bass_kernel_spmd(nc, in_maps, core_ids=list(range(B)))
    out = np.stack([np.ascontiguousarray(np.asarray(r["outT"]).T) for r in res.results], axis=0)
    return out.astype(np.float32)
```
